# Optimizing a Trainium2 kernel written in Bass

```python
import jax, jax.numpy as jnp
from jax import lax
import numpy as np

D_MODEL = 2048
BATCH = 4
SEQ = 4096
DEPTH = 4

N_MIXERS = 3
N_POOL_LAYERS = (DEPTH + 2) // 3
N_HGRN_LAYERS = (DEPTH + 1) // 3
N_RET_LAYERS = DEPTH // 3

EPS = 1e-6

POOL_WINDOWS = (2, 4, 8, 16)
POOL_GROUPS = len(POOL_WINDOWS)
POOL_GROUP_DIM = D_MODEL // POOL_GROUPS

HGRN_EXPAND = 128
HGRN_HEADS = D_MODEL // HGRN_EXPAND
HGRN_DK = HGRN_EXPAND
HGRN_DV = D_MODEL // HGRN_HEADS
HGRN_QK = HGRN_HEADS * HGRN_DK
HGRN_V = HGRN_HEADS * HGRN_DV
HGRN_IN = 2 * HGRN_QK + 2 * HGRN_V
HGRN_CHUNK = 16

RET_HEADS = 8
RET_DK = D_MODEL // RET_HEADS
RET_DV = 2 * RET_DK
RET_QK = RET_HEADS * RET_DK
RET_V = RET_HEADS * RET_DV
RET_IN = 2 * RET_QK + 2 * RET_V
RET_CHUNK = 64
ROPE_BASE = 10000.0

FFN_HIDDEN = -(-8 * D_MODEL // (3 * 256)) * 256

kernel_name = "hybrid_pool_hgrn2_retention_adaln"

F32 = jnp.float32


def _rms_norm(x, gain):
    xf = x.astype(F32)
    y = xf * lax.rsqrt(jnp.mean(xf * xf, axis=-1, keepdims=True) + EPS)
    return (y * gain.astype(F32)).astype(x.dtype)


def _modulate(u, shift, scale):
    return u * (1.0 + scale[:, None, :]) + shift[:, None, :]


def _split_heads(t, n_heads):
    b, s, _ = t.shape
    return t.reshape(b, s, n_heads, -1).transpose(0, 2, 1, 3)


def _merge_heads(t):
    b, h, s, d = t.shape
    return t.transpose(0, 2, 1, 3).reshape(b, s, h * d)


def _head_rms_norm(o, gain):
    h, d = o.shape[1], o.shape[3]
    o = o * lax.rsqrt(jnp.mean(o * o, axis=-1, keepdims=True) + EPS)
    return o * gain.astype(F32).reshape(1, h, 1, d)


def _rotary(t, positions):
    half = t.shape[-1] // 2
    inv_freq = ROPE_BASE ** (-jnp.arange(half, dtype=F32) / half)
    ang = positions.astype(F32)[:, None, :, None] * inv_freq
    cos, sin = jnp.cos(ang), jnp.sin(ang)
    t1, t2 = t[..., :half], t[..., half:]
    return jnp.concatenate([t1 * cos - t2 * sin, t1 * sin + t2 * cos], axis=-1)


def _pool_mixer(u, w_group, scale):
    b, s, _ = u.shape
    uf = u.astype(F32)
    count = jnp.arange(1, s + 1, dtype=F32)
    parts = []
    for gi, w in enumerate(POOL_WINDOWS):
        xg = uf[..., gi * POOL_GROUP_DIM:(gi + 1) * POOL_GROUP_DIM]
        cs = jnp.cumsum(xg, axis=1)
        prev = jnp.pad(cs[:, :s - w], ((0, 0), (w, 0), (0, 0)))
        mean = (cs - prev) / jnp.minimum(count, float(w))[None, :, None]
        parts.append(mean - xg)
    p = jnp.stack(parts, axis=2).astype(u.dtype)
    y = jnp.einsum('bsgc,gce->bsge', p, w_group).reshape(b, s, D_MODEL)
    return y * scale


def _gla_chunkwise(q, k, v, log_f):
    b, h, s, dk = q.shape
    dv = v.shape[-1]
    c = HGRN_CHUNK
    n = s // c
    qc = q.reshape(b, h, n, c, dk)
    kc = k.reshape(b, h, n, c, dk)
    vc = v.reshape(b, h, n, c, dv)
    cum = jnp.cumsum(log_f.reshape(b, h, n, c, dk), axis=3)
    cum_last = cum[:, :, :, -1:, :]
    q_dec = qc * jnp.exp(cum)
    k_inv = kc * jnp.exp(-cum)
    k_end = kc * jnp.exp(cum_last - cum)
    decay = jnp.exp(cum_last[:, :, :, 0, :])
    causal = jnp.tril(jnp.ones((c, c), dtype=bool))
    scores = jnp.where(causal, jnp.einsum('bhncd,bhnsd->bhncs', q_dec, k_inv), 0.0)
    o_intra = jnp.einsum('bhncs,bhnsv->bhncv', scores, vc)

    def step(state, xs):
        q_n, k_n, v_n, dec_n = xs
        o_n = jnp.einsum('bhcd,bhdv->bhcv', q_n, state)
        state = dec_n[..., None] * state + jnp.einsum('bhcd,bhcv->bhdv', k_n, v_n)
        return state, o_n

    xs = (jnp.moveaxis(q_dec, 2, 0), jnp.moveaxis(k_end, 2, 0),
          jnp.moveaxis(vc, 2, 0), jnp.moveaxis(decay, 2, 0))
    _, o_inter = lax.scan(step, jnp.zeros((b, h, dk, dv), F32), xs)
    o = o_intra + jnp.moveaxis(o_inter, 0, 2)
    return o.reshape(b, h, s, dv)


def _hgrn2_mixer(u, w_in, lower_bound, norm_g, w_out):
    proj = (u @ w_in).astype(F32)
    q, f_logit, i, g = jnp.split(proj, [HGRN_QK, 2 * HGRN_QK, 2 * HGRN_QK + HGRN_V], axis=-1)
    lb = lower_bound.astype(F32)
    log_f = jnp.log(lb + (1.0 - lb) * jax.nn.sigmoid(f_logit))
    k = (1.0 - lb) * jax.nn.sigmoid(-f_logit)
    o = _gla_chunkwise(_split_heads(q, HGRN_HEADS), _split_heads(k, HGRN_HEADS),
                       _split_heads(i, HGRN_HEADS), _split_heads(log_f, HGRN_HEADS))
    o = _merge_heads(_head_rms_norm(o, norm_g)) * jax.nn.silu(g)
    return o.astype(u.dtype) @ w_out


def _retention_chunkwise(q, k, v):
    b, h, s, dk = q.shape
    dv = v.shape[-1]
    c = RET_CHUNK
    n = s // c
    log_gamma = jnp.log(1.0 - 2.0 ** (-5.0 - jnp.arange(h, dtype=F32)))
    idx = jnp.arange(c, dtype=F32)
    diff = idx[:, None] - idx[None, :]
    dmat = jnp.where(diff >= 0, jnp.exp(log_gamma[:, None, None] * jnp.maximum(diff, 0.0)), 0.0)
    qc = q.reshape(b, h, n, c, dk)
    kc = k.reshape(b, h, n, c, dk)
    vc = v.reshape(b, h, n, c, dv)
    scores = jnp.einsum('bhncd,bhnsd->bhncs', qc, kc) * dmat[None, :, None]
    o_intra = jnp.einsum('bhncs,bhnsv->bhncv', scores, vc)
    q_dec = qc * jnp.exp(log_gamma[:, None] * (idx + 1.0))[None, :, None, :, None]
    k_dec = kc * jnp.exp(log_gamma[:, None] * (c - 1.0 - idx))[None, :, None, :, None]
    chunk_decay = jnp.exp(log_gamma * c)[None, :, None, None]

    def step(state, xs):
        q_n, k_n, v_n = xs
        o_n = jnp.einsum('bhcd,bhdv->bhcv', q_n, state)
        state = chunk_decay * state + jnp.einsum('bhcd,bhcv->bhdv', k_n, v_n)
        return state, o_n

    xs = (jnp.moveaxis(q_dec, 2, 0), jnp.moveaxis(k_dec, 2, 0), jnp.moveaxis(vc, 2, 0))
    _, o_inter = lax.scan(step, jnp.zeros((b, h, dk, dv), F32), xs)
    o = o_intra + jnp.moveaxis(o_inter, 0, 2)
    return o.reshape(b, h, s, dv)


def _retention_mixer(u, positions, w_in, norm_g, w_out):
    proj = (u @ w_in).astype(F32)
    q, k, v, g = jnp.split(proj, [RET_QK, 2 * RET_QK, 2 * RET_QK + RET_V], axis=-1)
    q = _rotary(_split_heads(q, RET_HEADS), positions)
    k = _rotary(_split_heads(k, RET_HEADS), positions) * (RET_DK ** -0.5)
    v = _split_heads(v, RET_HEADS)
    o = _head_rms_norm(_retention_chunkwise(q, k, v), norm_g)
    o = _merge_heads(o) * jax.nn.silu(g)
    return o.astype(u.dtype) @ w_out


def _swiglu(u, w_in, w_out):
    gate, up = jnp.split(u @ w_in, 2, axis=-1)
    return (jax.nn.silu(gate) * up) @ w_out


def setup_inputs(seed: int = 0) -> dict:
    key = jax.random.key(seed)
    ks = jax.random.split(key, 24)
    d = D_MODEL
    nrm = jax.random.normal
    offsets = jax.random.randint(ks[2], (BATCH, 1), 0, 1024, dtype=jnp.int32)
    positions = (offsets + jnp.arange(SEQ, dtype=jnp.int32)[None, :]).astype(jnp.int32)
    return {
        "x": nrm(ks[0], (BATCH, SEQ, d), F32),
        "c": nrm(ks[1], (BATCH, d), F32),
        "positions": positions,
        "w_ada": nrm(ks[3], (DEPTH, d, 6 * d), F32) * (0.5 * d ** -0.5),
        "b_ada": nrm(ks[4], (DEPTH, 6 * d), F32) * 0.02,
        "norm_mix_g": 1.0 + 0.05 * nrm(ks[5], (DEPTH, d), F32),
        "norm_ffn_g": 1.0 + 0.05 * nrm(ks[6], (DEPTH, d), F32),
        "pool_w": nrm(ks[7], (N_POOL_LAYERS, POOL_GROUPS, POOL_GROUP_DIM, POOL_GROUP_DIM), F32) * POOL_GROUP_DIM ** -0.5,
        "pool_scale": 1.0 + 0.05 * nrm(ks[8], (N_POOL_LAYERS, d), F32),
        "hgrn_w_in": nrm(ks[9], (N_HGRN_LAYERS, d, HGRN_IN), F32) * d ** -0.5,
        "hgrn_lb_logits": 0.1 * nrm(ks[10], (DEPTH, HGRN_QK), F32),
        "hgrn_norm_g": 1.0 + 0.05 * nrm(ks[11], (N_HGRN_LAYERS, HGRN_V), F32),
        "hgrn_w_out": nrm(ks[12], (N_HGRN_LAYERS, HGRN_V, d), F32) * HGRN_V ** -0.5,
        "ret_w_in": nrm(ks[13], (N_RET_LAYERS, d, RET_IN), F32) * d ** -0.5,
        "ret_norm_g": 1.0 + 0.05 * nrm(ks[14], (N_RET_LAYERS, RET_V), F32),
        "ret_w_out": nrm(ks[15], (N_RET_LAYERS, RET_V, d), F32) * RET_V ** -0.5,
        "ffn_w_in": nrm(ks[16], (DEPTH, d, 2 * FFN_HIDDEN), F32) * d ** -0.5,
        "ffn_w_out": nrm(ks[17], (DEPTH, FFN_HIDDEN, d), F32) * FFN_HIDDEN ** -0.5,
        "final_norm_g": 1.0 + 0.05 * nrm(ks[18], (d,), F32),
    }


def reference(x, c, positions, w_ada, b_ada, norm_mix_g, norm_ffn_g,
              pool_w, pool_scale,
              hgrn_w_in, hgrn_lb_logits, hgrn_norm_g, hgrn_w_out,
              ret_w_in, ret_norm_g, ret_w_out,
              ffn_w_in, ffn_w_out, final_norm_g):
    ada = jnp.einsum('bd,lde->lbe', jax.nn.silu(c), w_ada) + b_ada[:, None, :]
    ada = ada.astype(x.dtype)
    lb_soft = jax.nn.softmax(hgrn_lb_logits.astype(F32), axis=0)
    lb_all = jnp.cumsum(lb_soft, axis=0) - lb_soft[0:1]
    h = x
    for i in range(DEPTH):
        shift_m, scale_m, gate_m, shift_f, scale_f, gate_f = jnp.split(ada[i], 6, axis=-1)
        u = _modulate(_rms_norm(h, norm_mix_g[i]), shift_m, scale_m)
        kind, j = i % N_MIXERS, i // N_MIXERS
        if kind == 0:
            y = _pool_mixer(u, pool_w[j], pool_scale[j])
        elif kind == 1:
            y = _hgrn2_mixer(u, hgrn_w_in[j], lb_all[i], hgrn_norm_g[j], hgrn_w_out[j])
        else:
            y = _retention_mixer(u, positions, ret_w_in[j], ret_norm_g[j], ret_w_out[j])
        h = h + gate_m[:, None, :] * y.astype(h.dtype)
        u = _modulate(_rms_norm(h, norm_ffn_g[i]), shift_f, scale_f)
        h = h + gate_f[:, None, :] * _swiglu(u, ffn_w_in[i], ffn_w_out[i]).astype(h.dtype)
    return _rms_norm(h, final_norm_g)
```

```python
from contextlib import ExitStack
import math
import numpy as np
import concourse.bass as bass
import concourse.mybir as mybir
from concourse.bass_utils import run_bass_kernel_spmd

F32 = mybir.dt.float32
BF16 = mybir.dt.bfloat16
I32 = mybir.dt.int32
AF = mybir.ActivationFunctionType
ALU = mybir.AluOpType

D = 2048
KC = 16
DEPTH = 4
FH = 5632
FHC = 44
EPS = 1e-6
SEG = 1024
ENGS = ['pe', 'act', 'dve', 'pool', 'sp']
DEBUG = False


class Op:
    __slots__ = ('eng', 'fn', 'deps', 'dma', 'flag', 'event')

    def __init__(self, eng, fn, deps, dma):
        self.eng, self.fn, self.deps, self.dma = eng, fn, deps, dma
        self.flag = False
        self.event = None


class Prog:
    def __init__(self, nc, stack):
        self.nc = nc
        self.stack = stack
        self.eobj = {'pe': nc.tensor, 'act': nc.scalar, 'dve': nc.vector, 'pool': nc.gpsimd, 'sp': nc.sync}
        self.esem = {e: stack.enter_context(nc.semaphore('es_' + e)) for e in ENGS}
        self.ecnt = {e: 0 for e in ENGS}
        self.dsem = {}
        self.dcnt = {}
        self.known = {e: {} for e in ENGS}
        self.nphase = 0
        self.reset()

    def reset(self):
        self.ops = []
        self.lastw = {}
        self.rd = {}

    def op(self, eng, fn, reads=(), writes=(), dma=None):
        idx = len(self.ops)
        deps = set()
        for r in reads:
            w = self.lastw.get(r)
            if w is not None:
                deps.add(w)
        for wk in writes:
            w = self.lastw.get(wk)
            if w is not None:
                deps.add(w)
            rr = self.rd.get(wk)
            if rr:
                deps.update(rr.values())
        for r in reads:
            d = self.rd.setdefault(r, {})
            d[(eng, dma)] = idx
        for wk in writes:
            self.lastw[wk] = idx
            self.rd[wk] = {}
        deps.discard(idx)
        if eng == 'pe':
            deps = {d for d in deps if not (self.ops[d].eng == 'pe' and self.ops[d].dma is None)}
        self.ops.append(Op(eng, fn, deps, dma))
        return idx

    def _dsem(self, key):
        if key not in self.dsem:
            self.dsem[key] = self.stack.enter_context(self.nc.semaphore('ds_' + key))
            self.dcnt[key] = 0
        return self.dsem[key]

    def emit(self):
        ops = self.ops
        if not ops:
            return
        for o in ops:
            for d in o.deps:
                ops[d].flag = True
        for o in ops:
            if o.dma is not None:
                s = self._dsem(o.dma)
                self.dcnt[o.dma] += 16
                o.event = (o.dma, s, self.dcnt[o.dma], 16)
            elif o.flag:
                self.ecnt[o.eng] += 1
                o.event = ('e_' + o.eng, self.esem[o.eng], self.ecnt[o.eng], 1)
        per = {e: [] for e in ENGS}
        for o in ops:
            per[o.eng].append(o)

        def run(e, eng):
            kn = self.known[e]
            final = {}
            for o in per[e]:
                for d in sorted(o.deps):
                    name, s, val, _ = ops[d].event
                    if kn.get(name, 0) < val:
                        eng.wait_ge(s, val)
                        kn[name] = val
                ins = o.fn(eng)
                if o.event is not None:
                    name, s, val, inc = o.event
                    ins.then_inc(s, inc)
                    if o.dma is not None:
                        final[name] = (s, val)
            for name, (s, val) in final.items():
                if kn.get(name, 0) < val:
                    eng.wait_ge(s, val)
                    kn[name] = val

        with self.nc.Block() as block:
            if per['pe']:
                block.tensor(lambda eng: run('pe', eng))
            if per['act']:
                block.scalar(lambda eng: run('act', eng))
            if per['dve']:
                block.vector(lambda eng: run('dve', eng))
            if per['pool']:
                block.gpsimd(lambda eng: run('pool', eng))
            if per['sp']:
                block.sync(lambda eng: run('sp', eng))
        self.nphase += 1
        self.reset()


class Ctx:
    pass


def build_program(NT, layers=(0, 1, 2, 3), do_mixer=True, do_ffn=True):
    nc = bass.Bass("TRN2", target_bir_lowering=False)
    C = Ctx()
    C.nc = nc
    C.NT = NT
    dt_in = lambda n, s, d=F32: nc.dram_tensor(n, s, d, kind="ExternalInput").ap()
    x = dt_in("x", [NT, D])
    cvec = dt_in("c", [KC, 128])
    pos = dt_in("positions", [1, NT], I32)
    w_ada = dt_in("w_ada", [DEPTH, D, 6 * D])
    b_ada = dt_in("b_ada", [DEPTH, 6 * D])
    norm_mix_g = dt_in("norm_mix_g", [DEPTH * KC, 128])
    norm_ffn_g = dt_in("norm_ffn_g", [DEPTH * KC, 128])
    pool_w = dt_in("pool_w", [2, 4, 512, 512])
    pool_scale = dt_in("pool_scale", [2 * KC, 128])
    hgrn_w_in = dt_in("hgrn_w_in", [D, 8192])
    hgrn_lb = dt_in("hgrn_lb_logits", [DEPTH * KC, 128])
    hgrn_norm_g = dt_in("hgrn_norm_g", [KC, 128])
    hgrn_w_out = dt_in("hgrn_w_out", [D, D])
    ret_w_in = dt_in("ret_w_in", [D, 12288])
    ret_norm_g = dt_in("ret_norm_g", [32, 128])
    ret_w_out = dt_in("ret_w_out", [4096, D])
    ffn_w_in = dt_in("ffn_w_in", [DEPTH, D, 2 * FH])
    ffn_w_out = dt_in("ffn_w_out", [DEPTH, FH, D])
    final_g = dt_in("final_norm_g", [KC, 128])
    ident_d = dt_in("ident", [128, 128])
    invf_d = dt_in("invf", [128, 1])
    gtab_d = dt_in("gtab", [1, 8 * 3 * 128])
    out = nc.dram_tensor("out", [NT, D], F32, kind="ExternalOutput").ap()
    hT = nc.dram_tensor("hT", [D, NT], F32, kind="Internal").ap()
    ada_d = nc.dram_tensor("ada_d", [DEPTH * 96, 128], F32, kind="Internal").ap()
    hT3 = hT.rearrange("(c p) t -> p c t", p=128)

    with ExitStack() as gs:
        P = Prog(nc, gs)
        _cnt = [0]

        def sb(st, name, shape, dt):
            _cnt[0] += 1
            return st.enter_context(nc.sbuf_tensor("%s_%d" % (name, _cnt[0]), shape, dt))
        ps = [gs.enter_context(nc.psum_tensor("ps%d" % i, [128, 512], F32)) for i in range(8)]
        PK = lambda b: ('ps', b)
        ident = sb(gs, "ident", [128, 128], F32)
        identb = sb(gs, "identb", [128, 128], BF16)
        ones32 = sb(gs, "ones32", [128, 128], F32)
        epsc = sb(gs, "epsc", [128, 1], F32)
        gmix = sb(gs, "gmix", [128, 64], F32)
        gffn = sb(gs, "gffn", [128, 64], F32)
        pscl = sb(gs, "pscl", [128, 32], F32)
        lbl = sb(gs, "lbl", [128, 64], F32)
        hng = sb(gs, "hng", [128, 16], F32)
        rng_ = sb(gs, "rng", [128, 32], F32)
        fng = sb(gs, "fng", [128, 16], F32)
        adac = sb(gs, "adac", [128, DEPTH * 96], F32)
        modA = sb(gs, "modA", [128, DEPTH * 2 * 16], F32)

        with ExitStack() as st:
            rows = sb(st, "rows", [128, 128], F32)
            P.op('sp', lambda e: e.dma_start(out=ident[:], in_=ident_d), writes=['ident'], dma='c0')
            P.op('pool', lambda e: e.memset(ones32[:], 1.0), writes=['ones32'])
            P.op('pool', lambda e: e.memset(epsc[:], EPS), writes=['epsc'])
            P.op('dve', lambda e: e.tensor_copy(identb[:], ident[:]), reads=['ident'], writes=['identb'])
            cc = sb(st, "cc", [128, 16], F32)
            scb = sb(st, "scb", [128, 16], BF16)

            def to_cols(src_rows_ap, R, dst_ap, key):
                P.op('sp', lambda e: e.dma_start(out=rows[0:R, :], in_=src_rows_ap), writes=['rows'], dma='c1')
                P.op('pe', lambda e: e.transpose(ps[0][:, 0:R], rows[0:R, :], ident[0:R, 0:R]),
                     reads=['rows', 'ident'], writes=[PK(0)])
                P.op('dve', lambda e: e.tensor_copy(dst_ap, ps[0][:, 0:R]), reads=[PK(0)], writes=[key])

            to_cols(norm_mix_g, 64, gmix[:], 'gmix')
            to_cols(norm_ffn_g, 64, gffn[:], 'gffn')
            to_cols(pool_scale, 32, pscl[:], 'pscl')
            to_cols(hgrn_lb, 64, lbl[:], 'lbl')
            to_cols(hgrn_norm_g, 16, hng[:], 'hng')
            to_cols(ret_norm_g, 32, rng_[:], 'rng')
            to_cols(final_g, 16, fng[:], 'fng')
            to_cols(cvec, 16, cc[:], 'cc')
            P.op('act', lambda e: e.activation(out=scb[:], in_=cc[:], func=AF.Silu), reads=['cc'], writes=['scb'])
            arow = sb(st, "arow", [1, 6 * D], F32)
            brow = sb(st, "brow", [1, 6 * D], F32)
            wab = [sb(st, "wab%d" % i, [128, KC, 512], BF16) for i in range(2)]
            nb = 0
            for l in range(DEPTH):
                P.op('sp', lambda e, l=l: e.dma_start(out=brow[:], in_=b_ada[l:l + 1, :]), writes=['brow'], dma='c2')
                for j in range(24):
                    s = nb % 2
                    src = w_ada[l, :, j * 512:(j + 1) * 512].rearrange("(kc p) m -> p kc m", p=128)
                    P.op('pool', lambda e, s=s, src=src: e.dma_start(out=wab[s][:], in_=src),
                         writes=[('wab', s)], dma='wa%d' % s)
                    b = nb % 4
                    for kc in range(KC):
                        P.op('pe', lambda e, s=s, b=b, kc=kc: e.matmul(ps[b][0:1, :], scb[:, kc:kc + 1], wab[s][:, kc, :],
                                                                      start=(kc == 0), stop=(kc == KC - 1)),
                             reads=[('wab', s), 'scb'], writes=[PK(b)])
                    P.op('dve', lambda e, b=b, j=j: e.tensor_tensor(out=arow[:, j * 512:(j + 1) * 512], in0=ps[b][0:1, :],
                                                                    in1=brow[:, j * 512:(j + 1) * 512], op=ALU.add),
                         reads=[PK(b), 'brow'], writes=['arow'])
                    nb += 1
                P.op('sp', lambda e, l=l: e.dma_start(out=ada_d[l * 96:(l + 1) * 96, :].rearrange("(o r) c -> o (r c)", o=1),
                                                      in_=arow[:]), reads=['arow'], writes=['ada_d'], dma='c3')
            P.emit()
            for i in range(3):
                to_cols(ada_d[i * 128:(i + 1) * 128, :], 128, adac[:, i * 128:(i + 1) * 128], 'adac')
            for l in range(DEPTH):
                for sub, gt in ((0, gmix), (1, gffn)):
                    sc = adac[:, l * 96 + (1 + 3 * sub) * 16: l * 96 + (2 + 3 * sub) * 16]
                    dst = modA[:, (l * 2 + sub) * 16:(l * 2 + sub + 1) * 16]
                    P.op('dve', lambda e, sc=sc, dst=dst, gt=gt, l=l: e.scalar_tensor_tensor(
                        out=dst, in0=sc, scalar=1.0, in1=gt[:, l * 16:(l + 1) * 16], op0=ALU.add, op1=ALU.mult),
                        reads=['adac', 'gmix', 'gffn'], writes=['modA'])
            P.emit()

        ada_col = lambda l, k, c: adac[:, l * 96 + k * 16 + c: l * 96 + k * 16 + c + 1]

        with ExitStack() as st:
            xt = [sb(st, "xt%d" % i, [128, D], F32) for i in range(2)]
            stg = [sb(st, "stg%d" % i, [128, KC, 128], F32) for i in range(2)]
            for ti in range(NT // 128):
                s = ti % 2
                P.op('sp', lambda e, s=s, ti=ti: e.dma_start(out=xt[s][:], in_=x[ti * 128:(ti + 1) * 128, :]),
                     writes=[('xt', s)], dma='xt%d' % s)
                for q in range(4):
                    b = (ti * 4 + q) % 8
                    for i in range(4):
                        c = q * 4 + i
                        P.op('pe', lambda e, s=s, b=b, i=i, c=c: e.transpose(ps[b][:, i * 128:(i + 1) * 128],
                                                                             xt[s][:, c * 128:(c + 1) * 128], ident[:]),
                             reads=[('xt', s), 'ident'], writes=[PK(b)])
                    eng = 'dve' if q % 2 == 0 else 'act'
                    if eng == 'dve':
                        P.op('dve', lambda e, s=s, b=b, q=q: e.tensor_copy(
                            stg[s][:, q * 4:(q + 1) * 4, :].rearrange("p a b -> p (a b)"), ps[b][:]),
                            reads=[PK(b)], writes=[('stg', s)])
                    else:
                        P.op('act', lambda e, s=s, b=b, q=q: e.copy(
                            stg[s][:, q * 4:(q + 1) * 4, :].rearrange("p a b -> p (a b)"), ps[b][:]),
                            reads=[PK(b)], writes=[('stg', s)])
                P.op('sp', lambda e, s=s, ti=ti: e.dma_start(out=hT3[:, :, ti * 128:(ti + 1) * 128], in_=stg[s][:]),
                     reads=[('stg', s)], dma='st%d' % s)
            P.emit()

        def normmod(st_out, t0, N, Acol, Bcol, xT, xkey, halo=0):
            with ExitStack() as st:
                hall = sb(st, "hall", [128, KC, N], F32)
                sq = [sb(st, "sq%d" % i, [128, N], F32) for i in range(2)]
                rstd = sb(st, "rstd", [128, N], F32)
                tmp = [sb(st, "tmp%d" % i, [128, N], F32) for i in range(2)]
                nt = (N + 511) // 512
                for kc in range(KC):
                    P.op('sp', lambda e, kc=kc: e.dma_start(out=hall[:, kc, :], in_=hT[kc * 128:(kc + 1) * 128, t0:t0 + N]),
                         writes=[('hall', kc)], dma='ha%d' % (kc % 4))
                    s = kc % 2
                    P.op('act', lambda e, kc=kc, s=s: e.activation(out=sq[s][:], in_=hall[:, kc, :], func=AF.Square),
                         reads=[('hall', kc)], writes=[('sq', s)])
                    for n in range(nt):
                        w = min(512, N - n * 512)
                        P.op('pe', lambda e, kc=kc, s=s, n=n, w=w: e.matmul(ps[n][:, 0:w], ones32[:], sq[s][:, n * 512:n * 512 + w],
                                                                            start=(kc == 0), stop=(kc == KC - 1)),
                             reads=[('sq', s), 'ones32'], writes=[PK(n)])
                for n in range(nt):
                    w = min(512, N - n * 512)
                    P.op('act', lambda e, n=n, w=w: e.activation(out=rstd[:, n * 512:n * 512 + w], in_=ps[n][:, 0:w], func=AF.Ln,
                                                                 scale=1.0 / D, bias=epsc[:]),
                         reads=[PK(n), 'epsc'], writes=['rstd'])
                P.op('act', lambda e: e.activation(out=rstd[:], in_=rstd[:], func=AF.Exp, scale=-0.5),
                     reads=['rstd'], writes=['rstd'])
                for kc in range(KC):
                    s = kc % 2
                    P.op('dve', lambda e, kc=kc, s=s: e.scalar_tensor_tensor(out=tmp[s][:], in0=hall[:, kc, :], scalar=Acol(kc),
                                                                             in1=rstd[:], op0=ALU.mult, op1=ALU.mult),
                         reads=[('hall', kc), 'rstd', 'modA'], writes=[('tmp', s)])
                    P.op('act', lambda e, kc=kc, s=s: e.activation(out=xT[:, kc, halo:halo + N], in_=tmp[s][:], func=AF.Identity,
                                                                   bias=Bcol(kc), scale=1.0),
                         reads=[('tmp', s), 'adac'], writes=[(xkey, kc)])
                P.emit()

        def ffn_layer(l):
            for seg in range(NT // SEG):
                t0 = seg * SEG
                with ExitStack() as st:
                    xT = sb(st, "xT", [128, KC, SEG], BF16)
                    normmod(st, t0, SEG, lambda kc: modA[:, (l * 2 + 1) * 16 + kc:(l * 2 + 1) * 16 + kc + 1],
                            lambda kc: ada_col(l, 3, kc), xT, 'xT')
                    hid = sb(st, "hid", [128, FHC, SEG], BF16)
                    wg = [sb(st, "wg%d" % i, [128, KC, 128], BF16) for i in range(3)]
                    wu = [sb(st, "wu%d" % i, [128, KC, 128], BF16) for i in range(3)]
                    sg = [sb(st, "sg%d" % i, [128, 512], F32) for i in range(2)]
                    wo = [sb(st, "wo%d" % i, [128, FHC, 128], BF16) for i in range(2)]
                    hin = [sb(st, "hin%d" % i, [128, SEG], F32) for i in range(2)]
                    hout = [sb(st, "hout%d" % i, [128, SEG], F32) for i in range(2)]
                    NTT = SEG // 512
                    w_in_l = ffn_w_in[l]
                    w_out_l = ffn_w_out[l]

                    def load_in(j):
                        s = j % 3
                        srcg = w_in_l[:, j * 128:(j + 1) * 128].rearrange("(kc p) m -> p kc m", p=128)
                        srcu = w_in_l[:, FH + j * 128:FH + (j + 1) * 128].rearrange("(kc p) m -> p kc m", p=128)
                        P.op('pool', lambda e: e.dma_start(out=wg[s][:], in_=srcg), writes=[('wg', s)], dma='wg%d' % s)
                        P.op('pool', lambda e: e.dma_start(out=wu[s][:], in_=srcu), writes=[('wu', s)], dma='wu%d' % s)

                    def load_out(m):
                        s = m % 2
                        src = w_out_l[:, m * 128:(m + 1) * 128].rearrange("(kc p) m -> p kc m", p=128)
                        P.op('pool', lambda e: e.dma_start(out=wo[s][:], in_=src), writes=[('wo', s)], dma='wo%d' % s)

                    load_in(0)
                    load_in(1)
                    cnt = 0
                    for j in range(FHC):
                        if j + 2 < FHC:
                            load_in(j + 2)
                        elif j + 2 == FHC:
                            load_out(0)
                        elif j + 2 == FHC + 1:
                            load_out(1)
                        s = j % 3
                        for n in range(NTT):
                            bg = (cnt * 2) % 8
                            bu = bg + 1
                            cnt += 1
                            for kc in range(KC):
                                P.op('pe', lambda e, s=s, bg=bg, kc=kc, n=n: e.matmul(
                                    ps[bg][:], wg[s][:, kc, :], xT[:, kc, n * 512:(n + 1) * 512], start=(kc == 0), stop=(kc == KC - 1)),
                                    reads=[('wg', s), ('xT', kc)], writes=[PK(bg)])
                            for kc in range(KC):
                                P.op('pe', lambda e, s=s, bu=bu, kc=kc, n=n: e.matmul(
                                    ps[bu][:], wu[s][:, kc, :], xT[:, kc, n * 512:(n + 1) * 512], start=(kc == 0), stop=(kc == KC - 1)),
                                    reads=[('wu', s), ('xT', kc)], writes=[PK(bu)])
                            ss = cnt % 2
                            P.op('act', lambda e, ss=ss, bg=bg: e.activation(out=sg[ss][:], in_=ps[bg][:], func=AF.Silu),
                                 reads=[PK(bg)], writes=[('sg', ss)])
                            P.op('dve', lambda e, ss=ss, bu=bu, j=j, n=n: e.tensor_tensor(
                                out=hid[:, j, n * 512:(n + 1) * 512], in0=sg[ss][:], in1=ps[bu][:], op=ALU.mult),
                                reads=[('sg', ss), PK(bu)], writes=[('hid', j)])
                    for m in range(KC):
                        if m >= 1 and m + 1 < KC:
                            load_out(m + 1)
                        s = m % 2
                        P.op('sp', lambda e, s=s, m=m: e.dma_start(out=hin[s][:], in_=hT[m * 128:(m + 1) * 128, t0:t0 + SEG]),
                             writes=[('hin', s)], dma='hin%d' % s)
                        for n in range(NTT):
                            b = cnt % 8
                            cnt += 1
                            for kc in range(FHC):
                                P.op('pe', lambda e, s=s, b=b, kc=kc, n=n: e.matmul(
                                    ps[b][:], wo[s][:, kc, :], hid[:, kc, n * 512:(n + 1) * 512], start=(kc == 0), stop=(kc == FHC - 1)),
                                    reads=[('wo', s), ('hid', kc)], writes=[PK(b)])
                            P.op('dve', lambda e, s=s, b=b, m=m, n=n: e.scalar_tensor_tensor(
                                out=hout[s][:, n * 512:(n + 1) * 512], in0=ps[b][:], scalar=ada_col(l, 5, m),
                                in1=hin[s][:, n * 512:(n + 1) * 512], op0=ALU.mult, op1=ALU.add),
                                reads=[PK(b), ('hin', s), 'adac'], writes=[('hout', s)])
                        P.op('sp', lambda e, s=s, m=m: e.dma_start(out=hT[m * 128:(m + 1) * 128, t0:t0 + SEG], in_=hout[s][:]),
                             reads=[('hout', s)], dma='hout%d' % s)
                    P.emit()


        inv16 = sb(gs, "inv16", [128, 4, 16], F32)
        for g in range(4):
            w = 2 ** (g + 1)
            P.op('pool', lambda e, g=g, w=w: e.memset(inv16[:, g, :], 1.0 / w), writes=['inv16'])
            for t in range(w - 1):
                P.op('pool', lambda e, g=g, t=t: e.memset(inv16[:, g, t:t + 1], 1.0 / (t + 1)), writes=['inv16'])
        P.emit()

        def pool_layer(l):
            j = l // 3
            with ExitStack() as st0:
                gp = sb(st0, "gp", [128, 16], F32)
                P.op('dve', lambda e: e.tensor_tensor(out=gp[:], in0=adac[:, l * 96 + 32:l * 96 + 48],
                                                     in1=pscl[:, j * 16:(j + 1) * 16], op=ALU.mult),
                     reads=['adac', 'pscl'], writes=['gp'])
                halo_t = sb(st0, "halo_t", [128, KC, 16], F32)
                wp = sb(st0, "wp", [128, 4, 4, 512], BF16)
                for g in range(4):
                    P.op('pool', lambda e, g=g: e.dma_start(out=wp[:, g], in_=pool_w[j, g].rearrange("(kc p) m -> p kc m", p=128)),
                         writes=[('wp', g)], dma='wp')
                P.emit()
                for seg in range(NT // SEG):
                    t0 = seg * SEG
                    N = SEG
                    with ExitStack() as st:
                        uP = sb(st, "uP", [128, KC, 16 + N], F32)
                        Acol = lambda kc: modA[:, (l * 2) * 16 + kc:(l * 2) * 16 + kc + 1]
                        Bcol = lambda kc: ada_col(l, 0, kc)
                        if t0 == 0:
                            P.op('pool', lambda e: e.memset(uP[:, :, 0:16], 0.0), writes=[('uP', kc) for kc in range(KC)])
                        else:
                            P.op('pool', lambda e: e.tensor_copy(uP[:, :, 0:16], halo_t[:]), reads=['halo_t'],
                                 writes=[('uP', kc) for kc in range(KC)])
                        normmod(st, t0, N, Acol, Bcol, uP, 'uP', halo=16)
                        P.op('pool', lambda e: e.tensor_copy(halo_t[:], uP[:, :, N:N + 16]), reads=[('uP', kc) for kc in range(KC)],
                             writes=['halo_t'])
                        tA = sb(st, "tA", [128, 16 + N], F32)
                        tB = sb(st, "tB", [128, 16 + N], F32)
                        pT = sb(st, "pT", [128, KC, N], BF16)
                        hin = [sb(st, "hin%d" % i, [128, N], F32) for i in range(2)]
                        hout = [sb(st, "hout%d" % i, [128, N], F32) for i in range(2)]
                        E = 16 + N
                        for c in range(KC):
                            g = c // 4
                            w = 2 ** (g + 1)
                            u = uP[:, c, :]
                            P.op('dve', lambda e, u=u: e.tensor_tensor(out=tA[:, 1:E], in0=u[:, 1:E], in1=u[:, 0:E - 1], op=ALU.add),
                                 reads=[('uP', c)], writes=['tA'])
                            cur, curk = tA, 'tA'
                            if g >= 1:
                                P.op('dve', lambda e: e.tensor_tensor(out=tB[:, 3:E], in0=tA[:, 3:E], in1=tA[:, 1:E - 2], op=ALU.add),
                                     reads=['tA'], writes=['tB'])
                                cur, curk = tB, 'tB'
                            if g >= 2:
                                P.op('dve', lambda e: e.tensor_tensor(out=tA[:, 7:E], in0=tB[:, 7:E], in1=tB[:, 3:E - 4], op=ALU.add),
                                     reads=['tB'], writes=['tA'])
                                cur, curk = tA, 'tA'
                            if g >= 3:
                                P.op('dve', lambda e: e.tensor_tensor(out=tB[:, 15:E], in0=tA[:, 15:E], in1=tA[:, 7:E - 8], op=ALU.add),
                                     reads=['tA'], writes=['tB'])
                                cur, curk = tB, 'tB'
                            lo = 16 if t0 == 0 else 0
                            P.op('dve', lambda e, cur=cur, u=u, c=c, w=w, lo=lo: e.scalar_tensor_tensor(
                                out=pT[:, c, lo:N], in0=cur[:, 16 + lo:E], scalar=1.0 / w, in1=u[:, 16 + lo:E],
                                op0=ALU.mult, op1=ALU.subtract),
                                reads=[curk, ('uP', c)], writes=[('pT', c)])
                            if t0 == 0:
                                P.op('dve', lambda e, cur=cur, g=g: e.tensor_tensor(out=cur[:, 16:32], in0=cur[:, 16:32],
                                                                                    in1=inv16[:, g, :], op=ALU.mult),
                                     reads=[curk, 'inv16', ('pT', c)], writes=[curk])
                                P.op('dve', lambda e, cur=cur, u=u, c=c: e.tensor_tensor(out=pT[:, c, 0:16], in0=cur[:, 16:32],
                                                                                         in1=u[:, 16:32], op=ALU.subtract),
                                     reads=[curk, ('uP', c)], writes=[('pT', c)])
                        cnt = 0
                        for c in range(KC):
                            g, m = c // 4, c % 4
                            s = c % 2
                            P.op('sp', lambda e, s=s, c=c: e.dma_start(out=hin[s][:], in_=hT[c * 128:(c + 1) * 128, t0:t0 + N]),
                                 writes=[('hin', s)], dma='hin%d' % s)
                            for n in range(N // 512):
                                b = cnt % 8
                                cnt += 1
                                for kc in range(4):
                                    P.op('pe', lambda e, g=g, m=m, kc=kc, b=b, n=n: e.matmul(
                                        ps[b][:], wp[:, g, kc, m * 128:(m + 1) * 128], pT[:, 4 * g + kc, n * 512:(n + 1) * 512],
                                        start=(kc == 0), stop=(kc == 3)),
                                        reads=[('wp', g), ('pT', 4 * g + kc)], writes=[PK(b)])
                                P.op('dve', lambda e, s=s, b=b, c=c, n=n: e.scalar_tensor_tensor(
                                    out=hout[s][:, n * 512:(n + 1) * 512], in0=ps[b][:], scalar=gp[:, c:c + 1],
                                    in1=hin[s][:, n * 512:(n + 1) * 512], op0=ALU.mult, op1=ALU.add),
                                    reads=[PK(b), ('hin', s), 'gp'], writes=[('hout', s)])
                            P.op('sp', lambda e, s=s, c=c: e.dma_start(out=hT[c * 128:(c + 1) * 128, t0:t0 + N], in_=hout[s][:]),
                                 reads=[('hout', s)], dma='hout%d' % s)
                        P.emit()


        maskT = sb(gs, "maskT", [128, 128], F32)
        P.op('pool', lambda e: e.memset(maskT[:], 1.0), writes=['maskT'])
        P.op('pool', lambda e: e.affine_select(out=maskT[:], in_=maskT[:], pattern=[[1, 128]], compare_op=ALU.is_ge,
                                               fill=0.0, base=0, channel_multiplier=-1), reads=['maskT'], writes=['maskT'])
        P.emit()

        def lin_feat(uT, w2d, col0, nblk, N, epi, tag, kcn=KC, nbuf=3):
            with ExitStack() as st:
                wb = [sb(st, "lw%d" % i, [128, kcn, 128], BF16) for i in range(nbuf)]

                def load(j):
                    s_ = j % nbuf
                    src = w2d[:, col0 + j * 128: col0 + (j + 1) * 128].rearrange("(kc p) m -> p kc m", p=128)
                    P.op('pool', lambda e: e.dma_start(out=wb[s_][:], in_=src), writes=[('lw', s_)], dma='lw%d' % s_)
                for j in range(min(nbuf - 1, nblk)):
                    load(j)
                cnt = 0
                for j in range(nblk):
                    if j + nbuf - 1 < nblk:
                        load(j + nbuf - 1)
                    s_ = j % nbuf
                    for n in range(N // 512):
                        b = cnt % 4
                        cnt += 1
                        for kc in range(kcn):
                            P.op('pe', lambda e, s_=s_, b=b, kc=kc, n=n: e.matmul(
                                ps[b][:], wb[s_][:, kc, :], uT[:, kc, n * 512:(n + 1) * 512], start=(kc == 0), stop=(kc == kcn - 1)),
                                reads=[('lw', s_), (tag, kc)], writes=[PK(b)])
                        epi(j, n, b)
                P.emit()

        def lin_tok(uT, w2d, col0, nblk, N, dst_d, t0, tag):
            with ExitStack() as st:
                wb = [sb(st, "tw%d" % i, [128, KC, 512], BF16) for i in range(2)]
                vt = [sb(st, "vt%d" % i, [128, 512], BF16) for i in range(2)]
                cnt = 0
                for j in range(nblk):
                    s_ = j % 2
                    src = w2d[:, col0 + j * 512: col0 + (j + 1) * 512].rearrange("(kc p) m -> p kc m", p=128)
                    P.op('pool', lambda e, s_=s_, src=src: e.dma_start(out=wb[s_][:], in_=src), writes=[('tw', s_)], dma='tw%d' % s_)
                    for ti in range(N // 128):
                        b = 4 + cnt % 4
                        v_ = cnt % 2
                        cnt += 1
                        for kc in range(KC):
                            P.op('pe', lambda e, s_=s_, b=b, kc=kc, ti=ti: e.matmul(
                                ps[b][:], uT[:, kc, ti * 128:(ti + 1) * 128], wb[s_][:, kc, :], start=(kc == 0), stop=(kc == KC - 1)),
                                reads=[('tw', s_), (tag, kc)], writes=[PK(b)])
                        P.op('act', lambda e, v_=v_, b=b: e.copy(vt[v_][:], ps[b][:]), reads=[PK(b)], writes=[('vt', v_)])
                        P.op('sp', lambda e, v_=v_, ti=ti, j=j: e.dma_start(
                            out=dst_d[t0 + ti * 128:t0 + (ti + 1) * 128, j * 512:(j + 1) * 512], in_=vt[v_][:]),
                            reads=[('vt', v_)], dma='vt%d' % v_)
                P.emit()

        def out_proj(l, y_d, w2d, kcn):
            for seg in range(NT // SEG):
                t0 = seg * SEG
                with ExitStack() as st:
                    yT = sb(st, "yT", [128, kcn, SEG], BF16)
                    for kc in range(kcn):
                        P.op('sp', lambda e, kc=kc: e.dma_start(out=yT[:, kc, :], in_=y_d[kc * 128:(kc + 1) * 128, t0:t0 + SEG]),
                             writes=[('yT', kc)], dma='yl%d' % (kc % 4))
                    hin = [sb(st, "hin%d" % i, [128, SEG], F32) for i in range(2)]
                    hout = [sb(st, "hout%d" % i, [128, SEG], F32) for i in range(2)]

                    def epi(j, n, b):
                        s_ = j % 2
                        if n == 0:
                            P.op('sp', lambda e: e.dma_start(out=hin[s_][:], in_=hT[j * 128:(j + 1) * 128, t0:t0 + SEG]),
                                 writes=[('hin', s_)], dma='hin%d' % s_)
                        P.op('dve', lambda e: e.scalar_tensor_tensor(
                            out=hout[s_][:, n * 512:(n + 1) * 512], in0=ps[b][:], scalar=ada_col(l, 2, j),
                            in1=hin[s_][:, n * 512:(n + 1) * 512], op0=ALU.mult, op1=ALU.add),
                            reads=[PK(b), ('hin', s_), 'adac'], writes=[('hout', s_)])
                        if n == SEG // 512 - 1:
                            P.op('sp', lambda e: e.dma_start(out=hT[j * 128:(j + 1) * 128, t0:t0 + SEG], in_=hout[s_][:]),
                                 reads=[('hout', s_)], dma='hout%d' % s_)
                    lin_feat(yT, w2d, 0, KC, SEG, epi, 'yT', kcn=kcn, nbuf=2)

        def gla_chunks(H, nkc, nvc, Cc, qd_d, ki_d, ke_d, v_d, sg_d, y_d, dec_d, dec_imm, normg, slots):
            dv = nvc * 128
            nch = NT // Cc
            W = min(512, NT)
            cpw = W // Cc
            for h0 in range(0, H, slots):
                with ExitStack() as st:
                    hs = list(range(h0, min(H, h0 + slots)))
                    T = {}
                    for si, h in enumerate(hs):
                        t = {}
                        t['qd'] = sb(st, "qd", [128, nkc, NT], BF16)
                        t['ki'] = sb(st, "ki", [128, nkc, NT], BF16)
                        t['ke'] = sb(st, "ke", [128, nkc, NT], BF16)
                        t['v'] = sb(st, "v", [Cc, nch, dv], BF16)
                        t['sg'] = sb(st, "sg", [128, nvc, NT], BF16)
                        t['y'] = sb(st, "y", [128, nvc, NT], BF16)
                        t['S'] = sb(st, "S", [128, nkc, dv], F32)
                        t['Sb'] = sb(st, "Sb", [128, nkc, dv], BF16)
                        t['Pm'] = sb(st, "Pm", [Cc, Cc], BF16)
                        t['keT'] = sb(st, "keT", [Cc, nkc * 128], BF16)
                        t['ow'] = sb(st, "ow", [128, nvc, W], F32)
                        t['sq'] = sb(st, "sq", [128, nvc, W], F32)
                        t['rs'] = sb(st, "rs", [128, W], F32)
                        t['tmp'] = sb(st, "tmp", [128, W], F32)
                        if dec_d is not None:
                            t['dec'] = sb(st, "dec", [128, nch], F32)
                        T[si] = t
                        k = lambda nm, si=si: (nm, si)
                        for kc in range(nkc):
                            r0 = (h * nkc + kc) * 128
                            P.op('sp', lambda e, t=t, kc=kc, r0=r0: e.dma_start(out=t['qd'][:, kc, :], in_=qd_d[r0:r0 + 128, :]),
                                 writes=[k('qd')], dma='g0')
                            P.op('sp', lambda e, t=t, kc=kc, r0=r0: e.dma_start(out=t['ki'][:, kc, :], in_=ki_d[r0:r0 + 128, :]),
                                 writes=[k('ki')], dma='g1')
                            P.op('sp', lambda e, t=t, kc=kc, r0=r0: e.dma_start(out=t['ke'][:, kc, :], in_=ke_d[r0:r0 + 128, :]),
                                 writes=[k('ke')], dma='g2')
                        P.op('sp', lambda e, t=t, h=h: e.dma_start(
                            out=t['v'][:], in_=v_d[:, h * dv:(h + 1) * dv].rearrange("(n p) d -> p n d", p=Cc)),
                            writes=[k('v')], dma='g3')
                        for vc in range(nvc):
                            r0 = (h * nvc + vc) * 128
                            P.op('sp', lambda e, t=t, vc=vc, r0=r0: e.dma_start(out=t['sg'][:, vc, :], in_=sg_d[r0:r0 + 128, :]),
                                 writes=[k('sg')], dma='g4')
                        if dec_d is not None:
                            P.op('sp', lambda e, t=t, h=h: e.dma_start(out=t['dec'][:], in_=dec_d[h * 128:(h + 1) * 128, :]),
                                 writes=[k('dec')], dma='g5')
                        P.op('pool', lambda e, t=t: e.memset(t['S'][:], 0.0), writes=[k('S')])
                        P.op('pool', lambda e, t=t: e.memset(t['Sb'][:], 0.0), writes=[k('Sb')])
                    nb = 8 // len(hs)
                    for n in range(nch):
                        c0 = n * Cc
                        for si, h in enumerate(hs):
                            t = T[si]
                            k = lambda nm, si=si: (nm, si)
                            bA, bB, bC = si * nb, si * nb + 1, si * nb + 2
                            bD = [si * nb + 3 + kc for kc in range(nkc)]
                            for kc in range(nkc):
                                P.op('pe', lambda e, t=t, kc=kc, bA=bA, c0=c0: e.matmul(
                                    ps[bA][0:Cc, 0:Cc], t['ki'][:, kc, c0:c0 + Cc], t['qd'][:, kc, c0:c0 + Cc],
                                    start=(kc == 0), stop=(kc == nkc - 1)), reads=[k('ki'), k('qd')], writes=[PK(bA)])
                            P.op('dve', lambda e, t=t, bA=bA: e.tensor_tensor(out=t['Pm'][:], in0=ps[bA][0:Cc, 0:Cc],
                                                                              in1=maskT[0:Cc, 0:Cc], op=ALU.mult),
                                 reads=[PK(bA), 'maskT'], writes=[k('Pm')])
                            psb = ps[bB].bitcast(BF16)
                            for kc in range(nkc):
                                P.op('pe', lambda e, t=t, kc=kc, psb=psb, c0=c0: e.transpose(
                                    psb[0:Cc, kc * 128:(kc + 1) * 128], t['ke'][:, kc, c0:c0 + Cc], identb[:]),
                                    reads=[k('ke'), 'identb'], writes=[PK(bB)])
                            P.op('act', lambda e, t=t, psb=psb: e.copy(t['keT'][:], psb[0:Cc, 0:nkc * 128]),
                                 reads=[PK(bB)], writes=[k('keT')])
                            for vc in range(nvc):
                                P.op('pe', lambda e, t=t, vc=vc, bC=bC, n=n: e.matmul(
                                    ps[bC][:, vc * Cc:(vc + 1) * Cc], t['v'][:, n, vc * 128:(vc + 1) * 128], t['Pm'][:],
                                    start=True, stop=False), reads=[k('v'), k('Pm')], writes=[PK(bC)])
                                for kc in range(nkc):
                                    P.op('pe', lambda e, t=t, vc=vc, kc=kc, bC=bC, c0=c0: e.matmul(
                                        ps[bC][:, vc * Cc:(vc + 1) * Cc], t['Sb'][:, kc, vc * 128:(vc + 1) * 128],
                                        t['qd'][:, kc, c0:c0 + Cc], start=False, stop=(kc == nkc - 1)),
                                        reads=[k('Sb'), k('qd')], writes=[PK(bC)])
                            wo_ = (n % cpw) * Cc
                            P.op('act', lambda e, t=t, bC=bC, wo_=wo_: e.copy(
                                t['ow'][:, :, wo_:wo_ + Cc], ps[bC][:, 0:nvc * Cc].rearrange("p (a b) -> p a b", a=nvc)),
                                reads=[PK(bC)], writes=[k('ow')])
                            for kc in range(nkc):
                                P.op('pe', lambda e, t=t, kc=kc, n=n, b=bD[kc]: e.matmul(
                                    ps[b][:, 0:dv], t['keT'][:, kc * 128:(kc + 1) * 128], t['v'][:, n, :], start=True, stop=True),
                                    reads=[k('keT'), k('v')], writes=[PK(bD[kc])])
                                dsc = t['dec'][:, n:n + 1] if dec_d is not None else float(dec_imm[h])
                                P.op('dve', lambda e, t=t, kc=kc, b=bD[kc], dsc=dsc: e.scalar_tensor_tensor(
                                    out=t['S'][:, kc, :], in0=t['S'][:, kc, :], scalar=dsc, in1=ps[b][:, 0:dv],
                                    op0=ALU.mult, op1=ALU.add), reads=[PK(bD[kc]), k('S'), k('dec')], writes=[k('S')])
                            P.op('act', lambda e, t=t: e.copy(t['Sb'][:], t['S'][:]), reads=[k('S')], writes=[k('Sb')])
                            if (n + 1) % cpw == 0:
                                w0 = (n + 1) * Cc - W
                                P.op('act', lambda e, t=t: e.activation(out=t['sq'][:], in_=t['ow'][:], func=AF.Square),
                                     reads=[k('ow')], writes=[k('sq')])
                                for vc in range(nvc):
                                    P.op('pe', lambda e, t=t, vc=vc, bA=bA: e.matmul(
                                        ps[bA][:, 0:W], ones32[:], t['sq'][:, vc, :], start=(vc == 0), stop=(vc == nvc - 1)),
                                        reads=[k('sq'), 'ones32'], writes=[PK(bA)])
                                P.op('act', lambda e, t=t, bA=bA: e.activation(out=t['rs'][:], in_=ps[bA][:, 0:W], func=AF.Ln,
                                                                               scale=1.0 / dv, bias=epsc[:]),
                                     reads=[PK(bA), 'epsc'], writes=[k('rs')])
                                P.op('act', lambda e, t=t: e.activation(out=t['rs'][:], in_=t['rs'][:], func=AF.Exp, scale=-0.5),
                                     reads=[k('rs')], writes=[k('rs')])
                                for vc in range(nvc):
                                    gcol = normg[:, h * nvc + vc:h * nvc + vc + 1]
                                    P.op('dve', lambda e, t=t, vc=vc, gcol=gcol: e.scalar_tensor_tensor(
                                        out=t['tmp'][:], in0=t['ow'][:, vc, :], scalar=gcol, in1=t['rs'][:],
                                        op0=ALU.mult, op1=ALU.mult), reads=[k('ow'), k('rs')], writes=[k('tmp')])
                                    P.op('dve', lambda e, t=t, vc=vc, w0=w0: e.tensor_tensor(
                                        out=t['y'][:, vc, w0:w0 + W], in0=t['tmp'][:], in1=t['sg'][:, vc, w0:w0 + W], op=ALU.mult),
                                        reads=[k('tmp'), k('sg')], writes=[k('y')])
                    for si, h in enumerate(hs):
                        t = T[si]
                        for vc in range(nvc):
                            r0 = (h * nvc + vc) * 128
                            P.op('sp', lambda e, t=t, vc=vc, r0=r0: e.dma_start(out=y_d[r0:r0 + 128, :], in_=t['y'][:, vc, :]),
                                 reads=[('y', si)], dma='g6')
                    P.emit()

        def hgrn_layer(l):
            dram = lambda n, sh, dt: nc.dram_tensor(n, sh, dt, kind=("ExternalOutput" if DEBUG else "Internal")).ap()
            qd_d = dram("h_qd", [D, NT], BF16)
            ki_d = dram("h_ki", [D, NT], BF16)
            ke_d = dram("h_ke", [D, NT], BF16)
            v_d = dram("h_v", [NT, D], BF16)
            sg_d = dram("h_sg", [D, NT], BF16)
            y_d = dram("h_y", [D, NT], BF16)
            dec_d = dram("h_dec", [D, NT // 32], F32)
            with ExitStack() as st0:
                lb = sb(st0, "lb", [128, 16], F32)
                oml = sb(st0, "oml", [128, 16], F32)
                noml = sb(st0, "noml", [128, 16], F32)
                ex = sb(st0, "ex", [128, 64], F32)
                m32 = sb(st0, "m32", [128, 512], F32)
                P.op('act', lambda e: e.activation(out=ex[:], in_=lbl[:], func=AF.Exp), reads=['lbl'], writes=['ex'])
                P.op('dve', lambda e: e.tensor_tensor(out=lb[:], in0=ex[:, 0:16], in1=ex[:, 16:32], op=ALU.add), reads=['ex'], writes=['lb'])
                P.op('dve', lambda e: e.tensor_tensor(out=lb[:], in0=lb[:], in1=ex[:, 32:48], op=ALU.add), reads=['ex', 'lb'], writes=['lb'])
                P.op('dve', lambda e: e.tensor_tensor(out=lb[:], in0=lb[:], in1=ex[:, 48:64], op=ALU.add), reads=['ex', 'lb'], writes=['lb'])
                P.op('dve', lambda e: e.reciprocal(lb[:], lb[:]), reads=['lb'], writes=['lb'])
                P.op('dve', lambda e: e.tensor_tensor(out=oml[:], in0=ex[:, 16:32], in1=lb[:], op=ALU.mult), reads=['ex', 'lb'], writes=['oml'])
                for i in range(2, l + 1):
                    P.op('dve', lambda e, i=i: e.scalar_tensor_tensor(out=oml[:], in0=ex[:, 16 * i:16 * i + 16], scalar=1.0, in1=lb[:],
                                                                      op0=ALU.mult, op1=ALU.mult), reads=['ex', 'lb'], writes=['noml'])
                    P.op('dve', lambda e: e.tensor_tensor(out=oml[:], in0=oml[:], in1=noml[:], op=ALU.add), reads=['noml', 'oml'], writes=['oml'])
                P.op('dve', lambda e: e.tensor_copy(lb[:], oml[:]), reads=['oml'], writes=['lb'])
                P.op('dve', lambda e: e.tensor_scalar(oml[:], lb[:], -1.0, 1.0, op0=ALU.mult, op1=ALU.add), reads=['lb'], writes=['oml'])
                P.op('dve', lambda e: e.tensor_scalar(noml[:], oml[:], -1.0, None, op0=ALU.mult), reads=['oml'], writes=['noml'])
                P.op('pool', lambda e: e.memset(m32[:], 1.0), writes=['m32'])
                P.op('pool', lambda e: e.memset(m32[:].rearrange("p (a b) -> p a b", b=32)[:, :, 0:1], 0.0), writes=['m32'])
                P.emit()
                for seg in range(NT // SEG):
                    t0 = seg * SEG
                    with ExitStack() as st:
                        uT = sb(st, "uT", [128, KC, SEG], BF16)
                        normmod(st, t0, SEG, lambda kc: modA[:, (l * 2) * 16 + kc:(l * 2) * 16 + kc + 1],
                                lambda kc: ada_col(l, 0, kc), uT, 'uT')
                        with ExitStack() as st2:
                            F = lambda nm: [sb(st2, nm + "%d" % i, [128, 512], F32) for i in range(2)]
                            Bf = lambda nm: [sb(st2, nm + "%d" % i, [128, 512], BF16) for i in range(2)]
                            sgm, lf, kk, cum, ec, ei, ee = F("sgm"), F("lf"), F("kk"), F("cum"), F("ec"), F("ei"), F("ee")
                            qd, ki, ke = Bf("qd"), Bf("ki"), Bf("ke")
                            dcs = [sb(st2, "dcs%d" % i, [128, 16], F32) for i in range(2)]
                            wq = [sb(st2, "wq%d" % i, [128, KC, 128], BF16) for i in range(2)]
                            wf = [sb(st2, "wf%d" % i, [128, KC, 128], BF16) for i in range(2)]
                            cnt = 0
                            for h in range(16):
                                s_ = h % 2
                                for wt, c0, nm in ((wq, h * 128, 'wq'), (wf, 2048 + h * 128, 'wf')):
                                    src = hgrn_w_in[:, c0:c0 + 128].rearrange("(kc p) m -> p kc m", p=128)
                                    P.op('pool', lambda e, wt=wt, src=src, s_=s_: e.dma_start(out=wt[s_][:], in_=src),
                                         writes=[(nm, s_)], dma=nm + '%d' % s_)
                                for n in range(SEG // 512):
                                    bq, bf_ = (cnt * 2) % 8, (cnt * 2) % 8 + 1
                                    z = cnt % 2
                                    cnt += 1
                                    K_ = lambda nm, z=z: (nm, z)
                                    for kc in range(KC):
                                        P.op('pe', lambda e, s_=s_, kc=kc, n=n, bq=bq: e.matmul(
                                            ps[bq][:], wq[s_][:, kc, :], uT[:, kc, n * 512:(n + 1) * 512], start=(kc == 0), stop=(kc == KC - 1)),
                                            reads=[('wq', s_), ('uT', kc)], writes=[PK(bq)])
                                    for kc in range(KC):
                                        P.op('pe', lambda e, s_=s_, kc=kc, n=n, bf_=bf_: e.matmul(
                                            ps[bf_][:], wf[s_][:, kc, :], uT[:, kc, n * 512:(n + 1) * 512], start=(kc == 0), stop=(kc == KC - 1)),
                                            reads=[('wf', s_), ('uT', kc)], writes=[PK(bf_)])
                                    P.op('act', lambda e, z=z, bf_=bf_: e.activation(out=sgm[z][:], in_=ps[bf_][:], func=AF.Exp, scale=-1.0),
                                         reads=[PK(bf_)], writes=[K_('sgm')])
                                    P.op('dve', lambda e, z=z: e.tensor_scalar(sgm[z][:], sgm[z][:], 1.0, None, op0=ALU.add),
                                         reads=[K_('sgm')], writes=[K_('sgm')])
                                    P.op('dve', lambda e, z=z: e.reciprocal(sgm[z][:], sgm[z][:]), reads=[K_('sgm')], writes=[K_('sgm')])
                                    P.op('act', lambda e, z=z, h=h: e.activation(out=lf[z][:], in_=sgm[z][:], func=AF.Ln,
                                                                                 scale=oml[:, h:h + 1], bias=lb[:, h:h + 1]),
                                         reads=[K_('sgm'), 'oml', 'lb'], writes=[K_('lf')])
                                    P.op('dve', lambda e, z=z, h=h: e.tensor_scalar(kk[z][:], sgm[z][:], noml[:, h:h + 1], oml[:, h:h + 1],
                                                                                   op0=ALU.mult, op1=ALU.add),
                                         reads=[K_('sgm'), 'oml', 'noml'], writes=[K_('kk')])
                                    P.op('dve', lambda e, z=z: e.tensor_tensor_scan(cum[z][:], m32[:], lf[z][:], 0.0, op0=ALU.mult, op1=ALU.add),
                                         reads=[K_('lf'), 'm32'], writes=[K_('cum')])
                                    P.op('act', lambda e, z=z: e.activation(out=ec[z][:], in_=cum[z][:], func=AF.Exp),
                                         reads=[K_('cum')], writes=[K_('ec')])
                                    P.op('act', lambda e, z=z: e.activation(out=ei[z][:], in_=cum[z][:], func=AF.Exp, scale=-1.0),
                                         reads=[K_('cum')], writes=[K_('ei')])
                                    c3 = lambda a: a[:].rearrange("p (a b) -> p a b", b=32)
                                    P.op('dve', lambda e, z=z: e.tensor_tensor(out=c3(ee[z]), in0=c3(cum[z])[:, :, 31:32].broadcast_to([128, 16, 32]),
                                                                               in1=c3(cum[z]), op=ALU.subtract),
                                         reads=[K_('cum')], writes=[K_('ee')])
                                    P.op('act', lambda e, z=z: e.activation(out=ee[z][:], in_=ee[z][:], func=AF.Exp),
                                         reads=[K_('ee')], writes=[K_('ee')])
                                    P.op('dve', lambda e, z=z, bq=bq: e.tensor_tensor(out=qd[z][:], in0=ps[bq][:], in1=ec[z][:], op=ALU.mult),
                                         reads=[PK(bq), K_('ec')], writes=[K_('qd')])
                                    P.op('dve', lambda e, z=z: e.tensor_tensor(out=ki[z][:], in0=kk[z][:], in1=ei[z][:], op=ALU.mult),
                                         reads=[K_('kk'), K_('ei')], writes=[K_('ki')])
                                    P.op('dve', lambda e, z=z: e.tensor_tensor(out=ke[z][:], in0=kk[z][:], in1=ee[z][:], op=ALU.mult),
                                         reads=[K_('kk'), K_('ee')], writes=[K_('ke')])
                                    P.op('act', lambda e, z=z: e.copy(dcs[z][:], c3(ec[z])[:, :, 31]), reads=[K_('ec')], writes=[K_('dcs')])
                                    tt = t0 + n * 512
                                    for buf, dd, nm in ((qd, qd_d, 'qd'), (ki, ki_d, 'ki'), (ke, ke_d, 'ke')):
                                        P.op('sp', lambda e, z=z, buf=buf, dd=dd, h=h, tt=tt: e.dma_start(
                                            out=dd[h * 128:(h + 1) * 128, tt:tt + 512], in_=buf[z][:]),
                                            reads=[K_(nm)], dma='o' + nm + '%d' % z)
                                    P.op('sp', lambda e, z=z, h=h, tt=tt: e.dma_start(
                                        out=dec_d[h * 128:(h + 1) * 128, tt // 32:tt // 32 + 16], in_=dcs[z][:]),
                                        reads=[K_('dcs')], dma='odc%d' % z)
                            P.emit()
                        lin_tok(uT, hgrn_w_in, 4096, 4, SEG, v_d, t0, 'uT')
                        with ExitStack() as st2:
                            sgo = [sb(st2, "sgo%d" % i, [128, 512], BF16) for i in range(2)]

                            def epi(j, n, b):
                                z = (j * 2 + n) % 2
                                P.op('act', lambda e: e.activation(out=sgo[z][:], in_=ps[b][:], func=AF.Silu),
                                     reads=[PK(b)], writes=[('sgo', z)])
                                P.op('sp', lambda e: e.dma_start(out=sg_d[j * 128:(j + 1) * 128, t0 + n * 512:t0 + (n + 1) * 512], in_=sgo[z][:]),
                                     reads=[('sgo', z)], dma='osg%d' % z)
                            lin_feat(uT, hgrn_w_in, 6144, 16, SEG, epi, 'uT')
                gla_chunks(16, 1, 1, 32, qd_d, ki_d, ke_d, v_d, sg_d, y_d, dec_d, None, hng, slots=2)
                out_proj(l, y_d, hgrn_w_out, 16)


        def ret_layer(l):
            dram = lambda n, sh, dt: nc.dram_tensor(n, sh, dt, kind=("ExternalOutput" if DEBUG else "Internal")).ap()
            qd_d = dram("r_qd", [D, NT], BF16)
            ki_d = dram("r_ki", [D, NT], BF16)
            ke_d = dram("r_ke", [D, NT], BF16)
            v_d = dram("r_v", [NT, 4096], BF16)
            sg_d = dram("r_sg", [4096, NT], BF16)
            y_d = dram("r_y", [4096, NT], BF16)
            TWO_PI = 2.0 * math.pi
            PI_ = 3.1415925
            with ExitStack() as st0:
                invf = sb(st0, "invf", [128, 1], F32)
                gtab = sb(st0, "gtab", [128, 8, 3, 128], F32)
                P.op('sp', lambda e: e.dma_start(out=invf[:], in_=invf_d), writes=['invf'], dma='c0')
                P.op('sp', lambda e: e.dma_start(out=gtab[:].rearrange("p a b c -> p (a b c)"), in_=gtab_d.broadcast_to([128, 8 * 3 * 128])),
                     writes=['gtab'], dma='c1')
                P.emit()
                for seg in range(NT // SEG):
                    t0 = seg * SEG
                    with ExitStack() as st:
                        uT = sb(st, "uT", [128, KC, SEG], BF16)
                        normmod(st, t0, SEG, lambda kc: modA[:, (l * 2) * 16 + kc:(l * 2) * 16 + kc + 1],
                                lambda kc: ada_col(l, 0, kc), uT, 'uT')
                        with ExitStack() as st2:
                            cs = sb(st2, "cs", [128, SEG], F32)
                            sn = sb(st2, "sn", [128, SEG], F32)
                            posi = sb(st2, "posi", [128, SEG], I32)
                            ang = sb(st2, "ang", [128, SEG], F32)
                            w1 = sb(st2, "w1", [128, SEG], F32)
                            wi = sb(st2, "wi", [128, SEG], I32)
                            P.op('sp', lambda e: e.dma_start(out=posi[:], in_=pos[0:1, t0:t0 + SEG].broadcast_to([128, SEG])),
                                 writes=['posi'], dma='c0')
                            P.op('dve', lambda e: e.tensor_copy(ang[:], posi[:]), reads=['posi'], writes=['ang'])
                            P.op('dve', lambda e: e.tensor_scalar(ang[:], ang[:], invf[:, 0:1], None, op0=ALU.mult),
                                 reads=['ang', 'invf'], writes=['ang'])
                            for off, dst, dk_ in ((0.0, sn, 'sn'), (0.5 * math.pi, cs, 'cs')):
                                P.op('dve', lambda e, off=off: e.tensor_scalar(w1[:], ang[:], off, 1.0 / TWO_PI, op0=ALU.add, op1=ALU.mult),
                                     reads=['ang'], writes=['w1'])
                                P.op('dve', lambda e: e.tensor_copy(wi[:], w1[:]), reads=['w1'], writes=['wi'])
                                P.op('dve', lambda e: e.tensor_copy(w1[:], wi[:]), reads=['wi'], writes=['w1'])
                                P.op('dve', lambda e: e.tensor_scalar(w1[:], w1[:], -TWO_PI, None, op0=ALU.mult), reads=['w1'], writes=['w1'])
                                P.op('dve', lambda e, off=off: e.scalar_tensor_tensor(out=w1[:], in0=ang[:], scalar=off, in1=w1[:],
                                                                                      op0=ALU.add, op1=ALU.add),
                                     reads=['w1', 'ang'], writes=['w1'])
                                P.op('dve', lambda e: e.tensor_scalar(w1[:], w1[:], -PI_, PI_, op0=ALU.max, op1=ALU.min),
                                     reads=['w1'], writes=['w1'])
                                P.op('act', lambda e, dst=dst: e.activation(out=dst[:], in_=w1[:], func=AF.Sin), reads=['w1'], writes=[dk_])
                            F = lambda nm, n_: [sb(st2, nm + "%d" % i, [128, 512], F32) for i in range(n_)]
                            Bf = lambda nm, n_: [sb(st2, nm + "%d" % i, [128, 512], BF16) for i in range(n_)]
                            ta, tb, tc_, td = F("ta", 2), F("tb", 2), F("tc", 2), F("td", 2)
                            r1, r2 = F("r1", 2), F("r2", 2)
                            ob = Bf("ob", 8)
                            wr = [sb(st2, "wr%d" % i, [128, KC, 128], BF16) for i in range(8)]
                            cnt = 0
                            oc = 0
                            for h in range(8):
                                hs_ = (h % 2) * 4
                                cols = [h * 256, h * 256 + 128, 2048 + h * 256, 2048 + h * 256 + 128]
                                for i, c0 in enumerate(cols):
                                    src = ret_w_in[:, c0:c0 + 128].rearrange("(kc p) m -> p kc m", p=128)
                                    P.op('pool', lambda e, src=src, i=i, hs_=hs_: e.dma_start(out=wr[hs_ + i][:], in_=src),
                                         writes=[('wr', hs_ + i)], dma='wr%d' % (hs_ + i))
                                for n in range(SEG // 512):
                                    csn = cs[:, n * 512:(n + 1) * 512]
                                    snn = sn[:, n * 512:(n + 1) * 512]
                                    for qk in range(2):
                                        b1, b2 = (cnt * 2) % 8, (cnt * 2) % 8 + 1
                                        z = cnt % 2
                                        cnt += 1
                                        for bb, wi_ in ((b1, hs_ + qk * 2), (b2, hs_ + qk * 2 + 1)):
                                            for kc in range(KC):
                                                P.op('pe', lambda e, bb=bb, wi_=wi_, kc=kc, n=n: e.matmul(
                                                    ps[bb][:], wr[wi_][:, kc, :], uT[:, kc, n * 512:(n + 1) * 512],
                                                    start=(kc == 0), stop=(kc == KC - 1)),
                                                    reads=[('wr', wi_), ('uT', kc)], writes=[PK(bb)])
                                        for dst, bb, tr, nm in ((ta, b1, csn, 'ta'), (tb, b2, snn, 'tb'), (tc_, b1, snn, 'tc'), (td, b2, csn, 'td')):
                                            P.op('dve', lambda e, dst=dst, bb=bb, tr=tr, z=z: e.tensor_tensor(out=dst[z][:], in0=ps[bb][:], in1=tr, op=ALU.mult),
                                                 reads=[PK(bb), 'cs', 'sn'], writes=[(nm, z)])
                                        P.op('pool', lambda e, z=z: e.tensor_tensor(out=r1[z][:], in0=ta[z][:], in1=tb[z][:], op=ALU.subtract),
                                             reads=[('ta', z), ('tb', z)], writes=[('r1', z)])
                                        P.op('pool', lambda e, z=z: e.tensor_tensor(out=r2[z][:], in0=tc_[z][:], in1=td[z][:], op=ALU.add),
                                             reads=[('tc', z), ('td', z)], writes=[('r2', z)])
                                        tt = t0 + n * 512
                                        outs = [(0, qd_d)] if qk == 0 else [(1, ki_d), (2, ke_d)]
                                        for gi, dd in outs:
                                            for half, rr, rn in ((0, r1, 'r1'), (1, r2, 'r2')):
                                                o_ = oc % 8
                                                oc += 1
                                                gv = gtab[:, h, gi, :]
                                                P.op('pool', lambda e, o_=o_, rr=rr, z=z, gv=gv: e.tensor_tensor(
                                                    out=ob[o_][:].rearrange("p (a b) -> p a b", b=128),
                                                    in0=rr[z][:].rearrange("p (a b) -> p a b", b=128),
                                                    in1=gv.unsqueeze(1).broadcast_to([128, 4, 128]), op=ALU.mult),
                                                    reads=[(rn, z), 'gtab'], writes=[('ob', o_)])
                                                r0 = h * 256 + half * 128
                                                P.op('sp', lambda e, o_=o_, dd=dd, r0=r0, tt=tt: e.dma_start(
                                                    out=dd[r0:r0 + 128, tt:tt + 512], in_=ob[o_][:]), reads=[('ob', o_)], dma='ob%d' % o_)
                            P.emit()
                        lin_tok(uT, ret_w_in, 4096, 8, SEG, v_d, t0, 'uT')
                        with ExitStack() as st2:
                            sgo = [sb(st2, "sgo%d" % i, [128, 512], BF16) for i in range(2)]

                            def epi(j, n, b):
                                z = (j * 2 + n) % 2
                                P.op('act', lambda e: e.activation(out=sgo[z][:], in_=ps[b][:], func=AF.Silu),
                                     reads=[PK(b)], writes=[('sgo', z)])
                                P.op('sp', lambda e: e.dma_start(out=sg_d[j * 128:(j + 1) * 128, t0 + n * 512:t0 + (n + 1) * 512], in_=sgo[z][:]),
                                     reads=[('sgo', z)], dma='osg%d' % z)
                            lin_feat(uT, ret_w_in, 8192, 32, SEG, epi, 'uT')
            gam = [1.0 - 2.0 ** (-5.0 - h) for h in range(8)]
            gla_chunks(8, 2, 4, 128, qd_d, ki_d, ke_d, v_d, sg_d, y_d, None, [g_ ** 128 for g_ in gam], rng_, slots=1)
            out_proj(l, y_d, ret_w_out, 32)

        C.P, C.sb, C.ps, C.PK, C.hT, C.hT3 = P, sb, ps, PK, hT, hT3
        C.normmod, C.ada_col, C.modA = normmod, ada_col, modA

        for l in layers:
            if do_mixer:
                if l % 3 == 0:
                    pool_layer(l)
                elif l % 3 == 1:
                    hgrn_layer(l)
                else:
                    ret_layer(l)
            if do_ffn:
                ffn_layer(l)

        with ExitStack() as st:
            FN = min(512, NT)
            for seg in range(NT // FN):
                t0 = seg * FN
                with ExitStack() as st2:
                    yT = sb(st2, "yT", [128, KC, FN], F32)
                    z0 = sb(st2, "z0", [128, 1], F32)
                    P.op('pool', lambda e: e.memset(z0[:], 0.0), writes=['z0'])
                    normmod(st2, t0, FN, lambda kc: fng[:, kc:kc + 1], lambda kc: z0[:], yT, 'yT')
                    ot = [sb(st2, "ot%d" % i, [128, D], F32) for i in range(2)]
                    for ti in range(FN // 128):
                        s = ti % 2
                        for q in range(4):
                            b = (ti * 4 + q) % 8
                            for i in range(4):
                                c = q * 4 + i
                                P.op('pe', lambda e, b=b, i=i, c=c, ti=ti: e.transpose(
                                    ps[b][:, i * 128:(i + 1) * 128], yT[:, c, ti * 128:(ti + 1) * 128], ident[:]),
                                    reads=[('yT', c), 'ident'], writes=[PK(b)])
                            if q % 2 == 0:
                                P.op('dve', lambda e, s=s, b=b, q=q: e.tensor_copy(ot[s][:, q * 512:(q + 1) * 512], ps[b][:]),
                                     reads=[PK(b)], writes=[('ot', s)])
                            else:
                                P.op('act', lambda e, s=s, b=b, q=q: e.copy(ot[s][:, q * 512:(q + 1) * 512], ps[b][:]),
                                     reads=[PK(b)], writes=[('ot', s)])
                        P.op('sp', lambda e, s=s, ti=ti: e.dma_start(out=out[t0 + ti * 128:t0 + (ti + 1) * 128, :], in_=ot[s][:]),
                             reads=[('ot', s)], dma='ot%d' % s)
                    P.emit()
    return nc


_IDENT = np.eye(128, dtype=np.float32)
_INVF = (np.float32(10000.0) ** (-np.arange(128, dtype=np.float32) / np.float32(128))).astype(np.float32).reshape(128, 1)
_lg = np.log(1.0 - 2.0 ** (-5.0 - np.arange(8, dtype=np.float64)))
_t = np.arange(128, dtype=np.float64)
_GTAB = np.stack([np.exp(_lg[:, None] * (_t + 1.0)), np.exp(-_lg[:, None] * (_t + 1.0)) / 16.0,
                  np.exp(_lg[:, None] * (127.0 - _t)) / 16.0], axis=1).astype(np.float32).reshape(1, 8 * 3 * 128)


def make_in_map(inputs, b, t0, NT):
    g = lambda k: np.ascontiguousarray(np.asarray(inputs[k]))
    m = {
        "x": np.ascontiguousarray(g("x")[b, t0:t0 + NT]),
        "c": g("c")[b].reshape(KC, 128),
        "positions": np.ascontiguousarray(g("positions")[b:b + 1, t0:t0 + NT]).astype(np.int32),
        "w_ada": g("w_ada"), "b_ada": g("b_ada"),
        "norm_mix_g": g("norm_mix_g").reshape(DEPTH * KC, 128),
        "norm_ffn_g": g("norm_ffn_g").reshape(DEPTH * KC, 128),
        "pool_w": g("pool_w"), "pool_scale": g("pool_scale").reshape(2 * KC, 128),
        "hgrn_w_in": g("hgrn_w_in")[0], "hgrn_lb_logits": g("hgrn_lb_logits").reshape(DEPTH * KC, 128),
        "hgrn_norm_g": g("hgrn_norm_g").reshape(KC, 128), "hgrn_w_out": g("hgrn_w_out")[0],
        "ret_w_in": g("ret_w_in")[0], "ret_norm_g": g("ret_norm_g").reshape(32, 128), "ret_w_out": g("ret_w_out")[0],
        "ffn_w_in": g("ffn_w_in"), "ffn_w_out": g("ffn_w_out"),
        "final_norm_g": g("final_norm_g").reshape(KC, 128),
        "ident": _IDENT, "invf": _INVF, "gtab": _GTAB,
    }
    return m


def kernel(**inputs):
    B, S, _ = inputs["x"].shape
    nc = build_program(S)
    in_maps = [make_in_map(inputs, c % B, 0, S) for c in range(8)]
    res = run_bass_kernel_spmd(nc, in_maps, core_ids=list(range(8)))
    return np.stack([np.asarray(res.results[b]["out"]) for b in range(B)], axis=0).astype(np.float32)
```

```python
from contextlib import ExitStack
import math
import numpy as np
import concourse.bass as bass
import concourse.mybir as mybir
from concourse.bass_utils import run_bass_kernel_spmd

F32 = mybir.dt.float32
BF16 = mybir.dt.bfloat16
I32 = mybir.dt.int32
AF = mybir.ActivationFunctionType
ALU = mybir.AluOpType

D = 2048
KC = 16
DEPTH = 4
FH = 5632
FHC = 44
EPS = 1e-6
SEG = 1024
ENGS = ['pe', 'act', 'dve', 'pool', 'sp']
DEBUG = False


class Op:
    __slots__ = ('eng', 'fn', 'deps', 'dma', 'flag', 'event', 'inc')

    def __init__(self, eng, fn, deps, dma, inc=16):
        self.eng, self.fn, self.deps, self.dma, self.inc = eng, fn, deps, dma, inc
        self.flag = False
        self.event = None


class Prog:
    def __init__(self, nc, stack):
        self.nc = nc
        self.stack = stack
        self.eobj = {'pe': nc.tensor, 'act': nc.scalar, 'dve': nc.vector, 'pool': nc.gpsimd, 'sp': nc.sync}
        self.esem = {e: stack.enter_context(nc.semaphore('es_' + e)) for e in ENGS}
        self.ecnt = {e: 0 for e in ENGS}
        self.dsem = {}
        self.dcnt = {}
        self.known = {e: {} for e in ENGS}
        self.nphase = 0
        self.reset()

    def reset(self):
        self.ops = []
        self.lastw = {}
        self.rd = {}

    def op(self, eng, fn, reads=(), writes=(), dma=None, inc=16):
        idx = len(self.ops)
        deps = set()
        for r in reads:
            w = self.lastw.get(r)
            if w is not None:
                deps.add(w)
        for wk in writes:
            w = self.lastw.get(wk)
            if w is not None:
                deps.add(w)
            rr = self.rd.get(wk)
            if rr:
                deps.update(rr.values())
        for r in reads:
            d = self.rd.setdefault(r, {})
            d[(eng, dma)] = idx
        for wk in writes:
            self.lastw[wk] = idx
            self.rd[wk] = {}
        deps.discard(idx)
        if eng == 'pe':
            deps = {d for d in deps if not (self.ops[d].eng == 'pe' and self.ops[d].dma is None)}
        self.ops.append(Op(eng, fn, deps, dma, inc))
        return idx

    def _dsem(self, key):
        if key not in self.dsem:
            self.dsem[key] = self.stack.enter_context(self.nc.semaphore('ds_' + key))
            self.dcnt[key] = 0
        return self.dsem[key]

    def emit(self):
        ops = self.ops
        if not ops:
            return
        for o in ops:
            for d in o.deps:
                ops[d].flag = True
        for o in ops:
            if o.dma is not None:
                s = self._dsem(o.dma)
                self.dcnt[o.dma] += o.inc
                o.event = (o.dma, s, self.dcnt[o.dma], o.inc)
            elif o.flag:
                self.ecnt[o.eng] += 1
                o.event = ('e_' + o.eng, self.esem[o.eng], self.ecnt[o.eng], 1)
        per = {e: [] for e in ENGS}
        for o in ops:
            per[o.eng].append(o)

        def run(e, eng):
            kn = self.known[e]
            final = {}
            for o in per[e]:
                for d in sorted(o.deps):
                    name, s, val, _ = ops[d].event
                    if kn.get(name, 0) < val:
                        eng.wait_ge(s, val)
                        kn[name] = val
                ins = o.fn(eng)
                if o.event is not None:
                    name, s, val, inc = o.event
                    ins.then_inc(s, inc)
                    if o.dma is not None:
                        final[name] = (s, val)
            for name, (s, val) in final.items():
                if kn.get(name, 0) < val:
                    eng.wait_ge(s, val)
                    kn[name] = val

        with self.nc.Block() as block:
            if per['pe']:
                block.tensor(lambda eng: run('pe', eng))
            if per['act']:
                block.scalar(lambda eng: run('act', eng))
            if per['dve']:
                block.vector(lambda eng: run('dve', eng))
            if per['pool']:
                block.gpsimd(lambda eng: run('pool', eng))
            if per['sp']:
                block.sync(lambda eng: run('sp', eng))
        self.nphase += 1
        self.reset()


class Ctx:
    pass


def build_program(NT, layers=(0, 1, 2, 3), do_mixer=True, do_ffn=True, ncores=8):
    nc = bass.Bass("TRN2", target_bir_lowering=False)
    C = Ctx()
    C.nc = nc
    C.NT = NT
    dt_in = lambda n, s, d=F32: nc.dram_tensor(n, s, d, kind="ExternalInput").ap()
    x = dt_in("x", [NT, D])
    cvec = dt_in("c", [KC, 128])
    pos = dt_in("positions", [1, NT], I32)
    w_ada = dt_in("w_ada", [DEPTH, D, 6 * D])
    b_ada = dt_in("b_ada", [DEPTH, 6 * D])
    norm_mix_g = dt_in("norm_mix_g", [DEPTH * KC, 128])
    norm_ffn_g = dt_in("norm_ffn_g", [DEPTH * KC, 128])
    pool_w = dt_in("pool_w", [2, 4, 512, 512])
    pool_scale = dt_in("pool_scale", [2 * KC, 128])
    hgrn_w_in = dt_in("hgrn_w_in", [D, 8192])
    hgrn_lb = dt_in("hgrn_lb_logits", [DEPTH * KC, 128])
    hgrn_norm_g = dt_in("hgrn_norm_g", [KC, 128])
    hgrn_w_out = dt_in("hgrn_w_out", [D, D])
    ret_w_in = dt_in("ret_w_in", [D, 12288])
    ret_norm_g = dt_in("ret_norm_g", [32, 128])
    ret_w_out = dt_in("ret_w_out", [4096, D])
    ffn_w_in = dt_in("ffn_w_in", [DEPTH, D, 2 * FH])
    ffn_w_out = dt_in("ffn_w_out", [DEPTH, FH, D])
    final_g = dt_in("final_norm_g", [KC, 128])
    ident_d = dt_in("ident", [128, 128])
    invf_d = dt_in("invf", [128, 1])
    gtab_d = dt_in("gtab", [1, 8 * 3 * 128])
    gq2_d = dt_in("gq2", [8, NT])
    flag_d = dt_in("flag", [128, 1])
    xh_d = dt_in("xh", [16, D])
    hh = nc.dram_tensor("hh", [D, 16], F32, kind="Internal").ap()
    hh3 = hh.rearrange("(c p) t -> p c t", p=128)
    pairs = [[2 * i, 2 * i + 1] for i in range(ncores // 2)]
    out = nc.dram_tensor("out", [NT, D], F32, kind="ExternalOutput").ap()
    hT = nc.dram_tensor("hT", [D, NT], F32, kind="Internal").ap()
    ada_d = nc.dram_tensor("ada_d", [DEPTH * 96, 128], F32, kind="Internal").ap()
    hT3 = hT.rearrange("(c p) t -> p c t", p=128)

    with ExitStack() as gs:
        P = Prog(nc, gs)
        _cnt = [0]

        def sb(st, name, shape, dt):
            _cnt[0] += 1
            return st.enter_context(nc.sbuf_tensor("%s_%d" % (name, _cnt[0]), shape, dt))
        ps = [gs.enter_context(nc.psum_tensor("ps%d" % i, [128, 512], F32)) for i in range(8)]
        PK = lambda b: ('ps', b)
        ident = sb(gs, "ident", [128, 128], F32)
        identb = sb(gs, "identb", [128, 128], BF16)
        ones32 = sb(gs, "ones32", [128, 128], F32)
        epsc = sb(gs, "epsc", [128, 1], F32)
        flag = sb(gs, "flag", [128, 1], F32)
        gmix = sb(gs, "gmix", [128, 64], F32)
        gffn = sb(gs, "gffn", [128, 64], F32)
        pscl = sb(gs, "pscl", [128, 32], F32)
        lbl = sb(gs, "lbl", [128, 64], F32)
        hng = sb(gs, "hng", [128, 16], F32)
        rng_ = sb(gs, "rng", [128, 32], F32)
        fng = sb(gs, "fng", [128, 16], F32)
        adac = sb(gs, "adac", [128, DEPTH * 96], F32)
        modA = sb(gs, "modA", [128, DEPTH * 2 * 16], F32)

        with ExitStack() as st:
            rows = sb(st, "rows", [128, 128], F32)
            P.op('sp', lambda e: e.dma_start(out=ident[:], in_=ident_d), writes=['ident'], dma='c0')
            P.op('pool', lambda e: e.memset(ones32[:], 1.0), writes=['ones32'])
            P.op('pool', lambda e: e.memset(epsc[:], EPS), writes=['epsc'])
            P.op('sp', lambda e: e.dma_start(out=flag[:], in_=flag_d), writes=['flag'], dma='c4')
            P.op('dve', lambda e: e.tensor_copy(identb[:], ident[:]), reads=['ident'], writes=['identb'])
            cc = sb(st, "cc", [128, 16], F32)
            scb = sb(st, "scb", [128, 16], BF16)

            def to_cols(src_rows_ap, R, dst_ap, key):
                P.op('sp', lambda e: e.dma_start(out=rows[0:R, :], in_=src_rows_ap), writes=['rows'], dma='c1')
                P.op('pe', lambda e: e.transpose(ps[0][:, 0:R], rows[0:R, :], ident[0:R, 0:R]),
                     reads=['rows', 'ident'], writes=[PK(0)])
                P.op('dve', lambda e: e.tensor_copy(dst_ap, ps[0][:, 0:R]), reads=[PK(0)], writes=[key])

            to_cols(norm_mix_g, 64, gmix[:], 'gmix')
            to_cols(norm_ffn_g, 64, gffn[:], 'gffn')
            to_cols(pool_scale, 32, pscl[:], 'pscl')
            to_cols(hgrn_lb, 64, lbl[:], 'lbl')
            to_cols(hgrn_norm_g, 16, hng[:], 'hng')
            to_cols(ret_norm_g, 32, rng_[:], 'rng')
            to_cols(final_g, 16, fng[:], 'fng')
            to_cols(cvec, 16, cc[:], 'cc')
            P.op('act', lambda e: e.activation(out=scb[:], in_=cc[:], func=AF.Silu), reads=['cc'], writes=['scb'])
            arow = sb(st, "arow", [1, 6 * D], F32)
            brow = sb(st, "brow", [1, 6 * D], F32)
            wab = [sb(st, "wab%d" % i, [128, KC, 512], BF16) for i in range(2)]
            nb = 0
            for l in range(DEPTH):
                P.op('sp', lambda e, l=l: e.dma_start(out=brow[:], in_=b_ada[l:l + 1, :]), writes=['brow'], dma='c2')
                for j in range(24):
                    s = nb % 2
                    src = w_ada[l, :, j * 512:(j + 1) * 512].rearrange("(kc p) m -> p kc m", p=128)
                    P.op('pool', lambda e, s=s, src=src: e.dma_start(out=wab[s][:], in_=src),
                         writes=[('wab', s)], dma='wa%d' % s)
                    b = nb % 4
                    for kc in range(KC):
                        P.op('pe', lambda e, s=s, b=b, kc=kc: e.matmul(ps[b][0:1, :], scb[:, kc:kc + 1], wab[s][:, kc, :],
                                                                      start=(kc == 0), stop=(kc == KC - 1)),
                             reads=[('wab', s), 'scb'], writes=[PK(b)])
                    P.op('dve', lambda e, b=b, j=j: e.tensor_tensor(out=arow[:, j * 512:(j + 1) * 512], in0=ps[b][0:1, :],
                                                                    in1=brow[:, j * 512:(j + 1) * 512], op=ALU.add),
                         reads=[PK(b), 'brow'], writes=['arow'])
                    nb += 1
                P.op('sp', lambda e, l=l: e.dma_start(out=ada_d[l * 96:(l + 1) * 96, :].rearrange("(o r) c -> o (r c)", o=1),
                                                      in_=arow[:]), reads=['arow'], writes=['ada_d'], dma='c3')
            P.emit()
            for i in range(3):
                to_cols(ada_d[i * 128:(i + 1) * 128, :], 128, adac[:, i * 128:(i + 1) * 128], 'adac')
            for l in range(DEPTH):
                for sub, gt in ((0, gmix), (1, gffn)):
                    sc = adac[:, l * 96 + (1 + 3 * sub) * 16: l * 96 + (2 + 3 * sub) * 16]
                    dst = modA[:, (l * 2 + sub) * 16:(l * 2 + sub + 1) * 16]
                    P.op('dve', lambda e, sc=sc, dst=dst, gt=gt, l=l: e.scalar_tensor_tensor(
                        out=dst, in0=sc, scalar=1.0, in1=gt[:, l * 16:(l + 1) * 16], op0=ALU.add, op1=ALU.mult),
                        reads=['adac', 'gmix', 'gffn'], writes=['modA'])
            P.emit()

        ada_col = lambda l, k, c: adac[:, l * 96 + k * 16 + c: l * 96 + k * 16 + c + 1]

        with ExitStack() as st:
            xt = [sb(st, "xt%d" % i, [128, D], F32) for i in range(2)]
            stg = [sb(st, "stg%d" % i, [128, KC, 128], F32) for i in range(2)]
            for ti in range(NT // 128):
                s = ti % 2
                P.op('sp', lambda e, s=s, ti=ti: e.dma_start(out=xt[s][:], in_=x[ti * 128:(ti + 1) * 128, :]),
                     writes=[('xt', s)], dma='xt%d' % s)
                for q in range(4):
                    b = (ti * 4 + q) % 8
                    for i in range(4):
                        c = q * 4 + i
                        P.op('pe', lambda e, s=s, b=b, i=i, c=c: e.transpose(ps[b][:, i * 128:(i + 1) * 128],
                                                                             xt[s][:, c * 128:(c + 1) * 128], ident[:]),
                             reads=[('xt', s), 'ident'], writes=[PK(b)])
                    eng = 'dve' if q % 2 == 0 else 'act'
                    if eng == 'dve':
                        P.op('dve', lambda e, s=s, b=b, q=q: e.tensor_copy(
                            stg[s][:, q * 4:(q + 1) * 4, :].rearrange("p a b -> p (a b)"), ps[b][:]),
                            reads=[PK(b)], writes=[('stg', s)])
                    else:
                        P.op('act', lambda e, s=s, b=b, q=q: e.copy(
                            stg[s][:, q * 4:(q + 1) * 4, :].rearrange("p a b -> p (a b)"), ps[b][:]),
                            reads=[PK(b)], writes=[('stg', s)])
                P.op('sp', lambda e, s=s, ti=ti: e.dma_start(out=hT3[:, :, ti * 128:(ti + 1) * 128], in_=stg[s][:]),
                     reads=[('stg', s)], dma='st%d' % s)
            P.emit()
            xh = sb(st, "xh", [16, D], F32)
            sth = sb(st, "sth", [128, KC, 16], F32)
            P.op('sp', lambda e: e.dma_start(out=xh[:], in_=xh_d), writes=['xh'], dma='c0')
            for c in range(KC):
                P.op('pe', lambda e, c=c: e.transpose(ps[c % 8][:, 0:16], xh[0:16, c * 128:(c + 1) * 128], ident[0:16, 0:16]),
                     reads=['xh', 'ident'], writes=[PK(c % 8)])
                P.op('dve', lambda e, c=c: e.tensor_copy(sth[:, c, :], ps[c % 8][:, 0:16]), reads=[PK(c % 8)], writes=['sth'])
            P.op('sp', lambda e: e.dma_start(out=hh3, in_=sth[:]), reads=['sth'], dma='c1')
            P.emit()

        def normmod(st_out, t0, N, Acol, Bcol, xT, xkey, halo=0, src=None):
            with ExitStack() as st:
                hall = sb(st, "hall", [128, KC, N], F32)
                sq = [sb(st, "sq%d" % i, [128, N], F32) for i in range(2)]
                rstd = sb(st, "rstd", [128, N], F32)
                tmp = [sb(st, "tmp%d" % i, [128, N], F32) for i in range(2)]
                nt = (N + 511) // 512
                for kc in range(KC):
                    sap = hT[kc * 128:(kc + 1) * 128, t0:t0 + N] if src is None else src(kc)
                    P.op('sp', lambda e, kc=kc, sap=sap: e.dma_start(out=hall[:, kc, :], in_=sap),
                         writes=[('hall', kc)], dma='ha%d' % (kc % 4))
                    s = kc % 2
                    P.op('act', lambda e, kc=kc, s=s: e.activation(out=sq[s][:], in_=hall[:, kc, :], func=AF.Square),
                         reads=[('hall', kc)], writes=[('sq', s)])
                    for n in range(nt):
                        w = min(512, N - n * 512)
                        P.op('pe', lambda e, kc=kc, s=s, n=n, w=w: e.matmul(ps[n][:, 0:w], ones32[:], sq[s][:, n * 512:n * 512 + w],
                                                                            start=(kc == 0), stop=(kc == KC - 1)),
                             reads=[('sq', s), 'ones32'], writes=[PK(n)])
                for n in range(nt):
                    w = min(512, N - n * 512)
                    P.op('act', lambda e, n=n, w=w: e.activation(out=rstd[:, n * 512:n * 512 + w], in_=ps[n][:, 0:w], func=AF.Ln,
                                                                 scale=1.0 / D, bias=epsc[:]),
                         reads=[PK(n), 'epsc'], writes=['rstd'])
                P.op('act', lambda e: e.activation(out=rstd[:], in_=rstd[:], func=AF.Exp, scale=-0.5),
                     reads=['rstd'], writes=['rstd'])
                for kc in range(KC):
                    s = kc % 2
                    P.op('dve', lambda e, kc=kc, s=s: e.scalar_tensor_tensor(out=tmp[s][:], in0=hall[:, kc, :], scalar=Acol(kc),
                                                                             in1=rstd[:], op0=ALU.mult, op1=ALU.mult),
                         reads=[('hall', kc), 'rstd', 'modA'], writes=[('tmp', s)])
                    P.op('act', lambda e, kc=kc, s=s: e.activation(out=xT[:, kc, halo:halo + N], in_=tmp[s][:], func=AF.Identity,
                                                                   bias=Bcol(kc), scale=1.0),
                         reads=[('tmp', s), 'adac'], writes=[(xkey, kc)])
                P.emit()

        def ffn_layer(l):
            for seg in range(NT // SEG):
                t0 = seg * SEG
                with ExitStack() as st:
                    xT = sb(st, "xT", [128, KC, SEG], BF16)
                    normmod(st, t0, SEG, lambda kc: modA[:, (l * 2 + 1) * 16 + kc:(l * 2 + 1) * 16 + kc + 1],
                            lambda kc: ada_col(l, 3, kc), xT, 'xT')
                    hid = sb(st, "hid", [128, FHC, SEG], BF16)
                    wg = [sb(st, "wg%d" % i, [128, KC, 128], BF16) for i in range(3)]
                    wu = [sb(st, "wu%d" % i, [128, KC, 128], BF16) for i in range(3)]
                    sg = [sb(st, "sg%d" % i, [128, 512], F32) for i in range(2)]
                    wo = [sb(st, "wo%d" % i, [128, FHC, 128], BF16) for i in range(2)]
                    hin = [sb(st, "hin%d" % i, [128, SEG], F32) for i in range(2)]
                    hout = [sb(st, "hout%d" % i, [128, SEG], F32) for i in range(2)]
                    NTT = SEG // 512
                    w_in_l = ffn_w_in[l]
                    w_out_l = ffn_w_out[l]

                    def load_in(j):
                        s = j % 3
                        srcg = w_in_l[:, j * 128:(j + 1) * 128].rearrange("(kc p) m -> p kc m", p=128)
                        srcu = w_in_l[:, FH + j * 128:FH + (j + 1) * 128].rearrange("(kc p) m -> p kc m", p=128)
                        P.op('pool', lambda e: e.dma_start(out=wg[s][:], in_=srcg), writes=[('wg', s)], dma='wg%d' % s)
                        P.op('pool', lambda e: e.dma_start(out=wu[s][:], in_=srcu), writes=[('wu', s)], dma='wu%d' % s)

                    def load_out(m):
                        s = m % 2
                        src = w_out_l[:, m * 128:(m + 1) * 128].rearrange("(kc p) m -> p kc m", p=128)
                        P.op('pool', lambda e: e.dma_start(out=wo[s][:], in_=src), writes=[('wo', s)], dma='wo%d' % s)

                    load_in(0)
                    load_in(1)
                    cnt = 0
                    for j in range(FHC):
                        if j + 2 < FHC:
                            load_in(j + 2)
                        elif j + 2 == FHC:
                            load_out(0)
                        elif j + 2 == FHC + 1:
                            load_out(1)
                        s = j % 3
                        for n in range(NTT):
                            bg = (cnt * 2) % 8
                            bu = bg + 1
                            cnt += 1
                            for kc in range(KC):
                                P.op('pe', lambda e, s=s, bg=bg, kc=kc, n=n: e.matmul(
                                    ps[bg][:], wg[s][:, kc, :], xT[:, kc, n * 512:(n + 1) * 512], start=(kc == 0), stop=(kc == KC - 1)),
                                    reads=[('wg', s), ('xT', kc)], writes=[PK(bg)])
                            for kc in range(KC):
                                P.op('pe', lambda e, s=s, bu=bu, kc=kc, n=n: e.matmul(
                                    ps[bu][:], wu[s][:, kc, :], xT[:, kc, n * 512:(n + 1) * 512], start=(kc == 0), stop=(kc == KC - 1)),
                                    reads=[('wu', s), ('xT', kc)], writes=[PK(bu)])
                            ss = cnt % 2
                            P.op('act', lambda e, ss=ss, bg=bg: e.activation(out=sg[ss][:], in_=ps[bg][:], func=AF.Silu),
                                 reads=[PK(bg)], writes=[('sg', ss)])
                            P.op('dve', lambda e, ss=ss, bu=bu, j=j, n=n: e.tensor_tensor(
                                out=hid[:, j, n * 512:(n + 1) * 512], in0=sg[ss][:], in1=ps[bu][:], op=ALU.mult),
                                reads=[('sg', ss), PK(bu)], writes=[('hid', j)])
                    for m in range(KC):
                        if m >= 1 and m + 1 < KC:
                            load_out(m + 1)
                        s = m % 2
                        P.op('sp', lambda e, s=s, m=m: e.dma_start(out=hin[s][:], in_=hT[m * 128:(m + 1) * 128, t0:t0 + SEG]),
                             writes=[('hin', s)], dma='hin%d' % s)
                        for n in range(NTT):
                            b = cnt % 8
                            cnt += 1
                            for kc in range(FHC):
                                P.op('pe', lambda e, s=s, b=b, kc=kc, n=n: e.matmul(
                                    ps[b][:], wo[s][:, kc, :], hid[:, kc, n * 512:(n + 1) * 512], start=(kc == 0), stop=(kc == FHC - 1)),
                                    reads=[('wo', s), ('hid', kc)], writes=[PK(b)])
                            P.op('dve', lambda e, s=s, b=b, m=m, n=n: e.scalar_tensor_tensor(
                                out=hout[s][:, n * 512:(n + 1) * 512], in0=ps[b][:], scalar=ada_col(l, 5, m),
                                in1=hin[s][:, n * 512:(n + 1) * 512], op0=ALU.mult, op1=ALU.add),
                                reads=[PK(b), ('hin', s), 'adac'], writes=[('hout', s)])
                        P.op('sp', lambda e, s=s, m=m: e.dma_start(out=hT[m * 128:(m + 1) * 128, t0:t0 + SEG], in_=hout[s][:]),
                             reads=[('hout', s)], dma='hout%d' % s)
                    P.emit()


        inv16 = sb(gs, "inv16", [128, 4, 16], F32)
        for g in range(4):
            w = 2 ** (g + 1)
            P.op('pool', lambda e, g=g, w=w: e.memset(inv16[:, g, :], 1.0 / w), writes=['inv16'])
            for t in range(w - 1):
                P.op('pool', lambda e, g=g, t=t: e.memset(inv16[:, g, t:t + 1], 1.0 / (t + 1)), writes=['inv16'])
        P.emit()

        def pool_layer(l):
            j = l // 3
            hsrc = hh
            if l > 0:
                hsend = nc.dram_tensor("hsend%d" % l, [D, 16], F32, kind="Internal").ap()
                hrecv = nc.dram_tensor("hrecv%d" % l, [2 * D, 16], F32, kind="Internal").ap()
                P.op('sp', lambda e: e.dma_start(out=hsend, in_=hT[:, NT - 16:NT]), dma='c0')
                P.emit()
                exchange(hsend, hrecv, D, D)
                hsrc = hrecv
            with ExitStack() as st0:
                gp = sb(st0, "gp", [128, 16], F32)
                P.op('dve', lambda e: e.tensor_tensor(out=gp[:], in0=adac[:, l * 96 + 32:l * 96 + 48],
                                                     in1=pscl[:, j * 16:(j + 1) * 16], op=ALU.mult),
                     reads=['adac', 'pscl'], writes=['gp'])
                halo_t = sb(st0, "halo_t", [128, KC, 16], F32)
                inve = sb(st0, "inve", [128, 4, 16], F32)
                for g in range(4):
                    P.op('dve', lambda e, g=g: e.tensor_scalar(inve[:, g, :], inv16[:, g, :], -1.0, 1.0 / 2 ** (g + 1), op0=ALU.mult, op1=ALU.add),
                         reads=['inv16'], writes=['inve'])
                    P.op('dve', lambda e, g=g: e.scalar_tensor_tensor(out=inve[:, g, :], in0=inve[:, g, :], scalar=flag[:, 0:1],
                                                                      in1=inv16[:, g, :], op0=ALU.mult, op1=ALU.add),
                         reads=['inv16', 'inve', 'flag'], writes=['inve'])
                wp = sb(st0, "wp", [128, 4, 4, 512], BF16)
                for g in range(4):
                    P.op('pool', lambda e, g=g: e.dma_start(out=wp[:, g], in_=pool_w[j, g].rearrange("(kc p) m -> p kc m", p=128)),
                         writes=[('wp', g)], dma='wp')
                P.emit()
                for seg in range(NT // SEG):
                    t0 = seg * SEG
                    N = SEG
                    with ExitStack() as st:
                        uP = sb(st, "uP", [128, KC, 16 + N], F32)
                        Acol = lambda kc: modA[:, (l * 2) * 16 + kc:(l * 2) * 16 + kc + 1]
                        Bcol = lambda kc: ada_col(l, 0, kc)
                        if t0 == 0:
                            uh = sb(st, "uh", [128, KC, 16], F32)
                            normmod(st, 0, 16, Acol, Bcol, uh, 'uh', halo=0, src=lambda kc: hsrc[kc * 128:(kc + 1) * 128, :])
                            P.op('dve', lambda e: e.tensor_scalar(uP[:, :, 0:16], uh[:], flag[:, 0:1], None, op0=ALU.mult),
                                 reads=['flag'], writes=[('uP', kc) for kc in range(KC)])
                        else:
                            P.op('pool', lambda e: e.tensor_copy(uP[:, :, 0:16], halo_t[:]), reads=['halo_t'],
                                 writes=[('uP', kc) for kc in range(KC)])
                        normmod(st, t0, N, Acol, Bcol, uP, 'uP', halo=16)
                        P.op('pool', lambda e: e.tensor_copy(halo_t[:], uP[:, :, N:N + 16]), reads=[('uP', kc) for kc in range(KC)],
                             writes=['halo_t'])
                        tA = sb(st, "tA", [128, 16 + N], F32)
                        tB = sb(st, "tB", [128, 16 + N], F32)
                        pT = sb(st, "pT", [128, KC, N], BF16)
                        hin = [sb(st, "hin%d" % i, [128, N], F32) for i in range(2)]
                        hout = [sb(st, "hout%d" % i, [128, N], F32) for i in range(2)]
                        E = 16 + N
                        for c in range(KC):
                            g = c // 4
                            w = 2 ** (g + 1)
                            u = uP[:, c, :]
                            P.op('dve', lambda e, u=u: e.tensor_tensor(out=tA[:, 1:E], in0=u[:, 1:E], in1=u[:, 0:E - 1], op=ALU.add),
                                 reads=[('uP', c)], writes=['tA'])
                            cur, curk = tA, 'tA'
                            if g >= 1:
                                P.op('dve', lambda e: e.tensor_tensor(out=tB[:, 3:E], in0=tA[:, 3:E], in1=tA[:, 1:E - 2], op=ALU.add),
                                     reads=['tA'], writes=['tB'])
                                cur, curk = tB, 'tB'
                            if g >= 2:
                                P.op('dve', lambda e: e.tensor_tensor(out=tA[:, 7:E], in0=tB[:, 7:E], in1=tB[:, 3:E - 4], op=ALU.add),
                                     reads=['tB'], writes=['tA'])
                                cur, curk = tA, 'tA'
                            if g >= 3:
                                P.op('dve', lambda e: e.tensor_tensor(out=tB[:, 15:E], in0=tA[:, 15:E], in1=tA[:, 7:E - 8], op=ALU.add),
                                     reads=['tA'], writes=['tB'])
                                cur, curk = tB, 'tB'
                            lo = 16 if t0 == 0 else 0
                            P.op('dve', lambda e, cur=cur, u=u, c=c, w=w, lo=lo: e.scalar_tensor_tensor(
                                out=pT[:, c, lo:N], in0=cur[:, 16 + lo:E], scalar=1.0 / w, in1=u[:, 16 + lo:E],
                                op0=ALU.mult, op1=ALU.subtract),
                                reads=[curk, ('uP', c)], writes=[('pT', c)])
                            if t0 == 0:
                                P.op('dve', lambda e, cur=cur, g=g: e.tensor_tensor(out=cur[:, 16:32], in0=cur[:, 16:32],
                                                                                    in1=inve[:, g, :], op=ALU.mult),
                                     reads=[curk, 'inve', ('pT', c)], writes=[curk])
                                P.op('dve', lambda e, cur=cur, u=u, c=c: e.tensor_tensor(out=pT[:, c, 0:16], in0=cur[:, 16:32],
                                                                                         in1=u[:, 16:32], op=ALU.subtract),
                                     reads=[curk, ('uP', c)], writes=[('pT', c)])
                        cnt = 0
                        for c in range(KC):
                            g, m = c // 4, c % 4
                            s = c % 2
                            P.op('sp', lambda e, s=s, c=c: e.dma_start(out=hin[s][:], in_=hT[c * 128:(c + 1) * 128, t0:t0 + N]),
                                 writes=[('hin', s)], dma='hin%d' % s)
                            for n in range(N // 512):
                                b = cnt % 8
                                cnt += 1
                                for kc in range(4):
                                    P.op('pe', lambda e, g=g, m=m, kc=kc, b=b, n=n: e.matmul(
                                        ps[b][:], wp[:, g, kc, m * 128:(m + 1) * 128], pT[:, 4 * g + kc, n * 512:(n + 1) * 512],
                                        start=(kc == 0), stop=(kc == 3)),
                                        reads=[('wp', g), ('pT', 4 * g + kc)], writes=[PK(b)])
                                P.op('dve', lambda e, s=s, b=b, c=c, n=n: e.scalar_tensor_tensor(
                                    out=hout[s][:, n * 512:(n + 1) * 512], in0=ps[b][:], scalar=gp[:, c:c + 1],
                                    in1=hin[s][:, n * 512:(n + 1) * 512], op0=ALU.mult, op1=ALU.add),
                                    reads=[PK(b), ('hin', s), 'gp'], writes=[('hout', s)])
                            P.op('sp', lambda e, s=s, c=c: e.dma_start(out=hT[c * 128:(c + 1) * 128, t0:t0 + N], in_=hout[s][:]),
                                 reads=[('hout', s)], dma='hout%d' % s)
                        P.emit()


        maskT = sb(gs, "maskT", [128, 128], F32)
        P.op('pool', lambda e: e.memset(maskT[:], 1.0), writes=['maskT'])
        P.op('pool', lambda e: e.affine_select(out=maskT[:], in_=maskT[:], pattern=[[1, 128]], compare_op=ALU.is_ge,
                                               fill=0.0, base=0, channel_multiplier=-1), reads=['maskT'], writes=['maskT'])
        P.emit()

        def lin_feat(uT, w2d, col0, nblk, N, epi, tag, kcn=KC, nbuf=3):
            with ExitStack() as st:
                wb = [sb(st, "lw%d" % i, [128, kcn, 128], BF16) for i in range(nbuf)]

                def load(j):
                    s_ = j % nbuf
                    src = w2d[:, col0 + j * 128: col0 + (j + 1) * 128].rearrange("(kc p) m -> p kc m", p=128)
                    P.op('pool', lambda e: e.dma_start(out=wb[s_][:], in_=src), writes=[('lw', s_)], dma='lw%d' % s_)
                for j in range(min(nbuf - 1, nblk)):
                    load(j)
                cnt = 0
                for j in range(nblk):
                    if j + nbuf - 1 < nblk:
                        load(j + nbuf - 1)
                    s_ = j % nbuf
                    for n in range(N // 512):
                        b = cnt % 4
                        cnt += 1
                        for kc in range(kcn):
                            P.op('pe', lambda e, s_=s_, b=b, kc=kc, n=n: e.matmul(
                                ps[b][:], wb[s_][:, kc, :], uT[:, kc, n * 512:(n + 1) * 512], start=(kc == 0), stop=(kc == kcn - 1)),
                                reads=[('lw', s_), (tag, kc)], writes=[PK(b)])
                        epi(j, n, b)
                P.emit()

        def lin_tok(uT, w2d, col0, nblk, N, dst_d, t0, tag):
            with ExitStack() as st:
                wb = [sb(st, "tw%d" % i, [128, KC, 512], BF16) for i in range(2)]
                vt = [sb(st, "vt%d" % i, [128, 512], BF16) for i in range(2)]
                cnt = 0
                for j in range(nblk):
                    s_ = j % 2
                    src = w2d[:, col0 + j * 512: col0 + (j + 1) * 512].rearrange("(kc p) m -> p kc m", p=128)
                    P.op('pool', lambda e, s_=s_, src=src: e.dma_start(out=wb[s_][:], in_=src), writes=[('tw', s_)], dma='tw%d' % s_)
                    for ti in range(N // 128):
                        b = 4 + cnt % 4
                        v_ = cnt % 2
                        cnt += 1
                        for kc in range(KC):
                            P.op('pe', lambda e, s_=s_, b=b, kc=kc, ti=ti: e.matmul(
                                ps[b][:], uT[:, kc, ti * 128:(ti + 1) * 128], wb[s_][:, kc, :], start=(kc == 0), stop=(kc == KC - 1)),
                                reads=[('tw', s_), (tag, kc)], writes=[PK(b)])
                        P.op('act', lambda e, v_=v_, b=b: e.copy(vt[v_][:], ps[b][:]), reads=[PK(b)], writes=[('vt', v_)])
                        P.op('sp', lambda e, v_=v_, ti=ti, j=j: e.dma_start(
                            out=dst_d[t0 + ti * 128:t0 + (ti + 1) * 128, j * 512:(j + 1) * 512], in_=vt[v_][:]),
                            reads=[('vt', v_)], dma='vt%d' % v_)
                P.emit()

        def out_proj(l, y_d, w2d, kcn):
            for seg in range(NT // SEG):
                t0 = seg * SEG
                with ExitStack() as st:
                    yT = sb(st, "yT", [128, kcn, SEG], BF16)
                    for kc in range(kcn):
                        P.op('sp', lambda e, kc=kc: e.dma_start(out=yT[:, kc, :], in_=y_d[kc * 128:(kc + 1) * 128, t0:t0 + SEG]),
                             writes=[('yT', kc)], dma='yl%d' % (kc % 4))
                    hin = [sb(st, "hin%d" % i, [128, SEG], F32) for i in range(2)]
                    hout = [sb(st, "hout%d" % i, [128, SEG], F32) for i in range(2)]

                    def epi(j, n, b):
                        s_ = j % 2
                        if n == 0:
                            P.op('sp', lambda e: e.dma_start(out=hin[s_][:], in_=hT[j * 128:(j + 1) * 128, t0:t0 + SEG]),
                                 writes=[('hin', s_)], dma='hin%d' % s_)
                        P.op('dve', lambda e: e.scalar_tensor_tensor(
                            out=hout[s_][:, n * 512:(n + 1) * 512], in0=ps[b][:], scalar=ada_col(l, 2, j),
                            in1=hin[s_][:, n * 512:(n + 1) * 512], op0=ALU.mult, op1=ALU.add),
                            reads=[PK(b), ('hin', s_), 'adac'], writes=[('hout', s_)])
                        if n == SEG // 512 - 1:
                            P.op('sp', lambda e: e.dma_start(out=hT[j * 128:(j + 1) * 128, t0:t0 + SEG], in_=hout[s_][:]),
                                 reads=[('hout', s_)], dma='hout%d' % s_)
                    lin_feat(yT, w2d, 0, KC, SEG, epi, 'yT', kcn=kcn, nbuf=2)

        def gla_chunks(H, nkc, nvc, Cc, qd_d, ki_d, ke_d, v_d, o_d, send_d, dec_d, dec_imm, slots):
            dv = nvc * 128
            nch = NT // Cc
            W = min(512, NT)
            cpw = W // Cc
            for h0 in range(0, H, slots):
                with ExitStack() as st:
                    hs = list(range(h0, min(H, h0 + slots)))
                    T = {}
                    for si, h in enumerate(hs):
                        t = {}
                        t['qd'] = sb(st, "qd", [128, nkc, NT], BF16)
                        t['ki'] = sb(st, "ki", [128, nkc, NT], BF16)
                        t['ke'] = sb(st, "ke", [128, nkc, NT], BF16)
                        t['v'] = sb(st, "v", [Cc, nch, dv], BF16)
                        t['S'] = sb(st, "S", [128, nkc, dv], F32)
                        t['Sb'] = sb(st, "Sb", [128, nkc, dv], BF16)
                        t['Pm'] = sb(st, "Pm", [Cc, Cc], BF16)
                        t['keT'] = sb(st, "keT", [Cc, nkc * 128], BF16)
                        t['ow'] = [sb(st, "ow%d" % i, [128, nvc, W], F32) for i in range(2)]
                        if dec_d is not None:
                            t['dec'] = sb(st, "dec", [128, nch], F32)
                        T[si] = t
                        k = lambda nm, si=si: (nm, si)
                        for kc in range(nkc):
                            r0 = (h * nkc + kc) * 128
                            P.op('sp', lambda e, t=t, kc=kc, r0=r0: e.dma_start(out=t['qd'][:, kc, :], in_=qd_d[r0:r0 + 128, :]),
                                 writes=[k('qd')], dma='g0')
                            P.op('sp', lambda e, t=t, kc=kc, r0=r0: e.dma_start(out=t['ki'][:, kc, :], in_=ki_d[r0:r0 + 128, :]),
                                 writes=[k('ki')], dma='g1')
                            P.op('sp', lambda e, t=t, kc=kc, r0=r0: e.dma_start(out=t['ke'][:, kc, :], in_=ke_d[r0:r0 + 128, :]),
                                 writes=[k('ke')], dma='g2')
                        P.op('sp', lambda e, t=t, h=h: e.dma_start(
                            out=t['v'][:], in_=v_d[:, h * dv:(h + 1) * dv].rearrange("(n p) d -> p n d", p=Cc)),
                            writes=[k('v')], dma='g3')
                        if dec_d is not None:
                            P.op('sp', lambda e, t=t, h=h: e.dma_start(out=t['dec'][:], in_=dec_d[h * 128:(h + 1) * 128, :]),
                                 writes=[k('dec')], dma='g5')
                        P.op('pool', lambda e, t=t: e.memset(t['S'][:], 0.0), writes=[k('S')])
                        P.op('pool', lambda e, t=t: e.memset(t['Sb'][:], 0.0), writes=[k('Sb')])
                    nb = 8 // len(hs)
                    for n in range(nch):
                        c0 = n * Cc
                        for si, h in enumerate(hs):
                            t = T[si]
                            k = lambda nm, si=si: (nm, si)
                            bA, bB, bC = si * nb, si * nb + 1, si * nb + 2
                            bD = [si * nb + 3 + kc for kc in range(nkc)]
                            wsl = (n // cpw) % 2
                            for kc in range(nkc):
                                P.op('pe', lambda e, t=t, kc=kc, bA=bA, c0=c0: e.matmul(
                                    ps[bA][0:Cc, 0:Cc], t['ki'][:, kc, c0:c0 + Cc], t['qd'][:, kc, c0:c0 + Cc],
                                    start=(kc == 0), stop=(kc == nkc - 1)), reads=[k('ki'), k('qd')], writes=[PK(bA)])
                            P.op('dve', lambda e, t=t, bA=bA: e.tensor_tensor(out=t['Pm'][:], in0=ps[bA][0:Cc, 0:Cc],
                                                                              in1=maskT[0:Cc, 0:Cc], op=ALU.mult),
                                 reads=[PK(bA), 'maskT'], writes=[k('Pm')])
                            psb = ps[bB].bitcast(BF16)
                            for kc in range(nkc):
                                P.op('pe', lambda e, t=t, kc=kc, psb=psb, c0=c0: e.transpose(
                                    psb[0:Cc, kc * 128:(kc + 1) * 128], t['ke'][:, kc, c0:c0 + Cc], identb[:]),
                                    reads=[k('ke'), 'identb'], writes=[PK(bB)])
                            P.op('act', lambda e, t=t, psb=psb: e.copy(t['keT'][:], psb[0:Cc, 0:nkc * 128]),
                                 reads=[PK(bB)], writes=[k('keT')])
                            for vc in range(nvc):
                                P.op('pe', lambda e, t=t, vc=vc, bC=bC, n=n: e.matmul(
                                    ps[bC][:, vc * Cc:(vc + 1) * Cc], t['v'][:, n, vc * 128:(vc + 1) * 128], t['Pm'][:],
                                    start=True, stop=False), reads=[k('v'), k('Pm')], writes=[PK(bC)])
                                for kc in range(nkc):
                                    P.op('pe', lambda e, t=t, vc=vc, kc=kc, bC=bC, c0=c0: e.matmul(
                                        ps[bC][:, vc * Cc:(vc + 1) * Cc], t['Sb'][:, kc, vc * 128:(vc + 1) * 128],
                                        t['qd'][:, kc, c0:c0 + Cc], start=False, stop=(kc == nkc - 1)),
                                        reads=[k('Sb'), k('qd')], writes=[PK(bC)])
                            wo_ = (n % cpw) * Cc
                            P.op('act', lambda e, t=t, bC=bC, wo_=wo_, wsl=wsl: e.copy(
                                t['ow'][wsl][:, :, wo_:wo_ + Cc], ps[bC][:, 0:nvc * Cc].rearrange("p (a b) -> p a b", a=nvc)),
                                reads=[PK(bC)], writes=[('ow%d' % wsl, si)])
                            for kc in range(nkc):
                                P.op('pe', lambda e, t=t, kc=kc, n=n, b=bD[kc]: e.matmul(
                                    ps[b][:, 0:dv], t['keT'][:, kc * 128:(kc + 1) * 128], t['v'][:, n, :], start=True, stop=True),
                                    reads=[k('keT'), k('v')], writes=[PK(bD[kc])])
                                dsc = t['dec'][:, n:n + 1] if dec_d is not None else float(dec_imm[h])
                                P.op('dve', lambda e, t=t, kc=kc, b=bD[kc], dsc=dsc: e.scalar_tensor_tensor(
                                    out=t['S'][:, kc, :], in0=t['S'][:, kc, :], scalar=dsc, in1=ps[b][:, 0:dv],
                                    op0=ALU.mult, op1=ALU.add), reads=[PK(bD[kc]), k('S'), k('dec')], writes=[k('S')])
                            P.op('act', lambda e, t=t: e.copy(t['Sb'][:], t['S'][:]), reads=[k('S')], writes=[k('Sb')])
                            if (n + 1) % cpw == 0:
                                w0 = (n + 1) * Cc - W
                                for vc in range(nvc):
                                    r0 = (h * nvc + vc) * 128
                                    P.op('sp', lambda e, t=t, vc=vc, r0=r0, w0=w0, wsl=wsl: e.dma_start(
                                        out=o_d[r0:r0 + 128, w0:w0 + W], in_=t['ow'][wsl][:, vc, :]),
                                        reads=[('ow%d' % wsl, si)], dma='go%d%d' % (si, wsl))
                    for si, h in enumerate(hs):
                        t = T[si]
                        for kc in range(nkc):
                            r0 = (h * nkc + kc) * 128
                            P.op('sp', lambda e, t=t, kc=kc, r0=r0: e.dma_start(out=send_d[r0:r0 + 128, :], in_=t['Sb'][:, kc, :]),
                                 reads=[('Sb', si)], dma='g6')
                    P.emit()

        def exchange(send_d, recv_d, rows, step):
            for r0 in range(0, rows, step):
                P.op('pool', lambda e, r0=r0: e.collective_compute(
                    "AllGather", ALU.bypass, replica_groups=pairs, ins=[send_d[r0:r0 + step, :]],
                    outs=[recv_d[2 * r0:2 * r0 + 2 * step, :]]), dma='cc', inc=1)
            P.emit()

        def gla_post(H, nkc, nvc, qt_d, recv_d, rstep, o_d, sg_d, y_d, normg):
            dv = nvc * 128
            W = min(512, NT)
            with ExitStack() as st:
                Sr = [sb(st, "Sr%d" % i, [128, nkc, dv], BF16) for i in range(2)]
                qt = [sb(st, "qt%d" % i, [128, nkc, W], BF16) for i in range(2)]
                ow = [sb(st, "pow%d" % i, [128, nvc, W], F32) for i in range(2)]
                sgt = [sb(st, "psg%d" % i, [128, nvc, W], BF16) for i in range(2)]
                yt = [sb(st, "pyt%d" % i, [128, nvc, W], BF16) for i in range(2)]
                sq = sb(st, "psq", [128, nvc, W], F32)
                rs = sb(st, "prs", [128, W], F32)
                tmp = sb(st, "ptmp", [128, W], F32)
                cnt = 0
                for h in range(H):
                    hz = h % 2
                    for kc in range(nkc):
                        r = (h * nkc + kc) * 128
                        rr = (r // rstep) * 2 * rstep + (r % rstep)
                        P.op('sp', lambda e, hz=hz, kc=kc, rr=rr: e.dma_start(out=Sr[hz][:, kc, :], in_=recv_d[rr:rr + 128, :]),
                             writes=[('Sr', hz)], dma='p0%d' % hz)
                    P.op('dve', lambda e, hz=hz: e.tensor_scalar(Sr[hz][:], Sr[hz][:], flag[:, 0:1], None, op0=ALU.mult),
                         reads=[('Sr', hz), 'flag'], writes=[('Sr', hz)])
                    for wi_ in range(NT // W):
                        w0 = wi_ * W
                        z = cnt % 2
                        cnt += 1
                        for kc in range(nkc):
                            r = (h * nkc + kc) * 128
                            P.op('sp', lambda e, z=z, kc=kc, r=r, w0=w0: e.dma_start(out=qt[z][:, kc, :], in_=qt_d[r:r + 128, w0:w0 + W]),
                                 writes=[('qt', z)], dma='p1%d' % z)
                        for vc in range(nvc):
                            r = (h * nvc + vc) * 128
                            P.op('sp', lambda e, z=z, vc=vc, r=r, w0=w0: e.dma_start(out=ow[z][:, vc, :], in_=o_d[r:r + 128, w0:w0 + W]),
                                 writes=[('ow', z)], dma='p2%d' % z)
                            P.op('sp', lambda e, z=z, vc=vc, r=r, w0=w0: e.dma_start(out=sgt[z][:, vc, :], in_=sg_d[r:r + 128, w0:w0 + W]),
                                 writes=[('sgt', z)], dma='p3%d' % z)
                        for vc in range(nvc):
                            b = (cnt * nvc + vc) % 6
                            for kc in range(nkc):
                                P.op('pe', lambda e, hz=hz, z=z, vc=vc, kc=kc, b=b: e.matmul(
                                    ps[b][:, 0:W], Sr[hz][:, kc, vc * 128:(vc + 1) * 128], qt[z][:, kc, :],
                                    start=(kc == 0), stop=(kc == nkc - 1)), reads=[('Sr', hz), ('qt', z)], writes=[PK(b)])
                            P.op('dve', lambda e, z=z, vc=vc, b=b: e.tensor_tensor(out=ow[z][:, vc, :], in0=ow[z][:, vc, :],
                                                                                  in1=ps[b][:, 0:W], op=ALU.add),
                                 reads=[PK(b), ('ow', z)], writes=[('ow', z)])
                        P.op('act', lambda e, z=z: e.activation(out=sq[:], in_=ow[z][:], func=AF.Square),
                             reads=[('ow', z)], writes=['sq'])
                        bs = 6 + cnt % 2
                        for vc in range(nvc):
                            P.op('pe', lambda e, vc=vc, bs=bs: e.matmul(ps[bs][:, 0:W], ones32[:], sq[:, vc, :],
                                                                        start=(vc == 0), stop=(vc == nvc - 1)),
                                 reads=['sq', 'ones32'], writes=[PK(bs)])
                        P.op('act', lambda e, bs=bs: e.activation(out=rs[:], in_=ps[bs][:, 0:W], func=AF.Ln, scale=1.0 / dv, bias=epsc[:]),
                             reads=[PK(bs), 'epsc'], writes=['rs'])
                        P.op('act', lambda e: e.activation(out=rs[:], in_=rs[:], func=AF.Exp, scale=-0.5), reads=['rs'], writes=['rs'])
                        for vc in range(nvc):
                            gcol = normg[:, h * nvc + vc:h * nvc + vc + 1]
                            P.op('dve', lambda e, z=z, vc=vc, gcol=gcol: e.scalar_tensor_tensor(
                                out=tmp[:], in0=ow[z][:, vc, :], scalar=gcol, in1=rs[:], op0=ALU.mult, op1=ALU.mult),
                                reads=[('ow', z), 'rs'], writes=['tmp'])
                            P.op('dve', lambda e, z=z, vc=vc: e.tensor_tensor(out=yt[z][:, vc, :], in0=tmp[:], in1=sgt[z][:, vc, :], op=ALU.mult),
                                 reads=['tmp', ('sgt', z)], writes=[('yt', z)])
                            r = (h * nvc + vc) * 128
                            P.op('sp', lambda e, z=z, vc=vc, r=r, w0=w0: e.dma_start(out=y_d[r:r + 128, w0:w0 + W], in_=yt[z][:, vc, :]),
                                 reads=[('yt', z)], dma='p4%d' % z)
                P.emit()

        def hgrn_layer(l):
            dram = lambda n, sh, dt: nc.dram_tensor(n, sh, dt, kind=("ExternalOutput" if DEBUG else "Internal")).ap()
            qd_d = dram("h_qd", [D, NT], BF16)
            ki_d = dram("h_ki", [D, NT], BF16)
            ke_d = dram("h_ke", [D, NT], BF16)
            v_d = dram("h_v", [NT, D], BF16)
            sg_d = dram("h_sg", [D, NT], BF16)
            y_d = dram("h_y", [D, NT], BF16)
            dec_d = dram("h_dec", [D, NT // 32], F32)
            qt_d = dram("h_qt", [D, NT], BF16)
            o_d = dram("h_o", [D, NT], F32)
            send_d = dram("h_send", [D, 128], BF16)
            recv_d = dram("h_recv", [2 * D, 128], BF16)
            with ExitStack() as st0:
                lb = sb(st0, "lb", [128, 16], F32)
                oml = sb(st0, "oml", [128, 16], F32)
                noml = sb(st0, "noml", [128, 16], F32)
                ex = sb(st0, "ex", [128, 64], F32)
                m32 = sb(st0, "m32", [128, 512], F32)
                P.op('act', lambda e: e.activation(out=ex[:], in_=lbl[:], func=AF.Exp), reads=['lbl'], writes=['ex'])
                P.op('dve', lambda e: e.tensor_tensor(out=lb[:], in0=ex[:, 0:16], in1=ex[:, 16:32], op=ALU.add), reads=['ex'], writes=['lb'])
                P.op('dve', lambda e: e.tensor_tensor(out=lb[:], in0=lb[:], in1=ex[:, 32:48], op=ALU.add), reads=['ex', 'lb'], writes=['lb'])
                P.op('dve', lambda e: e.tensor_tensor(out=lb[:], in0=lb[:], in1=ex[:, 48:64], op=ALU.add), reads=['ex', 'lb'], writes=['lb'])
                P.op('dve', lambda e: e.reciprocal(lb[:], lb[:]), reads=['lb'], writes=['lb'])
                P.op('dve', lambda e: e.tensor_tensor(out=oml[:], in0=ex[:, 16:32], in1=lb[:], op=ALU.mult), reads=['ex', 'lb'], writes=['oml'])
                for i in range(2, l + 1):
                    P.op('dve', lambda e, i=i: e.scalar_tensor_tensor(out=oml[:], in0=ex[:, 16 * i:16 * i + 16], scalar=1.0, in1=lb[:],
                                                                      op0=ALU.mult, op1=ALU.mult), reads=['ex', 'lb'], writes=['noml'])
                    P.op('dve', lambda e: e.tensor_tensor(out=oml[:], in0=oml[:], in1=noml[:], op=ALU.add), reads=['noml', 'oml'], writes=['oml'])
                P.op('dve', lambda e: e.tensor_copy(lb[:], oml[:]), reads=['oml'], writes=['lb'])
                P.op('dve', lambda e: e.tensor_scalar(oml[:], lb[:], -1.0, 1.0, op0=ALU.mult, op1=ALU.add), reads=['lb'], writes=['oml'])
                P.op('dve', lambda e: e.tensor_scalar(noml[:], oml[:], -1.0, None, op0=ALU.mult), reads=['oml'], writes=['noml'])
                o512 = sb(st0, "o512", [128, 512], F32)
                cBl = sb(st0, "cBl", [128, 16], F32)
                P.op('pool', lambda e: e.memset(o512[:], 1.0), writes=['o512'])
                P.op('pool', lambda e: e.memset(cBl[:], 0.0), writes=['cBl'])
                P.op('pool', lambda e: e.memset(m32[:], 1.0), writes=['m32'])
                P.op('pool', lambda e: e.memset(m32[:].rearrange("p (a b) -> p a b", b=32)[:, :, 0:1], 0.0), writes=['m32'])
                P.emit()
                for seg in range(NT // SEG):
                    t0 = seg * SEG
                    with ExitStack() as st:
                        uT = sb(st, "uT", [128, KC, SEG], BF16)
                        normmod(st, t0, SEG, lambda kc: modA[:, (l * 2) * 16 + kc:(l * 2) * 16 + kc + 1],
                                lambda kc: ada_col(l, 0, kc), uT, 'uT')
                        with ExitStack() as st2:
                            F = lambda nm: [sb(st2, nm + "%d" % i, [128, 512], F32) for i in range(2)]
                            Bf = lambda nm: [sb(st2, nm + "%d" % i, [128, 512], BF16) for i in range(2)]
                            sgm, lf, kk, cum, ec, ei, ee = F("sgm"), F("lf"), F("kk"), F("cum"), F("ec"), F("ei"), F("ee")
                            qd, ki, ke = Bf("qd"), Bf("ki"), Bf("ke")
                            cB, eB, qtb = F("cB"), F("eB"), Bf("qtb")
                            dcs = [sb(st2, "dcs%d" % i, [128, 16], F32) for i in range(2)]
                            wq = [sb(st2, "wq%d" % i, [128, KC, 128], BF16) for i in range(2)]
                            wf = [sb(st2, "wf%d" % i, [128, KC, 128], BF16) for i in range(2)]
                            cnt = 0
                            for h in range(16):
                                s_ = h % 2
                                for wt, c0, nm in ((wq, h * 128, 'wq'), (wf, 2048 + h * 128, 'wf')):
                                    src = hgrn_w_in[:, c0:c0 + 128].rearrange("(kc p) m -> p kc m", p=128)
                                    P.op('pool', lambda e, wt=wt, src=src, s_=s_: e.dma_start(out=wt[s_][:], in_=src),
                                         writes=[(nm, s_)], dma=nm + '%d' % s_)
                                for n in range(SEG // 512):
                                    bq, bf_ = (cnt * 2) % 8, (cnt * 2) % 8 + 1
                                    z = cnt % 2
                                    cnt += 1
                                    K_ = lambda nm, z=z: (nm, z)
                                    for kc in range(KC):
                                        P.op('pe', lambda e, s_=s_, kc=kc, n=n, bq=bq: e.matmul(
                                            ps[bq][:], wq[s_][:, kc, :], uT[:, kc, n * 512:(n + 1) * 512], start=(kc == 0), stop=(kc == KC - 1)),
                                            reads=[('wq', s_), ('uT', kc)], writes=[PK(bq)])
                                    for kc in range(KC):
                                        P.op('pe', lambda e, s_=s_, kc=kc, n=n, bf_=bf_: e.matmul(
                                            ps[bf_][:], wf[s_][:, kc, :], uT[:, kc, n * 512:(n + 1) * 512], start=(kc == 0), stop=(kc == KC - 1)),
                                            reads=[('wf', s_), ('uT', kc)], writes=[PK(bf_)])
                                    P.op('act', lambda e, z=z, bf_=bf_: e.activation(out=sgm[z][:], in_=ps[bf_][:], func=AF.Exp, scale=-1.0),
                                         reads=[PK(bf_)], writes=[K_('sgm')])
                                    P.op('dve', lambda e, z=z: e.tensor_scalar(sgm[z][:], sgm[z][:], 1.0, None, op0=ALU.add),
                                         reads=[K_('sgm')], writes=[K_('sgm')])
                                    P.op('dve', lambda e, z=z: e.reciprocal(sgm[z][:], sgm[z][:]), reads=[K_('sgm')], writes=[K_('sgm')])
                                    P.op('act', lambda e, z=z, h=h: e.activation(out=lf[z][:], in_=sgm[z][:], func=AF.Ln,
                                                                                 scale=oml[:, h:h + 1], bias=lb[:, h:h + 1]),
                                         reads=[K_('sgm'), 'oml', 'lb'], writes=[K_('lf')])
                                    P.op('dve', lambda e, z=z, h=h: e.tensor_scalar(kk[z][:], sgm[z][:], noml[:, h:h + 1], oml[:, h:h + 1],
                                                                                   op0=ALU.mult, op1=ALU.add),
                                         reads=[K_('sgm'), 'oml', 'noml'], writes=[K_('kk')])
                                    P.op('dve', lambda e, z=z: e.tensor_tensor_scan(cum[z][:], m32[:], lf[z][:], 0.0, op0=ALU.mult, op1=ALU.add),
                                         reads=[K_('lf'), 'm32'], writes=[K_('cum')])
                                    P.op('act', lambda e, z=z: e.activation(out=ec[z][:], in_=cum[z][:], func=AF.Exp),
                                         reads=[K_('cum')], writes=[K_('ec')])
                                    P.op('act', lambda e, z=z: e.activation(out=ei[z][:], in_=cum[z][:], func=AF.Exp, scale=-1.0),
                                         reads=[K_('cum')], writes=[K_('ei')])
                                    c3 = lambda a: a[:].rearrange("p (a b) -> p a b", b=32)
                                    P.op('dve', lambda e, z=z: e.tensor_tensor(out=c3(ee[z]), in0=c3(cum[z])[:, :, 31:32].broadcast_to([128, 16, 32]),
                                                                               in1=c3(cum[z]), op=ALU.subtract),
                                         reads=[K_('cum')], writes=[K_('ee')])
                                    P.op('act', lambda e, z=z: e.activation(out=ee[z][:], in_=ee[z][:], func=AF.Exp),
                                         reads=[K_('ee')], writes=[K_('ee')])
                                    P.op('dve', lambda e, z=z, bq=bq: e.tensor_tensor(out=qd[z][:], in0=ps[bq][:], in1=ec[z][:], op=ALU.mult),
                                         reads=[PK(bq), K_('ec')], writes=[K_('qd')])
                                    P.op('dve', lambda e, z=z: e.tensor_tensor(out=ki[z][:], in0=kk[z][:], in1=ei[z][:], op=ALU.mult),
                                         reads=[K_('kk'), K_('ei')], writes=[K_('ki')])
                                    P.op('dve', lambda e, z=z: e.tensor_tensor(out=ke[z][:], in0=kk[z][:], in1=ee[z][:], op=ALU.mult),
                                         reads=[K_('kk'), K_('ee')], writes=[K_('ke')])
                                    P.op('act', lambda e, z=z: e.copy(dcs[z][:], c3(ec[z])[:, :, 31]), reads=[K_('ec')], writes=[K_('dcs')])
                                    tt = t0 + n * 512
                                    P.op('dve', lambda e, z=z, h=h: e.tensor_tensor_scan(cB[z][:], o512[:], lf[z][:], cBl[:, h:h + 1],
                                                                                       op0=ALU.mult, op1=ALU.add),
                                         reads=[K_('lf'), 'o512', 'cBl'], writes=[K_('cB')])
                                    P.op('act', lambda e, z=z: e.activation(out=eB[z][:], in_=cB[z][:], func=AF.Exp),
                                         reads=[K_('cB')], writes=[K_('eB')])
                                    P.op('dve', lambda e, z=z, h=h: e.tensor_copy(cBl[:, h:h + 1], cB[z][:, 511:512]),
                                         reads=[K_('cB')], writes=['cBl'])
                                    P.op('dve', lambda e, z=z, bq=bq: e.tensor_tensor(out=qtb[z][:], in0=ps[bq][:], in1=eB[z][:], op=ALU.mult),
                                         reads=[PK(bq), K_('eB')], writes=[K_('qtb')])
                                    P.op('sp', lambda e, z=z, h=h, tt=tt: e.dma_start(out=qt_d[h * 128:(h + 1) * 128, tt:tt + 512], in_=qtb[z][:]),
                                         reads=[K_('qtb')], dma='oqt%d' % z)
                                    for buf, dd, nm in ((qd, qd_d, 'qd'), (ki, ki_d, 'ki'), (ke, ke_d, 'ke')):
                                        P.op('sp', lambda e, z=z, buf=buf, dd=dd, h=h, tt=tt: e.dma_start(
                                            out=dd[h * 128:(h + 1) * 128, tt:tt + 512], in_=buf[z][:]),
                                            reads=[K_(nm)], dma='o' + nm + '%d' % z)
                                    P.op('sp', lambda e, z=z, h=h, tt=tt: e.dma_start(
                                        out=dec_d[h * 128:(h + 1) * 128, tt // 32:tt // 32 + 16], in_=dcs[z][:]),
                                        reads=[K_('dcs')], dma='odc%d' % z)
                            P.emit()
                        lin_tok(uT, hgrn_w_in, 4096, 4, SEG, v_d, t0, 'uT')
                        with ExitStack() as st2:
                            sgo = [sb(st2, "sgo%d" % i, [128, 512], BF16) for i in range(2)]

                            def epi(j, n, b):
                                z = (j * 2 + n) % 2
                                P.op('act', lambda e: e.activation(out=sgo[z][:], in_=ps[b][:], func=AF.Silu),
                                     reads=[PK(b)], writes=[('sgo', z)])
                                P.op('sp', lambda e: e.dma_start(out=sg_d[j * 128:(j + 1) * 128, t0 + n * 512:t0 + (n + 1) * 512], in_=sgo[z][:]),
                                     reads=[('sgo', z)], dma='osg%d' % z)
                            lin_feat(uT, hgrn_w_in, 6144, 16, SEG, epi, 'uT')
                gla_chunks(16, 1, 1, 32, qd_d, ki_d, ke_d, v_d, o_d, send_d, dec_d, None, slots=2)
                exchange(send_d, recv_d, D, D)
                gla_post(16, 1, 1, qt_d, recv_d, D, o_d, sg_d, y_d, hng)
                out_proj(l, y_d, hgrn_w_out, 16)


        def ret_layer(l):
            dram = lambda n, sh, dt: nc.dram_tensor(n, sh, dt, kind=("ExternalOutput" if DEBUG else "Internal")).ap()
            qd_d = dram("r_qd", [D, NT], BF16)
            ki_d = dram("r_ki", [D, NT], BF16)
            ke_d = dram("r_ke", [D, NT], BF16)
            v_d = dram("r_v", [NT, 4096], BF16)
            sg_d = dram("r_sg", [4096, NT], BF16)
            y_d = dram("r_y", [4096, NT], BF16)
            qt_d = dram("r_qt", [D, NT], BF16)
            o_d = dram("r_o", [4096, NT], F32)
            send_d = dram("r_send", [D, 512], BF16)
            recv_d = dram("r_recv", [2 * D, 512], BF16)
            TWO_PI = 2.0 * math.pi
            PI_ = 3.1415925
            with ExitStack() as st0:
                invf = sb(st0, "invf", [128, 1], F32)
                gtab = sb(st0, "gtab", [128, 8, 3, 128], F32)
                P.op('sp', lambda e: e.dma_start(out=invf[:], in_=invf_d), writes=['invf'], dma='c0')
                P.op('sp', lambda e: e.dma_start(out=gtab[:].rearrange("p a b c -> p (a b c)"), in_=gtab_d.broadcast_to([128, 8 * 3 * 128])),
                     writes=['gtab'], dma='c1')
                P.emit()
                for seg in range(NT // SEG):
                    t0 = seg * SEG
                    with ExitStack() as st:
                        uT = sb(st, "uT", [128, KC, SEG], BF16)
                        normmod(st, t0, SEG, lambda kc: modA[:, (l * 2) * 16 + kc:(l * 2) * 16 + kc + 1],
                                lambda kc: ada_col(l, 0, kc), uT, 'uT')
                        with ExitStack() as st2:
                            cs = sb(st2, "cs", [128, SEG], F32)
                            sn = sb(st2, "sn", [128, SEG], F32)
                            posi = sb(st2, "posi", [128, SEG], I32)
                            ang = sb(st2, "ang", [128, SEG], F32)
                            w1 = sb(st2, "w1", [128, SEG], F32)
                            wi = sb(st2, "wi", [128, SEG], I32)
                            P.op('sp', lambda e: e.dma_start(out=posi[:], in_=pos[0:1, t0:t0 + SEG].broadcast_to([128, SEG])),
                                 writes=['posi'], dma='c0')
                            P.op('dve', lambda e: e.tensor_copy(ang[:], posi[:]), reads=['posi'], writes=['ang'])
                            P.op('dve', lambda e: e.tensor_scalar(ang[:], ang[:], invf[:, 0:1], None, op0=ALU.mult),
                                 reads=['ang', 'invf'], writes=['ang'])
                            for off, dst, dk_ in ((0.0, sn, 'sn'), (0.5 * math.pi, cs, 'cs')):
                                P.op('dve', lambda e, off=off: e.tensor_scalar(w1[:], ang[:], off, 1.0 / TWO_PI, op0=ALU.add, op1=ALU.mult),
                                     reads=['ang'], writes=['w1'])
                                P.op('dve', lambda e: e.tensor_copy(wi[:], w1[:]), reads=['w1'], writes=['wi'])
                                P.op('dve', lambda e: e.tensor_copy(w1[:], wi[:]), reads=['wi'], writes=['w1'])
                                P.op('dve', lambda e: e.tensor_scalar(w1[:], w1[:], -TWO_PI, None, op0=ALU.mult), reads=['w1'], writes=['w1'])
                                P.op('dve', lambda e, off=off: e.scalar_tensor_tensor(out=w1[:], in0=ang[:], scalar=off, in1=w1[:],
                                                                                      op0=ALU.add, op1=ALU.add),
                                     reads=['w1', 'ang'], writes=['w1'])
                                P.op('dve', lambda e: e.tensor_scalar(w1[:], w1[:], -PI_, PI_, op0=ALU.max, op1=ALU.min),
                                     reads=['w1'], writes=['w1'])
                                P.op('act', lambda e, dst=dst: e.activation(out=dst[:], in_=w1[:], func=AF.Sin), reads=['w1'], writes=[dk_])
                            F = lambda nm, n_: [sb(st2, nm + "%d" % i, [128, 512], F32) for i in range(n_)]
                            Bf = lambda nm, n_: [sb(st2, nm + "%d" % i, [128, 512], BF16) for i in range(n_)]
                            ta, tb, tc_, td = F("ta", 2), F("tb", 2), F("tc", 2), F("td", 2)
                            r1, r2 = F("r1", 2), F("r2", 2)
                            ob = Bf("ob", 8)
                            g2 = F("g2", 2)
                            wr = [sb(st2, "wr%d" % i, [128, KC, 128], BF16) for i in range(8)]
                            cnt = 0
                            oc = 0
                            for h in range(8):
                                hs_ = (h % 2) * 4
                                cols = [h * 256, h * 256 + 128, 2048 + h * 256, 2048 + h * 256 + 128]
                                for i, c0 in enumerate(cols):
                                    src = ret_w_in[:, c0:c0 + 128].rearrange("(kc p) m -> p kc m", p=128)
                                    P.op('pool', lambda e, src=src, i=i, hs_=hs_: e.dma_start(out=wr[hs_ + i][:], in_=src),
                                         writes=[('wr', hs_ + i)], dma='wr%d' % (hs_ + i))
                                for n in range(SEG // 512):
                                    csn = cs[:, n * 512:(n + 1) * 512]
                                    snn = sn[:, n * 512:(n + 1) * 512]
                                    for qk in range(2):
                                        b1, b2 = (cnt * 2) % 8, (cnt * 2) % 8 + 1
                                        z = cnt % 2
                                        cnt += 1
                                        for bb, wi_ in ((b1, hs_ + qk * 2), (b2, hs_ + qk * 2 + 1)):
                                            for kc in range(KC):
                                                P.op('pe', lambda e, bb=bb, wi_=wi_, kc=kc, n=n: e.matmul(
                                                    ps[bb][:], wr[wi_][:, kc, :], uT[:, kc, n * 512:(n + 1) * 512],
                                                    start=(kc == 0), stop=(kc == KC - 1)),
                                                    reads=[('wr', wi_), ('uT', kc)], writes=[PK(bb)])
                                        for dst, bb, tr, nm in ((ta, b1, csn, 'ta'), (tb, b2, snn, 'tb'), (tc_, b1, snn, 'tc'), (td, b2, csn, 'td')):
                                            P.op('dve', lambda e, dst=dst, bb=bb, tr=tr, z=z: e.tensor_tensor(out=dst[z][:], in0=ps[bb][:], in1=tr, op=ALU.mult),
                                                 reads=[PK(bb), 'cs', 'sn'], writes=[(nm, z)])
                                        P.op('pool', lambda e, z=z: e.tensor_tensor(out=r1[z][:], in0=ta[z][:], in1=tb[z][:], op=ALU.subtract),
                                             reads=[('ta', z), ('tb', z)], writes=[('r1', z)])
                                        P.op('pool', lambda e, z=z: e.tensor_tensor(out=r2[z][:], in0=tc_[z][:], in1=td[z][:], op=ALU.add),
                                             reads=[('tc', z), ('td', z)], writes=[('r2', z)])
                                        tt = t0 + n * 512
                                        outs = [(0, qd_d)] if qk == 0 else [(1, ki_d), (2, ke_d)]
                                        if qk == 0:
                                            P.op('sp', lambda e, z=z, h=h, tt=tt: e.dma_start(
                                                out=g2[z][:], in_=gq2_d[h:h + 1, tt:tt + 512].broadcast_to([128, 512])),
                                                writes=[('g2', z)], dma='g2%d' % z)
                                            for half, rr, rn in ((0, r1, 'r1'), (1, r2, 'r2')):
                                                o_ = oc % 8
                                                oc += 1
                                                P.op('pool', lambda e, o_=o_, rr=rr, z=z: e.tensor_tensor(
                                                    out=ob[o_][:], in0=rr[z][:], in1=g2[z][:], op=ALU.mult),
                                                    reads=[(rn, z), ('g2', z)], writes=[('ob', o_)])
                                                r0 = h * 256 + half * 128
                                                P.op('sp', lambda e, o_=o_, r0=r0, tt=tt: e.dma_start(
                                                    out=qt_d[r0:r0 + 128, tt:tt + 512], in_=ob[o_][:]), reads=[('ob', o_)], dma='ob%d' % o_)
                                        for gi, dd in outs:
                                            for half, rr, rn in ((0, r1, 'r1'), (1, r2, 'r2')):
                                                o_ = oc % 8
                                                oc += 1
                                                gv = gtab[:, h, gi, :]
                                                P.op('pool', lambda e, o_=o_, rr=rr, z=z, gv=gv: e.tensor_tensor(
                                                    out=ob[o_][:].rearrange("p (a b) -> p a b", b=128),
                                                    in0=rr[z][:].rearrange("p (a b) -> p a b", b=128),
                                                    in1=gv.unsqueeze(1).broadcast_to([128, 4, 128]), op=ALU.mult),
                                                    reads=[(rn, z), 'gtab'], writes=[('ob', o_)])
                                                r0 = h * 256 + half * 128
                                                P.op('sp', lambda e, o_=o_, dd=dd, r0=r0, tt=tt: e.dma_start(
                                                    out=dd[r0:r0 + 128, tt:tt + 512], in_=ob[o_][:]), reads=[('ob', o_)], dma='ob%d' % o_)
                            P.emit()
                        lin_tok(uT, ret_w_in, 4096, 8, SEG, v_d, t0, 'uT')
                        with ExitStack() as st2:
                            sgo = [sb(st2, "sgo%d" % i, [128, 512], BF16) for i in range(2)]

                            def epi(j, n, b):
                                z = (j * 2 + n) % 2
                                P.op('act', lambda e: e.activation(out=sgo[z][:], in_=ps[b][:], func=AF.Silu),
                                     reads=[PK(b)], writes=[('sgo', z)])
                                P.op('sp', lambda e: e.dma_start(out=sg_d[j * 128:(j + 1) * 128, t0 + n * 512:t0 + (n + 1) * 512], in_=sgo[z][:]),
                                     reads=[('sgo', z)], dma='osg%d' % z)
                            lin_feat(uT, ret_w_in, 8192, 32, SEG, epi, 'uT')
            gam = [1.0 - 2.0 ** (-5.0 - h) for h in range(8)]
            gla_chunks(8, 2, 4, 128, qd_d, ki_d, ke_d, v_d, o_d, send_d, None, [g_ ** 128 for g_ in gam], slots=1)
            exchange(send_d, recv_d, D, 1024)
            gla_post(8, 2, 4, qt_d, recv_d, 1024, o_d, sg_d, y_d, rng_)
            out_proj(l, y_d, ret_w_out, 32)

        C.P, C.sb, C.ps, C.PK, C.hT, C.hT3 = P, sb, ps, PK, hT, hT3
        C.normmod, C.ada_col, C.modA = normmod, ada_col, modA

        for l in layers:
            if do_mixer:
                if l % 3 == 0:
                    pool_layer(l)
                elif l % 3 == 1:
                    hgrn_layer(l)
                else:
                    ret_layer(l)
            if do_ffn:
                ffn_layer(l)

        with ExitStack() as st:
            FN = min(512, NT)
            for seg in range(NT // FN):
                t0 = seg * FN
                with ExitStack() as st2:
                    yT = sb(st2, "yT", [128, KC, FN], F32)
                    z0 = sb(st2, "z0", [128, 1], F32)
                    P.op('pool', lambda e: e.memset(z0[:], 0.0), writes=['z0'])
                    normmod(st2, t0, FN, lambda kc: fng[:, kc:kc + 1], lambda kc: z0[:], yT, 'yT')
                    ot = [sb(st2, "ot%d" % i, [128, D], F32) for i in range(2)]
                    for ti in range(FN // 128):
                        s = ti % 2
                        for q in range(4):
                            b = (ti * 4 + q) % 8
                            for i in range(4):
                                c = q * 4 + i
                                P.op('pe', lambda e, b=b, i=i, c=c, ti=ti: e.transpose(
                                    ps[b][:, i * 128:(i + 1) * 128], yT[:, c, ti * 128:(ti + 1) * 128], ident[:]),
                                    reads=[('yT', c), 'ident'], writes=[PK(b)])
                            if q % 2 == 0:
                                P.op('dve', lambda e, s=s, b=b, q=q: e.tensor_copy(ot[s][:, q * 512:(q + 1) * 512], ps[b][:]),
                                     reads=[PK(b)], writes=[('ot', s)])
                            else:
                                P.op('act', lambda e, s=s, b=b, q=q: e.copy(ot[s][:, q * 512:(q + 1) * 512], ps[b][:]),
                                     reads=[PK(b)], writes=[('ot', s)])
                        P.op('sp', lambda e, s=s, ti=ti: e.dma_start(out=out[t0 + ti * 128:t0 + (ti + 1) * 128, :], in_=ot[s][:]),
                             reads=[('ot', s)], dma='ot%d' % s)
                    P.emit()
    return nc


_IDENT = np.eye(128, dtype=np.float32)
_INVF = (np.float32(10000.0) ** (-np.arange(128, dtype=np.float32) / np.float32(128))).astype(np.float32).reshape(128, 1)
_lg = np.log(1.0 - 2.0 ** (-5.0 - np.arange(8, dtype=np.float64)))
_t = np.arange(128, dtype=np.float64)
_GTAB = np.stack([np.exp(_lg[:, None] * (_t + 1.0)), np.exp(-_lg[:, None] * (_t + 1.0)) / 16.0,
                  np.exp(_lg[:, None] * (127.0 - _t)) / 16.0], axis=1).astype(np.float32).reshape(1, 8 * 3 * 128)


_GQ2 = np.exp(_lg[:, None] * (np.arange(4096, dtype=np.float64)[None, :] + 1.0)).astype(np.float32)


def make_in_map(inputs, b, t0, NT):
    g = lambda k: np.ascontiguousarray(np.asarray(inputs[k]))
    m = {
        "x": np.ascontiguousarray(g("x")[b, t0:t0 + NT]),
        "c": g("c")[b].reshape(KC, 128),
        "positions": np.ascontiguousarray(g("positions")[b:b + 1, t0:t0 + NT]).astype(np.int32),
        "w_ada": g("w_ada"), "b_ada": g("b_ada"),
        "norm_mix_g": g("norm_mix_g").reshape(DEPTH * KC, 128),
        "norm_ffn_g": g("norm_ffn_g").reshape(DEPTH * KC, 128),
        "pool_w": g("pool_w"), "pool_scale": g("pool_scale").reshape(2 * KC, 128),
        "hgrn_w_in": g("hgrn_w_in")[0], "hgrn_lb_logits": g("hgrn_lb_logits").reshape(DEPTH * KC, 128),
        "hgrn_norm_g": g("hgrn_norm_g").reshape(KC, 128), "hgrn_w_out": g("hgrn_w_out")[0],
        "ret_w_in": g("ret_w_in")[0], "ret_norm_g": g("ret_norm_g").reshape(32, 128), "ret_w_out": g("ret_w_out")[0],
        "ffn_w_in": g("ffn_w_in"), "ffn_w_out": g("ffn_w_out"),
        "final_norm_g": g("final_norm_g").reshape(KC, 128),
        "ident": _IDENT, "invf": _INVF, "gtab": _GTAB,
        "gq2": np.ascontiguousarray(_GQ2[:, :NT]),
        "flag": np.full((128, 1), 1.0 if t0 > 0 else 0.0, np.float32),
        "xh": (np.ascontiguousarray(g("x")[b, t0 - 16:t0]) if t0 > 0 else np.zeros((16, D), np.float32)),
    }
    return m


def kernel(**inputs):
    B, S, _ = inputs["x"].shape
    NT = S // 2
    nc = build_program(NT)
    in_maps = [make_in_map(inputs, c // 2, (c % 2) * NT, NT) for c in range(8)]
    res = run_bass_kernel_spmd(nc, in_maps, core_ids=list(range(8)))
    out = np.empty((B, S, D), np.float32)
    for c in range(8):
        out[c // 2, (c % 2) * NT:(c % 2 + 1) * NT] = np.asarray(res.results[c]["out"])
    return out
```

```python
from contextlib import ExitStack
import math
import numpy as np
import concourse.bass as bass
import concourse.mybir as mybir
from concourse.bass_utils import run_bass_kernel_spmd

F32 = mybir.dt.float32
BF16 = mybir.dt.bfloat16
I32 = mybir.dt.int32
AF = mybir.ActivationFunctionType
ALU = mybir.AluOpType

D = 2048
KC = 16
DEPTH = 4
FH = 5632
FHC = 44
EPS = 1e-6
SEG = 1024
ENGS = ['pe', 'act', 'dve', 'pool', 'sp']
DEBUG = False


class Op:
    __slots__ = ('eng', 'fn', 'deps', 'dma', 'flag', 'event', 'inc')

    def __init__(self, eng, fn, deps, dma, inc=16):
        self.eng, self.fn, self.deps, self.dma, self.inc = eng, fn, deps, dma, inc
        self.flag = False
        self.event = None


class Prog:
    def __init__(self, nc, stack):
        self.nc = nc
        self.stack = stack
        self.eobj = {'pe': nc.tensor, 'act': nc.scalar, 'dve': nc.vector, 'pool': nc.gpsimd, 'sp': nc.sync}
        self.esem = {e: stack.enter_context(nc.semaphore('es_' + e)) for e in ENGS}
        self.ecnt = {e: 0 for e in ENGS}
        self.dsem = {}
        self.dcnt = {}
        self.known = {e: {} for e in ENGS}
        self.nphase = 0
        self.reset()

    def reset(self):
        self.ops = []
        self.lastw = {}
        self.rd = {}

    def op(self, eng, fn, reads=(), writes=(), dma=None, inc=16):
        idx = len(self.ops)
        deps = set()
        for r in reads:
            w = self.lastw.get(r)
            if w is not None:
                deps.add(w)
        for wk in writes:
            w = self.lastw.get(wk)
            if w is not None:
                deps.add(w)
            rr = self.rd.get(wk)
            if rr:
                deps.update(rr.values())
        for r in reads:
            d = self.rd.setdefault(r, {})
            d[(eng, dma)] = idx
        for wk in writes:
            self.lastw[wk] = idx
            self.rd[wk] = {}
        deps.discard(idx)
        if eng == 'pe':
            deps = {d for d in deps if not (self.ops[d].eng == 'pe' and self.ops[d].dma is None)}
        self.ops.append(Op(eng, fn, deps, dma, inc))
        return idx

    def _dsem(self, key):
        if key not in self.dsem:
            self.dsem[key] = self.stack.enter_context(self.nc.semaphore('ds_' + key))
            self.dcnt[key] = 0
        return self.dsem[key]

    def emit(self):
        ops = self.ops
        if not ops:
            return
        for o in ops:
            for d in o.deps:
                ops[d].flag = True
        for o in ops:
            if o.dma is not None:
                s = self._dsem(o.dma)
                self.dcnt[o.dma] += o.inc
                o.event = (o.dma, s, self.dcnt[o.dma], o.inc)
            elif o.flag:
                self.ecnt[o.eng] += 1
                o.event = ('e_' + o.eng, self.esem[o.eng], self.ecnt[o.eng], 1)
        per = {e: [] for e in ENGS}
        for o in ops:
            per[o.eng].append(o)

        def run(e, eng):
            kn = self.known[e]
            final = {}
            for o in per[e]:
                for d in sorted(o.deps):
                    name, s, val, _ = ops[d].event
                    if kn.get(name, 0) < val:
                        eng.wait_ge(s, val)
                        kn[name] = val
                ins = o.fn(eng)
                if o.event is not None:
                    name, s, val, inc = o.event
                    ins.then_inc(s, inc)
                    if o.dma is not None:
                        final[name] = (s, val)
            for name, (s, val) in final.items():
                if kn.get(name, 0) < val:
                    eng.wait_ge(s, val)
                    kn[name] = val

        with self.nc.Block() as block:
            if per['pe']:
                block.tensor(lambda eng: run('pe', eng))
            if per['act']:
                block.scalar(lambda eng: run('act', eng))
            if per['dve']:
                block.vector(lambda eng: run('dve', eng))
            if per['pool']:
                block.gpsimd(lambda eng: run('pool', eng))
            if per['sp']:
                block.sync(lambda eng: run('sp', eng))
        self.nphase += 1
        self.reset()


class Ctx:
    pass


def build_program(NT, layers=(0, 1, 2, 3), do_mixer=True, do_ffn=True, ncores=8):
    nc = bass.Bass("TRN2", target_bir_lowering=False)
    C = Ctx()
    C.nc = nc
    C.NT = NT
    dt_in = lambda n, s, d=F32: nc.dram_tensor(n, s, d, kind="ExternalInput").ap()
    x = dt_in("x", [NT, D])
    cvec = dt_in("c", [KC, 128])
    pos = dt_in("positions", [1, NT], I32)
    w_ada = dt_in("w_ada", [DEPTH // 2, D, 6 * D])
    b_ada = dt_in("b_ada", [DEPTH // 2, 6 * D])
    norm_mix_g = dt_in("norm_mix_g", [DEPTH * KC, 128])
    norm_ffn_g = dt_in("norm_ffn_g", [DEPTH * KC, 128])
    pool_w = dt_in("pool_w", [2, 4, 512, 512])
    pool_scale = dt_in("pool_scale", [2 * KC, 128])
    hgrn_w_in = dt_in("hgrn_w_in", [D, 8192])
    hgrn_lb = dt_in("hgrn_lb_logits", [DEPTH * KC, 128])
    hgrn_norm_g = dt_in("hgrn_norm_g", [KC, 128])
    hgrn_w_out = dt_in("hgrn_w_out", [D, D])
    ret_w_in = dt_in("ret_w_in", [D, 12288])
    ret_norm_g = dt_in("ret_norm_g", [32, 128])
    ret_w_out = dt_in("ret_w_out", [4096, D])
    ffn_w_in = dt_in("ffn_w_in", [DEPTH, D, 2 * FH])
    ffn_w_out = dt_in("ffn_w_out", [DEPTH, FH, D])
    final_g = dt_in("final_norm_g", [KC, 128])
    ident_d = dt_in("ident", [128, 128])
    invf_d = dt_in("invf", [128, 1])
    gtab_d = dt_in("gtab", [1, 8 * 3 * 128])
    gq2_d = dt_in("gq2", [8, NT])
    flag_d = dt_in("flag", [128, 1])
    xh_d = dt_in("xh", [16, D])
    hh = nc.dram_tensor("hh", [D, 16], F32, kind="Internal").ap()
    hh3 = hh.rearrange("(c p) t -> p c t", p=128)
    pairs = [[2 * i, 2 * i + 1] for i in range(ncores // 2)]
    out = nc.dram_tensor("out", [NT, D], F32, kind="ExternalOutput").ap()
    hT = nc.dram_tensor("hT", [D, NT], F32, kind="Internal").ap()
    ada_d = nc.dram_tensor("ada_d", [DEPTH * 96, 128], F32, kind="Internal").ap()
    asend_d = nc.dram_tensor("asend_d", [DEPTH // 2 * 96, 128], F32, kind="Internal").ap()
    hT3 = hT.rearrange("(c p) t -> p c t", p=128)

    with ExitStack() as gs:
        P = Prog(nc, gs)
        _cnt = [0]

        def sb(st, name, shape, dt):
            _cnt[0] += 1
            return st.enter_context(nc.sbuf_tensor("%s_%d" % (name, _cnt[0]), shape, dt))
        ps = [gs.enter_context(nc.psum_tensor("ps%d" % i, [128, 512], F32)) for i in range(8)]
        PK = lambda b: ('ps', b)
        ident = sb(gs, "ident", [128, 128], F32)
        identb = sb(gs, "identb", [128, 128], BF16)
        ones32 = sb(gs, "ones32", [128, 128], F32)
        epsc = sb(gs, "epsc", [128, 1], F32)
        flag = sb(gs, "flag", [128, 1], F32)
        gmix = sb(gs, "gmix", [128, 64], F32)
        gffn = sb(gs, "gffn", [128, 64], F32)
        pscl = sb(gs, "pscl", [128, 32], F32)
        lbl = sb(gs, "lbl", [128, 64], F32)
        hng = sb(gs, "hng", [128, 16], F32)
        rng_ = sb(gs, "rng", [128, 32], F32)
        fng = sb(gs, "fng", [128, 16], F32)
        adac = sb(gs, "adac", [128, DEPTH * 96], F32)
        modA = sb(gs, "modA", [128, DEPTH * 2 * 16], F32)

        with ExitStack() as st:
            rows = sb(st, "rows", [128, 128], F32)
            P.op('sp', lambda e: e.dma_start(out=ident[:], in_=ident_d), writes=['ident'], dma='c0')
            P.op('pool', lambda e: e.memset(ones32[:], 1.0), writes=['ones32'])
            P.op('pool', lambda e: e.memset(epsc[:], EPS), writes=['epsc'])
            P.op('sp', lambda e: e.dma_start(out=flag[:], in_=flag_d), writes=['flag'], dma='c4')
            P.op('dve', lambda e: e.tensor_copy(identb[:], ident[:]), reads=['ident'], writes=['identb'])
            cc = sb(st, "cc", [128, 16], F32)
            scb = sb(st, "scb", [128, 16], BF16)

            def to_cols(src_rows_ap, R, dst_ap, key):
                P.op('sp', lambda e: e.dma_start(out=rows[0:R, :], in_=src_rows_ap), writes=['rows'], dma='c1')
                P.op('pe', lambda e: e.transpose(ps[0][:, 0:R], rows[0:R, :], ident[0:R, 0:R]),
                     reads=['rows', 'ident'], writes=[PK(0)])
                P.op('dve', lambda e: e.tensor_copy(dst_ap, ps[0][:, 0:R]), reads=[PK(0)], writes=[key])

            to_cols(norm_mix_g, 64, gmix[:], 'gmix')
            to_cols(norm_ffn_g, 64, gffn[:], 'gffn')
            to_cols(pool_scale, 32, pscl[:], 'pscl')
            to_cols(hgrn_lb, 64, lbl[:], 'lbl')
            to_cols(hgrn_norm_g, 16, hng[:], 'hng')
            to_cols(ret_norm_g, 32, rng_[:], 'rng')
            to_cols(final_g, 16, fng[:], 'fng')
            to_cols(cvec, 16, cc[:], 'cc')
            P.op('act', lambda e: e.activation(out=scb[:], in_=cc[:], func=AF.Silu), reads=['cc'], writes=['scb'])
            arow = sb(st, "arow", [1, 6 * D], F32)
            brow = sb(st, "brow", [1, 6 * D], F32)
            wab = [sb(st, "wab%d" % i, [128, KC, 512], BF16) for i in range(2)]
            nb = 0
            for l in range(DEPTH // 2):
                P.op('sp', lambda e, l=l: e.dma_start(out=brow[:], in_=b_ada[l:l + 1, :]), writes=['brow'], dma='c2')
                for j in range(24):
                    s = nb % 2
                    src = w_ada[l, :, j * 512:(j + 1) * 512].rearrange("(kc p) m -> p kc m", p=128)
                    P.op('pool', lambda e, s=s, src=src: e.dma_start(out=wab[s][:], in_=src),
                         writes=[('wab', s)], dma='wa%d' % s)
                    b = nb % 4
                    for kc in range(KC):
                        P.op('pe', lambda e, s=s, b=b, kc=kc: e.matmul(ps[b][0:1, :], scb[:, kc:kc + 1], wab[s][:, kc, :],
                                                                      start=(kc == 0), stop=(kc == KC - 1)),
                             reads=[('wab', s), 'scb'], writes=[PK(b)])
                    P.op('dve', lambda e, b=b, j=j: e.tensor_tensor(out=arow[:, j * 512:(j + 1) * 512], in0=ps[b][0:1, :],
                                                                    in1=brow[:, j * 512:(j + 1) * 512], op=ALU.add),
                         reads=[PK(b), 'brow'], writes=['arow'])
                    nb += 1
                P.op('sp', lambda e, l=l: e.dma_start(out=asend_d[l * 96:(l + 1) * 96, :].rearrange("(o r) c -> o (r c)", o=1),
                                                      in_=arow[:]), reads=['arow'], writes=['ada_d'], dma='c3')
            P.emit()
            P.op('pool', lambda e: e.collective_compute("AllGather", ALU.bypass, replica_groups=pairs, ins=[asend_d], outs=[ada_d]),
                 dma='cc', inc=1)
            P.emit()
            for i in range(3):
                to_cols(ada_d[i * 128:(i + 1) * 128, :], 128, adac[:, i * 128:(i + 1) * 128], 'adac')
            for l in range(DEPTH):
                for sub, gt in ((0, gmix), (1, gffn)):
                    sc = adac[:, l * 96 + (1 + 3 * sub) * 16: l * 96 + (2 + 3 * sub) * 16]
                    dst = modA[:, (l * 2 + sub) * 16:(l * 2 + sub + 1) * 16]
                    P.op('dve', lambda e, sc=sc, dst=dst, gt=gt, l=l: e.scalar_tensor_tensor(
                        out=dst, in0=sc, scalar=1.0, in1=gt[:, l * 16:(l + 1) * 16], op0=ALU.add, op1=ALU.mult),
                        reads=['adac', 'gmix', 'gffn'], writes=['modA'])
            P.emit()

        ada_col = lambda l, k, c: adac[:, l * 96 + k * 16 + c: l * 96 + k * 16 + c + 1]

        with ExitStack() as st:
            xt = [sb(st, "xt%d" % i, [128, D], F32) for i in range(2)]
            stg = [sb(st, "stg%d" % i, [128, KC, 128], F32) for i in range(2)]
            for ti in range(NT // 128):
                s = ti % 2
                P.op('sp', lambda e, s=s, ti=ti: e.dma_start(out=xt[s][:], in_=x[ti * 128:(ti + 1) * 128, :]),
                     writes=[('xt', s)], dma='xt%d' % s)
                for q in range(4):
                    b = (ti * 4 + q) % 8
                    for i in range(4):
                        c = q * 4 + i
                        P.op('pe', lambda e, s=s, b=b, i=i, c=c: e.transpose(ps[b][:, i * 128:(i + 1) * 128],
                                                                             xt[s][:, c * 128:(c + 1) * 128], ident[:]),
                             reads=[('xt', s), 'ident'], writes=[PK(b)])
                    eng = 'dve' if q % 2 == 0 else 'act'
                    if eng == 'dve':
                        P.op('dve', lambda e, s=s, b=b, q=q: e.tensor_copy(
                            stg[s][:, q * 4:(q + 1) * 4, :].rearrange("p a b -> p (a b)"), ps[b][:]),
                            reads=[PK(b)], writes=[('stg', s)])
                    else:
                        P.op('act', lambda e, s=s, b=b, q=q: e.copy(
                            stg[s][:, q * 4:(q + 1) * 4, :].rearrange("p a b -> p (a b)"), ps[b][:]),
                            reads=[PK(b)], writes=[('stg', s)])
                P.op('sp', lambda e, s=s, ti=ti: e.dma_start(out=hT3[:, :, ti * 128:(ti + 1) * 128], in_=stg[s][:]),
                     reads=[('stg', s)], dma='st%d' % s)
            P.emit()
            xh = sb(st, "xh", [16, D], F32)
            sth = sb(st, "sth", [128, KC, 16], F32)
            P.op('sp', lambda e: e.dma_start(out=xh[:], in_=xh_d), writes=['xh'], dma='c0')
            for c in range(KC):
                P.op('pe', lambda e, c=c: e.transpose(ps[c % 8][:, 0:16], xh[0:16, c * 128:(c + 1) * 128], ident[0:16, 0:16]),
                     reads=['xh', 'ident'], writes=[PK(c % 8)])
                P.op('dve', lambda e, c=c: e.tensor_copy(sth[:, c, :], ps[c % 8][:, 0:16]), reads=[PK(c % 8)], writes=['sth'])
            P.op('sp', lambda e: e.dma_start(out=hh3, in_=sth[:]), reads=['sth'], dma='c1')
            P.emit()

        def normmod(st_out, t0, N, Acol, Bcol, xT, xkey, halo=0, src=None):
            with ExitStack() as st:
                hall = sb(st, "hall", [128, KC, N], F32)
                sq = [sb(st, "sq%d" % i, [128, N], F32) for i in range(2)]
                rstd = sb(st, "rstd", [128, N], F32)
                tmp = [sb(st, "tmp%d" % i, [128, N], F32) for i in range(2)]
                nt = (N + 511) // 512
                for kc in range(KC):
                    sap = hT[kc * 128:(kc + 1) * 128, t0:t0 + N] if src is None else src(kc)
                    P.op('sp', lambda e, kc=kc, sap=sap: e.dma_start(out=hall[:, kc, :], in_=sap),
                         writes=[('hall', kc)], dma='ha%d' % (kc % 4))
                    s = kc % 2
                    P.op('act', lambda e, kc=kc, s=s: e.activation(out=sq[s][:], in_=hall[:, kc, :], func=AF.Square),
                         reads=[('hall', kc)], writes=[('sq', s)])
                    for n in range(nt):
                        w = min(512, N - n * 512)
                        P.op('pe', lambda e, kc=kc, s=s, n=n, w=w: e.matmul(ps[n][:, 0:w], ones32[:], sq[s][:, n * 512:n * 512 + w],
                                                                            start=(kc == 0), stop=(kc == KC - 1)),
                             reads=[('sq', s), 'ones32'], writes=[PK(n)])
                for n in range(nt):
                    w = min(512, N - n * 512)
                    P.op('act', lambda e, n=n, w=w: e.activation(out=rstd[:, n * 512:n * 512 + w], in_=ps[n][:, 0:w], func=AF.Ln,
                                                                 scale=1.0 / D, bias=epsc[:]),
                         reads=[PK(n), 'epsc'], writes=['rstd'])
                P.op('act', lambda e: e.activation(out=rstd[:], in_=rstd[:], func=AF.Exp, scale=-0.5),
                     reads=['rstd'], writes=['rstd'])
                for kc in range(KC):
                    s = kc % 2
                    P.op('dve', lambda e, kc=kc, s=s: e.scalar_tensor_tensor(out=tmp[s][:], in0=hall[:, kc, :], scalar=Acol(kc),
                                                                             in1=rstd[:], op0=ALU.mult, op1=ALU.mult),
                         reads=[('hall', kc), 'rstd', 'modA'], writes=[('tmp', s)])
                    P.op('act', lambda e, kc=kc, s=s: e.activation(out=xT[:, kc, halo:halo + N], in_=tmp[s][:], func=AF.Identity,
                                                                   bias=Bcol(kc), scale=1.0),
                         reads=[('tmp', s), 'adac'], writes=[(xkey, kc)])
                P.emit()

        def ffn_layer(l):
            for seg in range(NT // SEG):
                t0 = seg * SEG
                with ExitStack() as st:
                    xT = sb(st, "xT", [128, KC, SEG], BF16)
                    normmod(st, t0, SEG, lambda kc: modA[:, (l * 2 + 1) * 16 + kc:(l * 2 + 1) * 16 + kc + 1],
                            lambda kc: ada_col(l, 3, kc), xT, 'xT')
                    hid = sb(st, "hid", [128, FHC, SEG], BF16)
                    wg = [sb(st, "wg%d" % i, [128, KC, 128], BF16) for i in range(3)]
                    wu = [sb(st, "wu%d" % i, [128, KC, 128], BF16) for i in range(3)]
                    sg = [sb(st, "sg%d" % i, [128, 512], F32) for i in range(2)]
                    wo = [sb(st, "wo%d" % i, [128, FHC, 128], BF16) for i in range(2)]
                    hin = [sb(st, "hin%d" % i, [128, SEG], F32) for i in range(2)]
                    hout = [sb(st, "hout%d" % i, [128, SEG], F32) for i in range(2)]
                    NTT = SEG // 512
                    w_in_l = ffn_w_in[l]
                    w_out_l = ffn_w_out[l]

                    def load_in(j):
                        s = j % 3
                        srcg = w_in_l[:, j * 128:(j + 1) * 128].rearrange("(kc p) m -> p kc m", p=128)
                        srcu = w_in_l[:, FH + j * 128:FH + (j + 1) * 128].rearrange("(kc p) m -> p kc m", p=128)
                        P.op('pool', lambda e: e.dma_start(out=wg[s][:], in_=srcg), writes=[('wg', s)], dma='wg%d' % s)
                        P.op('pool', lambda e: e.dma_start(out=wu[s][:], in_=srcu), writes=[('wu', s)], dma='wu%d' % s)

                    def load_out(m):
                        s = m % 2
                        src = w_out_l[:, m * 128:(m + 1) * 128].rearrange("(kc p) m -> p kc m", p=128)
                        P.op('pool', lambda e: e.dma_start(out=wo[s][:], in_=src), writes=[('wo', s)], dma='wo%d' % s)

                    load_in(0)
                    load_in(1)
                    cnt = 0
                    for j in range(FHC):
                        if j + 2 < FHC:
                            load_in(j + 2)
                        elif j + 2 == FHC:
                            load_out(0)
                        elif j + 2 == FHC + 1:
                            load_out(1)
                        s = j % 3
                        for n in range(NTT):
                            bg = (cnt * 2) % 8
                            bu = bg + 1
                            cnt += 1
                            for kc in range(KC):
                                P.op('pe', lambda e, s=s, bg=bg, kc=kc, n=n: e.matmul(
                                    ps[bg][:], wg[s][:, kc, :], xT[:, kc, n * 512:(n + 1) * 512], start=(kc == 0), stop=(kc == KC - 1)),
                                    reads=[('wg', s), ('xT', kc)], writes=[PK(bg)])
                            for kc in range(KC):
                                P.op('pe', lambda e, s=s, bu=bu, kc=kc, n=n: e.matmul(
                                    ps[bu][:], wu[s][:, kc, :], xT[:, kc, n * 512:(n + 1) * 512], start=(kc == 0), stop=(kc == KC - 1)),
                                    reads=[('wu', s), ('xT', kc)], writes=[PK(bu)])
                            ss = cnt % 2
                            P.op('act', lambda e, ss=ss, bg=bg: e.activation(out=sg[ss][:], in_=ps[bg][:], func=AF.Silu),
                                 reads=[PK(bg)], writes=[('sg', ss)])
                            P.op('dve', lambda e, ss=ss, bu=bu, j=j, n=n: e.tensor_tensor(
                                out=hid[:, j, n * 512:(n + 1) * 512], in0=sg[ss][:], in1=ps[bu][:], op=ALU.mult),
                                reads=[('sg', ss), PK(bu)], writes=[('hid', j)])
                    for m in range(KC):
                        if m >= 1 and m + 1 < KC:
                            load_out(m + 1)
                        s = m % 2
                        P.op('sp', lambda e, s=s, m=m: e.dma_start(out=hin[s][:], in_=hT[m * 128:(m + 1) * 128, t0:t0 + SEG]),
                             writes=[('hin', s)], dma='hin%d' % s)
                        for n in range(NTT):
                            b = cnt % 8
                            cnt += 1
                            for kc in range(FHC):
                                P.op('pe', lambda e, s=s, b=b, kc=kc, n=n: e.matmul(
                                    ps[b][:], wo[s][:, kc, :], hid[:, kc, n * 512:(n + 1) * 512], start=(kc == 0), stop=(kc == FHC - 1)),
                                    reads=[('wo', s), ('hid', kc)], writes=[PK(b)])
                            P.op('dve', lambda e, s=s, b=b, m=m, n=n: e.scalar_tensor_tensor(
                                out=hout[s][:, n * 512:(n + 1) * 512], in0=ps[b][:], scalar=ada_col(l, 5, m),
                                in1=hin[s][:, n * 512:(n + 1) * 512], op0=ALU.mult, op1=ALU.add),
                                reads=[PK(b), ('hin', s), 'adac'], writes=[('hout', s)])
                        P.op('sp', lambda e, s=s, m=m: e.dma_start(out=hT[m * 128:(m + 1) * 128, t0:t0 + SEG], in_=hout[s][:]),
                             reads=[('hout', s)], dma='hout%d' % s)
                    P.emit()


        inv16 = sb(gs, "inv16", [128, 4, 16], F32)
        for g in range(4):
            w = 2 ** (g + 1)
            P.op('pool', lambda e, g=g, w=w: e.memset(inv16[:, g, :], 1.0 / w), writes=['inv16'])
            for t in range(w - 1):
                P.op('pool', lambda e, g=g, t=t: e.memset(inv16[:, g, t:t + 1], 1.0 / (t + 1)), writes=['inv16'])
        P.emit()

        def pool_layer(l):
            j = l // 3
            hsrc = hh
            if l > 0:
                hsend = nc.dram_tensor("hsend%d" % l, [D, 16], F32, kind="Internal").ap()
                hrecv = nc.dram_tensor("hrecv%d" % l, [2 * D, 16], F32, kind="Internal").ap()
                P.op('sp', lambda e: e.dma_start(out=hsend, in_=hT[:, NT - 16:NT]), dma='c0')
                P.emit()
                exchange(hsend, hrecv, D, D)
                hsrc = hrecv
            with ExitStack() as st0:
                gp = sb(st0, "gp", [128, 16], F32)
                P.op('dve', lambda e: e.tensor_tensor(out=gp[:], in0=adac[:, l * 96 + 32:l * 96 + 48],
                                                     in1=pscl[:, j * 16:(j + 1) * 16], op=ALU.mult),
                     reads=['adac', 'pscl'], writes=['gp'])
                halo_t = sb(st0, "halo_t", [128, KC, 16], F32)
                inve = sb(st0, "inve", [128, 4, 16], F32)
                for g in range(4):
                    P.op('dve', lambda e, g=g: e.tensor_scalar(inve[:, g, :], inv16[:, g, :], -1.0, 1.0 / 2 ** (g + 1), op0=ALU.mult, op1=ALU.add),
                         reads=['inv16'], writes=['inve'])
                    P.op('dve', lambda e, g=g: e.scalar_tensor_tensor(out=inve[:, g, :], in0=inve[:, g, :], scalar=flag[:, 0:1],
                                                                      in1=inv16[:, g, :], op0=ALU.mult, op1=ALU.add),
                         reads=['inv16', 'inve', 'flag'], writes=['inve'])
                wp = sb(st0, "wp", [128, 4, 4, 512], BF16)
                for g in range(4):
                    P.op('pool', lambda e, g=g: e.dma_start(out=wp[:, g], in_=pool_w[j, g].rearrange("(kc p) m -> p kc m", p=128)),
                         writes=[('wp', g)], dma='wp')
                P.emit()
                for seg in range(NT // SEG):
                    t0 = seg * SEG
                    N = SEG
                    with ExitStack() as st:
                        uP = sb(st, "uP", [128, KC, 16 + N], F32)
                        Acol = lambda kc: modA[:, (l * 2) * 16 + kc:(l * 2) * 16 + kc + 1]
                        Bcol = lambda kc: ada_col(l, 0, kc)
                        if t0 == 0:
                            uh = sb(st, "uh", [128, KC, 16], F32)
                            normmod(st, 0, 16, Acol, Bcol, uh, 'uh', halo=0, src=lambda kc: hsrc[kc * 128:(kc + 1) * 128, :])
                            P.op('dve', lambda e: e.tensor_scalar(uP[:, :, 0:16], uh[:], flag[:, 0:1], None, op0=ALU.mult),
                                 reads=['flag'], writes=[('uP', kc) for kc in range(KC)])
                        else:
                            P.op('pool', lambda e: e.tensor_copy(uP[:, :, 0:16], halo_t[:]), reads=['halo_t'],
                                 writes=[('uP', kc) for kc in range(KC)])
                        normmod(st, t0, N, Acol, Bcol, uP, 'uP', halo=16)
                        P.op('pool', lambda e: e.tensor_copy(halo_t[:], uP[:, :, N:N + 16]), reads=[('uP', kc) for kc in range(KC)],
                             writes=['halo_t'])
                        tA = sb(st, "tA", [128, 16 + N], F32)
                        tB = sb(st, "tB", [128, 16 + N], F32)
                        pT = sb(st, "pT", [128, KC, N], BF16)
                        hin = [sb(st, "hin%d" % i, [128, N], F32) for i in range(2)]
                        hout = [sb(st, "hout%d" % i, [128, N], F32) for i in range(2)]
                        E = 16 + N
                        for c in range(KC):
                            g = c // 4
                            w = 2 ** (g + 1)
                            u = uP[:, c, :]
                            P.op('dve', lambda e, u=u: e.tensor_tensor(out=tA[:, 1:E], in0=u[:, 1:E], in1=u[:, 0:E - 1], op=ALU.add),
                                 reads=[('uP', c)], writes=['tA'])
                            cur, curk = tA, 'tA'
                            if g >= 1:
                                P.op('dve', lambda e: e.tensor_tensor(out=tB[:, 3:E], in0=tA[:, 3:E], in1=tA[:, 1:E - 2], op=ALU.add),
                                     reads=['tA'], writes=['tB'])
                                cur, curk = tB, 'tB'
                            if g >= 2:
                                P.op('dve', lambda e: e.tensor_tensor(out=tA[:, 7:E], in0=tB[:, 7:E], in1=tB[:, 3:E - 4], op=ALU.add),
                                     reads=['tB'], writes=['tA'])
                                cur, curk = tA, 'tA'
                            if g >= 3:
                                P.op('dve', lambda e: e.tensor_tensor(out=tB[:, 15:E], in0=tA[:, 15:E], in1=tA[:, 7:E - 8], op=ALU.add),
                                     reads=['tA'], writes=['tB'])
                                cur, curk = tB, 'tB'
                            lo = 16 if t0 == 0 else 0
                            P.op('dve', lambda e, cur=cur, u=u, c=c, w=w, lo=lo: e.scalar_tensor_tensor(
                                out=pT[:, c, lo:N], in0=cur[:, 16 + lo:E], scalar=1.0 / w, in1=u[:, 16 + lo:E],
                                op0=ALU.mult, op1=ALU.subtract),
                                reads=[curk, ('uP', c)], writes=[('pT', c)])
                            if t0 == 0:
                                P.op('dve', lambda e, cur=cur, g=g: e.tensor_tensor(out=cur[:, 16:32], in0=cur[:, 16:32],
                                                                                    in1=inve[:, g, :], op=ALU.mult),
                                     reads=[curk, 'inve', ('pT', c)], writes=[curk])
                                P.op('dve', lambda e, cur=cur, u=u, c=c: e.tensor_tensor(out=pT[:, c, 0:16], in0=cur[:, 16:32],
                                                                                         in1=u[:, 16:32], op=ALU.subtract),
                                     reads=[curk, ('uP', c)], writes=[('pT', c)])
                        cnt = 0
                        for c in range(KC):
                            g, m = c // 4, c % 4
                            s = c % 2
                            P.op('sp', lambda e, s=s, c=c: e.dma_start(out=hin[s][:], in_=hT[c * 128:(c + 1) * 128, t0:t0 + N]),
                                 writes=[('hin', s)], dma='hin%d' % s)
                            for n in range(N // 512):
                                b = cnt % 8
                                cnt += 1
                                for kc in range(4):
                                    P.op('pe', lambda e, g=g, m=m, kc=kc, b=b, n=n: e.matmul(
                                        ps[b][:], wp[:, g, kc, m * 128:(m + 1) * 128], pT[:, 4 * g + kc, n * 512:(n + 1) * 512],
                                        start=(kc == 0), stop=(kc == 3)),
                                        reads=[('wp', g), ('pT', 4 * g + kc)], writes=[PK(b)])
                                P.op('dve', lambda e, s=s, b=b, c=c, n=n: e.scalar_tensor_tensor(
                                    out=hout[s][:, n * 512:(n + 1) * 512], in0=ps[b][:], scalar=gp[:, c:c + 1],
                                    in1=hin[s][:, n * 512:(n + 1) * 512], op0=ALU.mult, op1=ALU.add),
                                    reads=[PK(b), ('hin', s), 'gp'], writes=[('hout', s)])
                            P.op('sp', lambda e, s=s, c=c: e.dma_start(out=hT[c * 128:(c + 1) * 128, t0:t0 + N], in_=hout[s][:]),
                                 reads=[('hout', s)], dma='hout%d' % s)
                        P.emit()


        maskT = sb(gs, "maskT", [128, 128], F32)
        P.op('pool', lambda e: e.memset(maskT[:], 1.0), writes=['maskT'])
        P.op('pool', lambda e: e.affine_select(out=maskT[:], in_=maskT[:], pattern=[[1, 128]], compare_op=ALU.is_ge,
                                               fill=0.0, base=0, channel_multiplier=-1), reads=['maskT'], writes=['maskT'])
        P.emit()

        def lin_feat(uT, w2d, col0, nblk, N, epi, tag, kcn=KC, nbuf=3):
            with ExitStack() as st:
                wb = [sb(st, "lw%d" % i, [128, kcn, 128], BF16) for i in range(nbuf)]

                def load(j):
                    s_ = j % nbuf
                    src = w2d[:, col0 + j * 128: col0 + (j + 1) * 128].rearrange("(kc p) m -> p kc m", p=128)
                    P.op('pool', lambda e: e.dma_start(out=wb[s_][:], in_=src), writes=[('lw', s_)], dma='lw%d' % s_)
                for j in range(min(nbuf - 1, nblk)):
                    load(j)
                cnt = 0
                for j in range(nblk):
                    if j + nbuf - 1 < nblk:
                        load(j + nbuf - 1)
                    s_ = j % nbuf
                    for n in range(N // 512):
                        b = cnt % 4
                        cnt += 1
                        for kc in range(kcn):
                            P.op('pe', lambda e, s_=s_, b=b, kc=kc, n=n: e.matmul(
                                ps[b][:], wb[s_][:, kc, :], uT[:, kc, n * 512:(n + 1) * 512], start=(kc == 0), stop=(kc == kcn - 1)),
                                reads=[('lw', s_), (tag, kc)], writes=[PK(b)])
                        epi(j, n, b)
                P.emit()

        def lin_tok(uT, w2d, col0, nblk, N, dst_d, t0, tag):
            with ExitStack() as st:
                wb = [sb(st, "tw%d" % i, [128, KC, 512], BF16) for i in range(2)]
                vt = [sb(st, "vt%d" % i, [128, 512], BF16) for i in range(2)]
                cnt = 0
                for j in range(nblk):
                    s_ = j % 2
                    src = w2d[:, col0 + j * 512: col0 + (j + 1) * 512].rearrange("(kc p) m -> p kc m", p=128)
                    P.op('pool', lambda e, s_=s_, src=src: e.dma_start(out=wb[s_][:], in_=src), writes=[('tw', s_)], dma='tw%d' % s_)
                    for ti in range(N // 128):
                        b = 4 + cnt % 4
                        v_ = cnt % 2
                        cnt += 1
                        for kc in range(KC):
                            P.op('pe', lambda e, s_=s_, b=b, kc=kc, ti=ti: e.matmul(
                                ps[b][:], uT[:, kc, ti * 128:(ti + 1) * 128], wb[s_][:, kc, :], start=(kc == 0), stop=(kc == KC - 1)),
                                reads=[('tw', s_), (tag, kc)], writes=[PK(b)])
                        P.op('act', lambda e, v_=v_, b=b: e.copy(vt[v_][:], ps[b][:]), reads=[PK(b)], writes=[('vt', v_)])
                        P.op('sp', lambda e, v_=v_, ti=ti, j=j: e.dma_start(
                            out=dst_d[t0 + ti * 128:t0 + (ti + 1) * 128, j * 512:(j + 1) * 512], in_=vt[v_][:]),
                            reads=[('vt', v_)], dma='vt%d' % v_)
                P.emit()

        def out_proj(l, y_d, w2d, kcn):
            for seg in range(NT // SEG):
                t0 = seg * SEG
                with ExitStack() as st:
                    yT = sb(st, "yT", [128, kcn, SEG], BF16)
                    for kc in range(kcn):
                        P.op('sp', lambda e, kc=kc: e.dma_start(out=yT[:, kc, :], in_=y_d[kc * 128:(kc + 1) * 128, t0:t0 + SEG]),
                             writes=[('yT', kc)], dma='yl%d' % (kc % 4))
                    hin = [sb(st, "hin%d" % i, [128, SEG], F32) for i in range(2)]
                    hout = [sb(st, "hout%d" % i, [128, SEG], F32) for i in range(2)]

                    def epi(j, n, b):
                        s_ = j % 2
                        if n == 0:
                            P.op('sp', lambda e: e.dma_start(out=hin[s_][:], in_=hT[j * 128:(j + 1) * 128, t0:t0 + SEG]),
                                 writes=[('hin', s_)], dma='hin%d' % s_)
                        P.op('dve', lambda e: e.scalar_tensor_tensor(
                            out=hout[s_][:, n * 512:(n + 1) * 512], in0=ps[b][:], scalar=ada_col(l, 2, j),
                            in1=hin[s_][:, n * 512:(n + 1) * 512], op0=ALU.mult, op1=ALU.add),
                            reads=[PK(b), ('hin', s_), 'adac'], writes=[('hout', s_)])
                        if n == SEG // 512 - 1:
                            P.op('sp', lambda e: e.dma_start(out=hT[j * 128:(j + 1) * 128, t0:t0 + SEG], in_=hout[s_][:]),
                                 reads=[('hout', s_)], dma='hout%d' % s_)
                    lin_feat(yT, w2d, 0, KC, SEG, epi, 'yT', kcn=kcn, nbuf=2)

        def gla_chunks(H, nkc, nvc, Cc, qd_d, ki_d, ke_d, v_d, o_d, send_d, dec_d, dec_imm, slots):
            dv = nvc * 128
            nch = NT // Cc
            W = min(512, NT)
            cpw = W // Cc
            for h0 in range(0, H, slots):
                with ExitStack() as st:
                    hs = list(range(h0, min(H, h0 + slots)))
                    T = {}
                    for si, h in enumerate(hs):
                        t = {}
                        t['qd'] = sb(st, "qd", [128, nkc, NT], BF16)
                        t['ki'] = sb(st, "ki", [128, nkc, NT], BF16)
                        t['ke'] = sb(st, "ke", [128, nkc, NT], BF16)
                        t['v'] = sb(st, "v", [Cc, nch, dv], BF16)
                        t['S'] = sb(st, "S", [128, nkc, dv], F32)
                        t['Sb'] = sb(st, "Sb", [128, nkc, dv], BF16)
                        t['Pm'] = [sb(st, "Pm%d" % i, [Cc, Cc], BF16) for i in range(2)]
                        t['keT'] = [sb(st, "keT%d" % i, [Cc, nkc * 128], BF16) for i in range(2)]
                        t['ow'] = [sb(st, "ow%d" % i, [128, nvc, W], F32) for i in range(2)]
                        if dec_d is not None:
                            t['dec'] = sb(st, "dec", [128, nch], F32)
                        T[si] = t
                        k = lambda nm, si=si: (nm, si)
                        for kc in range(nkc):
                            r0 = (h * nkc + kc) * 128
                            P.op('sp', lambda e, t=t, kc=kc, r0=r0: e.dma_start(out=t['qd'][:, kc, :], in_=qd_d[r0:r0 + 128, :]),
                                 writes=[k('qd')], dma='g0')
                            P.op('sp', lambda e, t=t, kc=kc, r0=r0: e.dma_start(out=t['ki'][:, kc, :], in_=ki_d[r0:r0 + 128, :]),
                                 writes=[k('ki')], dma='g1')
                            P.op('sp', lambda e, t=t, kc=kc, r0=r0: e.dma_start(out=t['ke'][:, kc, :], in_=ke_d[r0:r0 + 128, :]),
                                 writes=[k('ke')], dma='g2')
                        P.op('sp', lambda e, t=t, h=h: e.dma_start(
                            out=t['v'][:], in_=v_d[:, h * dv:(h + 1) * dv].rearrange("(n p) d -> p n d", p=Cc)),
                            writes=[k('v')], dma='g3')
                        if dec_d is not None:
                            P.op('sp', lambda e, t=t, h=h: e.dma_start(out=t['dec'][:], in_=dec_d[h * 128:(h + 1) * 128, :]),
                                 writes=[k('dec')], dma='g5')
                        P.op('pool', lambda e, t=t: e.memset(t['S'][:], 0.0), writes=[k('S')])
                        P.op('pool', lambda e, t=t: e.memset(t['Sb'][:], 0.0), writes=[k('Sb')])
                    nb = 8 // len(hs)

                    def pre(si, n):
                        t = T[si]
                        k = lambda nm: (nm, si)
                        c0 = n * Cc
                        p_ = n % 2
                        bA, bB = si * nb, si * nb + 1
                        for kc in range(nkc):
                            P.op('pe', lambda e, kc=kc: e.matmul(
                                ps[bA][0:Cc, 0:Cc], t['ki'][:, kc, c0:c0 + Cc], t['qd'][:, kc, c0:c0 + Cc],
                                start=(kc == 0), stop=(kc == nkc - 1)), reads=[k('ki'), k('qd')], writes=[PK(bA)])
                        P.op('dve', lambda e: e.tensor_tensor(out=t['Pm'][p_][:], in0=ps[bA][0:Cc, 0:Cc],
                                                              in1=maskT[0:Cc, 0:Cc], op=ALU.mult),
                             reads=[PK(bA), 'maskT'], writes=[('Pm%d' % p_, si)])
                        psb = ps[bB].bitcast(BF16)
                        for kc in range(nkc):
                            P.op('pe', lambda e, kc=kc: e.transpose(
                                psb[0:Cc, kc * 128:(kc + 1) * 128], t['ke'][:, kc, c0:c0 + Cc], identb[:]),
                                reads=[k('ke'), 'identb'], writes=[PK(bB)])
                        P.op('act', lambda e: e.copy(t['keT'][p_][:], psb[0:Cc, 0:nkc * 128]),
                             reads=[PK(bB)], writes=[('keT%d' % p_, si)])

                    def main(si, n):
                        t = T[si]
                        h = hs[si]
                        k = lambda nm: (nm, si)
                        c0 = n * Cc
                        p_ = n % 2
                        bC = si * nb + 2
                        bD = [si * nb + 3 + kc for kc in range(nkc)]
                        wsl = (n // cpw) % 2
                        for vc in range(nvc):
                            P.op('pe', lambda e, vc=vc: e.matmul(
                                ps[bC][:, vc * Cc:(vc + 1) * Cc], t['v'][:, n, vc * 128:(vc + 1) * 128], t['Pm'][p_][:],
                                start=True, stop=False), reads=[k('v'), ('Pm%d' % p_, si)], writes=[PK(bC)])
                            for kc in range(nkc):
                                P.op('pe', lambda e, vc=vc, kc=kc: e.matmul(
                                    ps[bC][:, vc * Cc:(vc + 1) * Cc], t['Sb'][:, kc, vc * 128:(vc + 1) * 128],
                                    t['qd'][:, kc, c0:c0 + Cc], start=False, stop=(kc == nkc - 1)),
                                    reads=[k('Sb'), k('qd')], writes=[PK(bC)])
                        wo_ = (n % cpw) * Cc
                        P.op('act', lambda e: e.copy(
                            t['ow'][wsl][:, :, wo_:wo_ + Cc], ps[bC][:, 0:nvc * Cc].rearrange("p (a b) -> p a b", a=nvc)),
                            reads=[PK(bC)], writes=[('ow%d' % wsl, si)])
                        for kc in range(nkc):
                            P.op('pe', lambda e, kc=kc, b=bD[kc]: e.matmul(
                                ps[b][:, 0:dv], t['keT'][p_][:, kc * 128:(kc + 1) * 128], t['v'][:, n, :], start=True, stop=True),
                                reads=[('keT%d' % p_, si), k('v')], writes=[PK(bD[kc])])
                            dsc = t['dec'][:, n:n + 1] if dec_d is not None else float(dec_imm[h])
                            P.op('dve', lambda e, kc=kc, b=bD[kc], dsc=dsc: e.scalar_tensor_tensor(
                                out=t['S'][:, kc, :], in0=t['S'][:, kc, :], scalar=dsc, in1=ps[b][:, 0:dv],
                                op0=ALU.mult, op1=ALU.add), reads=[PK(bD[kc]), k('S'), k('dec')], writes=[k('S')])
                        P.op('act', lambda e: e.copy(t['Sb'][:], t['S'][:]), reads=[k('S')], writes=[k('Sb')])
                        if (n + 1) % cpw == 0:
                            w0 = (n + 1) * Cc - W
                            for vc in range(nvc):
                                r0 = (h * nvc + vc) * 128
                                P.op('sp', lambda e, vc=vc, r0=r0: e.dma_start(
                                    out=o_d[r0:r0 + 128, w0:w0 + W], in_=t['ow'][wsl][:, vc, :]),
                                    reads=[('ow%d' % wsl, si)], dma='go%d%d' % (si, wsl))

                    for si in range(len(hs)):
                        pre(si, 0)
                    for n in range(nch):
                        for si in range(len(hs)):
                            if n + 1 < nch:
                                pre(si, n + 1)
                            main(si, n)
                    for si, h in enumerate(hs):
                        t = T[si]
                        for kc in range(nkc):
                            r0 = (h * nkc + kc) * 128
                            P.op('sp', lambda e, t=t, kc=kc, r0=r0: e.dma_start(out=send_d[r0:r0 + 128, :], in_=t['Sb'][:, kc, :]),
                                 reads=[('Sb', si)], dma='g6')
                    P.emit()

        def exchange(send_d, recv_d, rows, step):
            for r0 in range(0, rows, step):
                P.op('pool', lambda e, r0=r0: e.collective_compute(
                    "AllGather", ALU.bypass, replica_groups=pairs, ins=[send_d[r0:r0 + step, :]],
                    outs=[recv_d[2 * r0:2 * r0 + 2 * step, :]]), dma='cc', inc=1)
            P.emit()

        def gla_post(H, nkc, nvc, qt_d, recv_d, rstep, o_d, sg_d, y_d, normg):
            dv = nvc * 128
            W = min(512, NT)
            with ExitStack() as st:
                Sr = [sb(st, "Sr%d" % i, [128, nkc, dv], BF16) for i in range(2)]
                qt = [sb(st, "qt%d" % i, [128, nkc, W], BF16) for i in range(2)]
                ow = [sb(st, "pow%d" % i, [128, nvc, W], F32) for i in range(2)]
                sgt = [sb(st, "psg%d" % i, [128, nvc, W], BF16) for i in range(2)]
                yt = [sb(st, "pyt%d" % i, [128, nvc, W], BF16) for i in range(2)]
                sq = sb(st, "psq", [128, nvc, W], F32)
                rs = sb(st, "prs", [128, W], F32)
                tmp = sb(st, "ptmp", [128, W], F32)
                cnt = 0
                for h in range(H):
                    hz = h % 2
                    for kc in range(nkc):
                        r = (h * nkc + kc) * 128
                        rr = (r // rstep) * 2 * rstep + (r % rstep)
                        P.op('sp', lambda e, hz=hz, kc=kc, rr=rr: e.dma_start(out=Sr[hz][:, kc, :], in_=recv_d[rr:rr + 128, :]),
                             writes=[('Sr', hz)], dma='p0%d' % hz)
                    P.op('dve', lambda e, hz=hz: e.tensor_scalar(Sr[hz][:], Sr[hz][:], flag[:, 0:1], None, op0=ALU.mult),
                         reads=[('Sr', hz), 'flag'], writes=[('Sr', hz)])
                    for wi_ in range(NT // W):
                        w0 = wi_ * W
                        z = cnt % 2
                        cnt += 1
                        for kc in range(nkc):
                            r = (h * nkc + kc) * 128
                            P.op('sp', lambda e, z=z, kc=kc, r=r, w0=w0: e.dma_start(out=qt[z][:, kc, :], in_=qt_d[r:r + 128, w0:w0 + W]),
                                 writes=[('qt', z)], dma='p1%d' % z)
                        for vc in range(nvc):
                            r = (h * nvc + vc) * 128
                            P.op('sp', lambda e, z=z, vc=vc, r=r, w0=w0: e.dma_start(out=ow[z][:, vc, :], in_=o_d[r:r + 128, w0:w0 + W]),
                                 writes=[('ow', z)], dma='p2%d' % z)
                            P.op('sp', lambda e, z=z, vc=vc, r=r, w0=w0: e.dma_start(out=sgt[z][:, vc, :], in_=sg_d[r:r + 128, w0:w0 + W]),
                                 writes=[('sgt', z)], dma='p3%d' % z)
                        for vc in range(nvc):
                            b = (cnt * nvc + vc) % 6
                            for kc in range(nkc):
                                P.op('pe', lambda e, hz=hz, z=z, vc=vc, kc=kc, b=b: e.matmul(
                                    ps[b][:, 0:W], Sr[hz][:, kc, vc * 128:(vc + 1) * 128], qt[z][:, kc, :],
                                    start=(kc == 0), stop=(kc == nkc - 1)), reads=[('Sr', hz), ('qt', z)], writes=[PK(b)])
                            P.op('dve', lambda e, z=z, vc=vc, b=b: e.tensor_tensor(out=ow[z][:, vc, :], in0=ow[z][:, vc, :],
                                                                                  in1=ps[b][:, 0:W], op=ALU.add),
                                 reads=[PK(b), ('ow', z)], writes=[('ow', z)])
                        P.op('act', lambda e, z=z: e.activation(out=sq[:], in_=ow[z][:], func=AF.Square),
                             reads=[('ow', z)], writes=['sq'])
                        bs = 6 + cnt % 2
                        for vc in range(nvc):
                            P.op('pe', lambda e, vc=vc, bs=bs: e.matmul(ps[bs][:, 0:W], ones32[:], sq[:, vc, :],
                                                                        start=(vc == 0), stop=(vc == nvc - 1)),
                                 reads=['sq', 'ones32'], writes=[PK(bs)])
                        P.op('act', lambda e, bs=bs: e.activation(out=rs[:], in_=ps[bs][:, 0:W], func=AF.Ln, scale=1.0 / dv, bias=epsc[:]),
                             reads=[PK(bs), 'epsc'], writes=['rs'])
                        P.op('act', lambda e: e.activation(out=rs[:], in_=rs[:], func=AF.Exp, scale=-0.5), reads=['rs'], writes=['rs'])
                        for vc in range(nvc):
                            gcol = normg[:, h * nvc + vc:h * nvc + vc + 1]
                            P.op('dve', lambda e, z=z, vc=vc, gcol=gcol: e.scalar_tensor_tensor(
                                out=tmp[:], in0=ow[z][:, vc, :], scalar=gcol, in1=rs[:], op0=ALU.mult, op1=ALU.mult),
                                reads=[('ow', z), 'rs'], writes=['tmp'])
                            P.op('dve', lambda e, z=z, vc=vc: e.tensor_tensor(out=yt[z][:, vc, :], in0=tmp[:], in1=sgt[z][:, vc, :], op=ALU.mult),
                                 reads=['tmp', ('sgt', z)], writes=[('yt', z)])
                            r = (h * nvc + vc) * 128
                            P.op('sp', lambda e, z=z, vc=vc, r=r, w0=w0: e.dma_start(out=y_d[r:r + 128, w0:w0 + W], in_=yt[z][:, vc, :]),
                                 reads=[('yt', z)], dma='p4%d' % z)
                P.emit()

        def hgrn_layer(l):
            dram = lambda n, sh, dt: nc.dram_tensor(n, sh, dt, kind=("ExternalOutput" if DEBUG else "Internal")).ap()
            qd_d = dram("h_qd", [D, NT], BF16)
            ki_d = dram("h_ki", [D, NT], BF16)
            ke_d = dram("h_ke", [D, NT], BF16)
            v_d = dram("h_v", [NT, D], BF16)
            sg_d = dram("h_sg", [D, NT], BF16)
            y_d = dram("h_y", [D, NT], BF16)
            dec_d = dram("h_dec", [D, NT // 32], F32)
            qt_d = dram("h_qt", [D, NT], BF16)
            o_d = dram("h_o", [D, NT], F32)
            send_d = dram("h_send", [D, 128], BF16)
            recv_d = dram("h_recv", [2 * D, 128], BF16)
            with ExitStack() as st0:
                lb = sb(st0, "lb", [128, 16], F32)
                oml = sb(st0, "oml", [128, 16], F32)
                noml = sb(st0, "noml", [128, 16], F32)
                ex = sb(st0, "ex", [128, 64], F32)
                m32 = sb(st0, "m32", [128, 512], F32)
                P.op('act', lambda e: e.activation(out=ex[:], in_=lbl[:], func=AF.Exp), reads=['lbl'], writes=['ex'])
                P.op('dve', lambda e: e.tensor_tensor(out=lb[:], in0=ex[:, 0:16], in1=ex[:, 16:32], op=ALU.add), reads=['ex'], writes=['lb'])
                P.op('dve', lambda e: e.tensor_tensor(out=lb[:], in0=lb[:], in1=ex[:, 32:48], op=ALU.add), reads=['ex', 'lb'], writes=['lb'])
                P.op('dve', lambda e: e.tensor_tensor(out=lb[:], in0=lb[:], in1=ex[:, 48:64], op=ALU.add), reads=['ex', 'lb'], writes=['lb'])
                P.op('dve', lambda e: e.reciprocal(lb[:], lb[:]), reads=['lb'], writes=['lb'])
                P.op('dve', lambda e: e.tensor_tensor(out=oml[:], in0=ex[:, 16:32], in1=lb[:], op=ALU.mult), reads=['ex', 'lb'], writes=['oml'])
                for i in range(2, l + 1):
                    P.op('dve', lambda e, i=i: e.scalar_tensor_tensor(out=oml[:], in0=ex[:, 16 * i:16 * i + 16], scalar=1.0, in1=lb[:],
                                                                      op0=ALU.mult, op1=ALU.mult), reads=['ex', 'lb'], writes=['noml'])
                    P.op('dve', lambda e: e.tensor_tensor(out=oml[:], in0=oml[:], in1=noml[:], op=ALU.add), reads=['noml', 'oml'], writes=['oml'])
                P.op('dve', lambda e: e.tensor_copy(lb[:], oml[:]), reads=['oml'], writes=['lb'])
                P.op('dve', lambda e: e.tensor_scalar(oml[:], lb[:], -1.0, 1.0, op0=ALU.mult, op1=ALU.add), reads=['lb'], writes=['oml'])
                P.op('dve', lambda e: e.tensor_scalar(noml[:], oml[:], -1.0, None, op0=ALU.mult), reads=['oml'], writes=['noml'])
                o512 = sb(st0, "o512", [128, 512], F32)
                cBl = sb(st0, "cBl", [128, 16], F32)
                P.op('pool', lambda e: e.memset(o512[:], 1.0), writes=['o512'])
                P.op('pool', lambda e: e.memset(cBl[:], 0.0), writes=['cBl'])
                P.op('pool', lambda e: e.memset(m32[:], 1.0), writes=['m32'])
                P.op('pool', lambda e: e.memset(m32[:].rearrange("p (a b) -> p a b", b=32)[:, :, 0:1], 0.0), writes=['m32'])
                P.emit()
                for seg in range(NT // SEG):
                    t0 = seg * SEG
                    with ExitStack() as st:
                        uT = sb(st, "uT", [128, KC, SEG], BF16)
                        normmod(st, t0, SEG, lambda kc: modA[:, (l * 2) * 16 + kc:(l * 2) * 16 + kc + 1],
                                lambda kc: ada_col(l, 0, kc), uT, 'uT')
                        with ExitStack() as st2:
                            F = lambda nm: [sb(st2, nm + "%d" % i, [128, 512], F32) for i in range(2)]
                            Bf = lambda nm: [sb(st2, nm + "%d" % i, [128, 512], BF16) for i in range(2)]
                            sgm, lf, kk, cum, ec, ei, ee = F("sgm"), F("lf"), F("kk"), F("cum"), F("ec"), F("ei"), F("ee")
                            qd, ki, ke = Bf("qd"), Bf("ki"), Bf("ke")
                            cB, eB, qtb = F("cB"), F("eB"), Bf("qtb")
                            dcs = [sb(st2, "dcs%d" % i, [128, 16], F32) for i in range(2)]
                            wq = [sb(st2, "wq%d" % i, [128, KC, 128], BF16) for i in range(2)]
                            wf = [sb(st2, "wf%d" % i, [128, KC, 128], BF16) for i in range(2)]
                            cnt = 0
                            for h in range(16):
                                s_ = h % 2
                                for wt, c0, nm in ((wq, h * 128, 'wq'), (wf, 2048 + h * 128, 'wf')):
                                    src = hgrn_w_in[:, c0:c0 + 128].rearrange("(kc p) m -> p kc m", p=128)
                                    P.op('pool', lambda e, wt=wt, src=src, s_=s_: e.dma_start(out=wt[s_][:], in_=src),
                                         writes=[(nm, s_)], dma=nm + '%d' % s_)
                                for n in range(SEG // 512):
                                    bq, bf_ = (cnt * 2) % 8, (cnt * 2) % 8 + 1
                                    z = cnt % 2
                                    cnt += 1
                                    K_ = lambda nm, z=z: (nm, z)
                                    for kc in range(KC):
                                        P.op('pe', lambda e, s_=s_, kc=kc, n=n, bq=bq: e.matmul(
                                            ps[bq][:], wq[s_][:, kc, :], uT[:, kc, n * 512:(n + 1) * 512], start=(kc == 0), stop=(kc == KC - 1)),
                                            reads=[('wq', s_), ('uT', kc)], writes=[PK(bq)])
                                    for kc in range(KC):
                                        P.op('pe', lambda e, s_=s_, kc=kc, n=n, bf_=bf_: e.matmul(
                                            ps[bf_][:], wf[s_][:, kc, :], uT[:, kc, n * 512:(n + 1) * 512], start=(kc == 0), stop=(kc == KC - 1)),
                                            reads=[('wf', s_), ('uT', kc)], writes=[PK(bf_)])
                                    P.op('act', lambda e, z=z, bf_=bf_: e.activation(out=sgm[z][:], in_=ps[bf_][:], func=AF.Exp, scale=-1.0),
                                         reads=[PK(bf_)], writes=[K_('sgm')])
                                    P.op('dve', lambda e, z=z: e.tensor_scalar(sgm[z][:], sgm[z][:], 1.0, None, op0=ALU.add),
                                         reads=[K_('sgm')], writes=[K_('sgm')])
                                    P.op('dve', lambda e, z=z: e.reciprocal(sgm[z][:], sgm[z][:]), reads=[K_('sgm')], writes=[K_('sgm')])
                                    P.op('act', lambda e, z=z, h=h: e.activation(out=lf[z][:], in_=sgm[z][:], func=AF.Ln,
                                                                                 scale=oml[:, h:h + 1], bias=lb[:, h:h + 1]),
                                         reads=[K_('sgm'), 'oml', 'lb'], writes=[K_('lf')])
                                    P.op('dve', lambda e, z=z, h=h: e.tensor_scalar(kk[z][:], sgm[z][:], noml[:, h:h + 1], oml[:, h:h + 1],
                                                                                   op0=ALU.mult, op1=ALU.add),
                                         reads=[K_('sgm'), 'oml', 'noml'], writes=[K_('kk')])
                                    P.op('dve', lambda e, z=z: e.tensor_tensor_scan(cum[z][:], m32[:], lf[z][:], 0.0, op0=ALU.mult, op1=ALU.add),
                                         reads=[K_('lf'), 'm32'], writes=[K_('cum')])
                                    P.op('act', lambda e, z=z: e.activation(out=ec[z][:], in_=cum[z][:], func=AF.Exp),
                                         reads=[K_('cum')], writes=[K_('ec')])
                                    P.op('act', lambda e, z=z: e.activation(out=ei[z][:], in_=cum[z][:], func=AF.Exp, scale=-1.0),
                                         reads=[K_('cum')], writes=[K_('ei')])
                                    c3 = lambda a: a[:].rearrange("p (a b) -> p a b", b=32)
                                    P.op('dve', lambda e, z=z: e.tensor_tensor(out=c3(ee[z]), in0=c3(cum[z])[:, :, 31:32].broadcast_to([128, 16, 32]),
                                                                               in1=c3(cum[z]), op=ALU.subtract),
                                         reads=[K_('cum')], writes=[K_('ee')])
                                    P.op('act', lambda e, z=z: e.activation(out=ee[z][:], in_=ee[z][:], func=AF.Exp),
                                         reads=[K_('ee')], writes=[K_('ee')])
                                    P.op('dve', lambda e, z=z, bq=bq: e.tensor_tensor(out=qd[z][:], in0=ps[bq][:], in1=ec[z][:], op=ALU.mult),
                                         reads=[PK(bq), K_('ec')], writes=[K_('qd')])
                                    P.op('dve', lambda e, z=z: e.tensor_tensor(out=ki[z][:], in0=kk[z][:], in1=ei[z][:], op=ALU.mult),
                                         reads=[K_('kk'), K_('ei')], writes=[K_('ki')])
                                    P.op('dve', lambda e, z=z: e.tensor_tensor(out=ke[z][:], in0=kk[z][:], in1=ee[z][:], op=ALU.mult),
                                         reads=[K_('kk'), K_('ee')], writes=[K_('ke')])
                                    P.op('act', lambda e, z=z: e.copy(dcs[z][:], c3(ec[z])[:, :, 31]), reads=[K_('ec')], writes=[K_('dcs')])
                                    tt = t0 + n * 512
                                    P.op('dve', lambda e, z=z, h=h: e.tensor_tensor_scan(cB[z][:], o512[:], lf[z][:], cBl[:, h:h + 1],
                                                                                       op0=ALU.mult, op1=ALU.add),
                                         reads=[K_('lf'), 'o512', 'cBl'], writes=[K_('cB')])
                                    P.op('act', lambda e, z=z: e.activation(out=eB[z][:], in_=cB[z][:], func=AF.Exp),
                                         reads=[K_('cB')], writes=[K_('eB')])
                                    P.op('dve', lambda e, z=z, h=h: e.tensor_copy(cBl[:, h:h + 1], cB[z][:, 511:512]),
                                         reads=[K_('cB')], writes=['cBl'])
                                    P.op('dve', lambda e, z=z, bq=bq: e.tensor_tensor(out=qtb[z][:], in0=ps[bq][:], in1=eB[z][:], op=ALU.mult),
                                         reads=[PK(bq), K_('eB')], writes=[K_('qtb')])
                                    P.op('sp', lambda e, z=z, h=h, tt=tt: e.dma_start(out=qt_d[h * 128:(h + 1) * 128, tt:tt + 512], in_=qtb[z][:]),
                                         reads=[K_('qtb')], dma='oqt%d' % z)
                                    for buf, dd, nm in ((qd, qd_d, 'qd'), (ki, ki_d, 'ki'), (ke, ke_d, 'ke')):
                                        P.op('sp', lambda e, z=z, buf=buf, dd=dd, h=h, tt=tt: e.dma_start(
                                            out=dd[h * 128:(h + 1) * 128, tt:tt + 512], in_=buf[z][:]),
                                            reads=[K_(nm)], dma='o' + nm + '%d' % z)
                                    P.op('sp', lambda e, z=z, h=h, tt=tt: e.dma_start(
                                        out=dec_d[h * 128:(h + 1) * 128, tt // 32:tt // 32 + 16], in_=dcs[z][:]),
                                        reads=[K_('dcs')], dma='odc%d' % z)
                            P.emit()
                        lin_tok(uT, hgrn_w_in, 4096, 4, SEG, v_d, t0, 'uT')
                        with ExitStack() as st2:
                            sgo = [sb(st2, "sgo%d" % i, [128, 512], BF16) for i in range(2)]

                            def epi(j, n, b):
                                z = (j * 2 + n) % 2
                                P.op('act', lambda e: e.activation(out=sgo[z][:], in_=ps[b][:], func=AF.Silu),
                                     reads=[PK(b)], writes=[('sgo', z)])
                                P.op('sp', lambda e: e.dma_start(out=sg_d[j * 128:(j + 1) * 128, t0 + n * 512:t0 + (n + 1) * 512], in_=sgo[z][:]),
                                     reads=[('sgo', z)], dma='osg%d' % z)
                            lin_feat(uT, hgrn_w_in, 6144, 16, SEG, epi, 'uT')
                gla_chunks(16, 1, 1, 32, qd_d, ki_d, ke_d, v_d, o_d, send_d, dec_d, None, slots=2)
                exchange(send_d, recv_d, D, D)
                gla_post(16, 1, 1, qt_d, recv_d, D, o_d, sg_d, y_d, hng)
                out_proj(l, y_d, hgrn_w_out, 16)


        def ret_layer(l):
            dram = lambda n, sh, dt: nc.dram_tensor(n, sh, dt, kind=("ExternalOutput" if DEBUG else "Internal")).ap()
            qd_d = dram("r_qd", [D, NT], BF16)
            ki_d = dram("r_ki", [D, NT], BF16)
            ke_d = dram("r_ke", [D, NT], BF16)
            v_d = dram("r_v", [NT, 4096], BF16)
            sg_d = dram("r_sg", [4096, NT], BF16)
            y_d = dram("r_y", [4096, NT], BF16)
            qt_d = dram("r_qt", [D, NT], BF16)
            o_d = dram("r_o", [4096, NT], F32)
            send_d = dram("r_send", [D, 512], BF16)
            recv_d = dram("r_recv", [2 * D, 512], BF16)
            TWO_PI = 2.0 * math.pi
            PI_ = 3.1415925
            with ExitStack() as st0:
                invf = sb(st0, "invf", [128, 1], F32)
                gtab = sb(st0, "gtab", [128, 8, 3, 128], F32)
                P.op('sp', lambda e: e.dma_start(out=invf[:], in_=invf_d), writes=['invf'], dma='c0')
                P.op('sp', lambda e: e.dma_start(out=gtab[:].rearrange("p a b c -> p (a b c)"), in_=gtab_d.broadcast_to([128, 8 * 3 * 128])),
                     writes=['gtab'], dma='c1')
                P.emit()
                for seg in range(NT // SEG):
                    t0 = seg * SEG
                    with ExitStack() as st:
                        uT = sb(st, "uT", [128, KC, SEG], BF16)
                        normmod(st, t0, SEG, lambda kc: modA[:, (l * 2) * 16 + kc:(l * 2) * 16 + kc + 1],
                                lambda kc: ada_col(l, 0, kc), uT, 'uT')
                        with ExitStack() as st2:
                            cs = sb(st2, "cs", [128, SEG], F32)
                            sn = sb(st2, "sn", [128, SEG], F32)
                            posi = sb(st2, "posi", [128, SEG], I32)
                            ang = sb(st2, "ang", [128, SEG], F32)
                            w1 = sb(st2, "w1", [128, SEG], F32)
                            wi = sb(st2, "wi", [128, SEG], I32)
                            P.op('sp', lambda e: e.dma_start(out=posi[:], in_=pos[0:1, t0:t0 + SEG].broadcast_to([128, SEG])),
                                 writes=['posi'], dma='c0')
                            P.op('dve', lambda e: e.tensor_copy(ang[:], posi[:]), reads=['posi'], writes=['ang'])
                            P.op('dve', lambda e: e.tensor_scalar(ang[:], ang[:], invf[:, 0:1], None, op0=ALU.mult),
                                 reads=['ang', 'invf'], writes=['ang'])
                            for off, dst, dk_ in ((0.0, sn, 'sn'), (0.5 * math.pi, cs, 'cs')):
                                P.op('dve', lambda e, off=off: e.tensor_scalar(w1[:], ang[:], off, 1.0 / TWO_PI, op0=ALU.add, op1=ALU.mult),
                                     reads=['ang'], writes=['w1'])
                                P.op('dve', lambda e: e.tensor_copy(wi[:], w1[:]), reads=['w1'], writes=['wi'])
                                P.op('dve', lambda e: e.tensor_copy(w1[:], wi[:]), reads=['wi'], writes=['w1'])
                                P.op('dve', lambda e: e.tensor_scalar(w1[:], w1[:], -TWO_PI, None, op0=ALU.mult), reads=['w1'], writes=['w1'])
                                P.op('dve', lambda e, off=off: e.scalar_tensor_tensor(out=w1[:], in0=ang[:], scalar=off, in1=w1[:],
                                                                                      op0=ALU.add, op1=ALU.add),
                                     reads=['w1', 'ang'], writes=['w1'])
                                P.op('dve', lambda e: e.tensor_scalar(w1[:], w1[:], -PI_, PI_, op0=ALU.max, op1=ALU.min),
                                     reads=['w1'], writes=['w1'])
                                P.op('act', lambda e, dst=dst: e.activation(out=dst[:], in_=w1[:], func=AF.Sin), reads=['w1'], writes=[dk_])
                            F = lambda nm, n_: [sb(st2, nm + "%d" % i, [128, 512], F32) for i in range(n_)]
                            Bf = lambda nm, n_: [sb(st2, nm + "%d" % i, [128, 512], BF16) for i in range(n_)]
                            ta, tb, tc_, td = F("ta", 2), F("tb", 2), F("tc", 2), F("td", 2)
                            r1, r2 = F("r1", 2), F("r2", 2)
                            ob = Bf("ob", 8)
                            g2 = F("g2", 2)
                            wr = [sb(st2, "wr%d" % i, [128, KC, 128], BF16) for i in range(8)]
                            cnt = 0
                            oc = 0
                            for h in range(8):
                                hs_ = (h % 2) * 4
                                cols = [h * 256, h * 256 + 128, 2048 + h * 256, 2048 + h * 256 + 128]
                                for i, c0 in enumerate(cols):
                                    src = ret_w_in[:, c0:c0 + 128].rearrange("(kc p) m -> p kc m", p=128)
                                    P.op('pool', lambda e, src=src, i=i, hs_=hs_: e.dma_start(out=wr[hs_ + i][:], in_=src),
                                         writes=[('wr', hs_ + i)], dma='wr%d' % (hs_ + i))
                                for n in range(SEG // 512):
                                    csn = cs[:, n * 512:(n + 1) * 512]
                                    snn = sn[:, n * 512:(n + 1) * 512]
                                    for qk in range(2):
                                        b1, b2 = (cnt * 2) % 8, (cnt * 2) % 8 + 1
                                        z = cnt % 2
                                        cnt += 1
                                        for bb, wi_ in ((b1, hs_ + qk * 2), (b2, hs_ + qk * 2 + 1)):
                                            for kc in range(KC):
                                                P.op('pe', lambda e, bb=bb, wi_=wi_, kc=kc, n=n: e.matmul(
                                                    ps[bb][:], wr[wi_][:, kc, :], uT[:, kc, n * 512:(n + 1) * 512],
                                                    start=(kc == 0), stop=(kc == KC - 1)),
                                                    reads=[('wr', wi_), ('uT', kc)], writes=[PK(bb)])
                                        for dst, bb, tr, nm in ((ta, b1, csn, 'ta'), (tb, b2, snn, 'tb'), (tc_, b1, snn, 'tc'), (td, b2, csn, 'td')):
                                            P.op('dve', lambda e, dst=dst, bb=bb, tr=tr, z=z: e.tensor_tensor(out=dst[z][:], in0=ps[bb][:], in1=tr, op=ALU.mult),
                                                 reads=[PK(bb), 'cs', 'sn'], writes=[(nm, z)])
                                        P.op('dve', lambda e, z=z: e.tensor_tensor(out=r1[z][:], in0=ta[z][:], in1=tb[z][:], op=ALU.subtract),
                                             reads=[('ta', z), ('tb', z)], writes=[('r1', z)])
                                        P.op('dve', lambda e, z=z: e.tensor_tensor(out=r2[z][:], in0=tc_[z][:], in1=td[z][:], op=ALU.add),
                                             reads=[('tc', z), ('td', z)], writes=[('r2', z)])
                                        tt = t0 + n * 512
                                        outs = [(0, qd_d)] if qk == 0 else [(1, ki_d), (2, ke_d)]
                                        if qk == 0:
                                            P.op('sp', lambda e, z=z, h=h, tt=tt: e.dma_start(
                                                out=g2[z][:], in_=gq2_d[h:h + 1, tt:tt + 512].broadcast_to([128, 512])),
                                                writes=[('g2', z)], dma='g2%d' % z)
                                            for half, rr, rn in ((0, r1, 'r1'), (1, r2, 'r2')):
                                                o_ = oc % 8
                                                oc += 1
                                                P.op('pool', lambda e, o_=o_, rr=rr, z=z: e.tensor_tensor(
                                                    out=ob[o_][:], in0=rr[z][:], in1=g2[z][:], op=ALU.mult),
                                                    reads=[(rn, z), ('g2', z)], writes=[('ob', o_)])
                                                r0 = h * 256 + half * 128
                                                P.op('sp', lambda e, o_=o_, r0=r0, tt=tt: e.dma_start(
                                                    out=qt_d[r0:r0 + 128, tt:tt + 512], in_=ob[o_][:]), reads=[('ob', o_)], dma='ob%d' % o_)
                                        for gi, dd in outs:
                                            for half, rr, rn in ((0, r1, 'r1'), (1, r2, 'r2')):
                                                o_ = oc % 8
                                                oc += 1
                                                gv = gtab[:, h, gi, :]
                                                P.op('dve', lambda e, o_=o_, rr=rr, z=z, gv=gv: e.tensor_tensor(
                                                    out=ob[o_][:].rearrange("p (a b) -> p a b", b=128),
                                                    in0=rr[z][:].rearrange("p (a b) -> p a b", b=128),
                                                    in1=gv.unsqueeze(1).broadcast_to([128, 4, 128]), op=ALU.mult),
                                                    reads=[(rn, z), 'gtab'], writes=[('ob', o_)])
                                                r0 = h * 256 + half * 128
                                                P.op('sp', lambda e, o_=o_, dd=dd, r0=r0, tt=tt: e.dma_start(
                                                    out=dd[r0:r0 + 128, tt:tt + 512], in_=ob[o_][:]), reads=[('ob', o_)], dma='ob%d' % o_)
                            P.emit()
                        lin_tok(uT, ret_w_in, 4096, 8, SEG, v_d, t0, 'uT')
                        with ExitStack() as st2:
                            sgo = [sb(st2, "sgo%d" % i, [128, 512], BF16) for i in range(2)]

                            def epi(j, n, b):
                                z = (j * 2 + n) % 2
                                P.op('act', lambda e: e.activation(out=sgo[z][:], in_=ps[b][:], func=AF.Silu),
                                     reads=[PK(b)], writes=[('sgo', z)])
                                P.op('sp', lambda e: e.dma_start(out=sg_d[j * 128:(j + 1) * 128, t0 + n * 512:t0 + (n + 1) * 512], in_=sgo[z][:]),
                                     reads=[('sgo', z)], dma='osg%d' % z)
                            lin_feat(uT, ret_w_in, 8192, 32, SEG, epi, 'uT')
            gam = [1.0 - 2.0 ** (-5.0 - h) for h in range(8)]
            gla_chunks(8, 2, 4, 128, qd_d, ki_d, ke_d, v_d, o_d, send_d, None, [g_ ** 128 for g_ in gam], slots=1)
            exchange(send_d, recv_d, D, 1024)
            gla_post(8, 2, 4, qt_d, recv_d, 1024, o_d, sg_d, y_d, rng_)
            out_proj(l, y_d, ret_w_out, 32)

        C.P, C.sb, C.ps, C.PK, C.hT, C.hT3 = P, sb, ps, PK, hT, hT3
        C.normmod, C.ada_col, C.modA = normmod, ada_col, modA

        for l in layers:
            if do_mixer:
                if l % 3 == 0:
                    pool_layer(l)
                elif l % 3 == 1:
                    hgrn_layer(l)
                else:
                    ret_layer(l)
            if do_ffn:
                ffn_layer(l)

        with ExitStack() as st:
            FN = min(512, NT)
            for seg in range(NT // FN):
                t0 = seg * FN
                with ExitStack() as st2:
                    yT = sb(st2, "yT", [128, KC, FN], F32)
                    z0 = sb(st2, "z0", [128, 1], F32)
                    P.op('pool', lambda e: e.memset(z0[:], 0.0), writes=['z0'])
                    normmod(st2, t0, FN, lambda kc: fng[:, kc:kc + 1], lambda kc: z0[:], yT, 'yT')
                    ot = [sb(st2, "ot%d" % i, [128, D], F32) for i in range(2)]
                    for ti in range(FN // 128):
                        s = ti % 2
                        for q in range(4):
                            b = (ti * 4 + q) % 8
                            for i in range(4):
                                c = q * 4 + i
                                P.op('pe', lambda e, b=b, i=i, c=c, ti=ti: e.transpose(
                                    ps[b][:, i * 128:(i + 1) * 128], yT[:, c, ti * 128:(ti + 1) * 128], ident[:]),
                                    reads=[('yT', c), 'ident'], writes=[PK(b)])
                            if q % 2 == 0:
                                P.op('dve', lambda e, s=s, b=b, q=q: e.tensor_copy(ot[s][:, q * 512:(q + 1) * 512], ps[b][:]),
                                     reads=[PK(b)], writes=[('ot', s)])
                            else:
                                P.op('act', lambda e, s=s, b=b, q=q: e.copy(ot[s][:, q * 512:(q + 1) * 512], ps[b][:]),
                                     reads=[PK(b)], writes=[('ot', s)])
                        P.op('sp', lambda e, s=s, ti=ti: e.dma_start(out=out[t0 + ti * 128:t0 + (ti + 1) * 128, :], in_=ot[s][:]),
                             reads=[('ot', s)], dma='ot%d' % s)
                    P.emit()
    return nc


_IDENT = np.eye(128, dtype=np.float32)
_INVF = (np.float32(10000.0) ** (-np.arange(128, dtype=np.float32) / np.float32(128))).astype(np.float32).reshape(128, 1)
_lg = np.log(1.0 - 2.0 ** (-5.0 - np.arange(8, dtype=np.float64)))
_t = np.arange(128, dtype=np.float64)
_GTAB = np.stack([np.exp(_lg[:, None] * (_t + 1.0)), np.exp(-_lg[:, None] * (_t + 1.0)) / 16.0,
                  np.exp(_lg[:, None] * (127.0 - _t)) / 16.0], axis=1).astype(np.float32).reshape(1, 8 * 3 * 128)


_GQ2 = np.exp(_lg[:, None] * (np.arange(4096, dtype=np.float64)[None, :] + 1.0)).astype(np.float32)


def make_in_map(inputs, b, t0, NT):
    g = lambda k: np.ascontiguousarray(np.asarray(inputs[k]))
    hs = 1 if t0 > 0 else 0
    m = {
        "x": np.ascontiguousarray(g("x")[b, t0:t0 + NT]),
        "c": g("c")[b].reshape(KC, 128),
        "positions": np.ascontiguousarray(g("positions")[b:b + 1, t0:t0 + NT]).astype(np.int32),
        "w_ada": np.ascontiguousarray(g("w_ada")[hs * 2:hs * 2 + 2]), "b_ada": np.ascontiguousarray(g("b_ada")[hs * 2:hs * 2 + 2]),
        "norm_mix_g": g("norm_mix_g").reshape(DEPTH * KC, 128),
        "norm_ffn_g": g("norm_ffn_g").reshape(DEPTH * KC, 128),
        "pool_w": g("pool_w"), "pool_scale": g("pool_scale").reshape(2 * KC, 128),
        "hgrn_w_in": g("hgrn_w_in")[0], "hgrn_lb_logits": g("hgrn_lb_logits").reshape(DEPTH * KC, 128),
        "hgrn_norm_g": g("hgrn_norm_g").reshape(KC, 128), "hgrn_w_out": g("hgrn_w_out")[0],
        "ret_w_in": g("ret_w_in")[0], "ret_norm_g": g("ret_norm_g").reshape(32, 128), "ret_w_out": g("ret_w_out")[0],
        "ffn_w_in": g("ffn_w_in"), "ffn_w_out": g("ffn_w_out"),
        "final_norm_g": g("final_norm_g").reshape(KC, 128),
        "ident": _IDENT, "invf": _INVF, "gtab": _GTAB,
        "gq2": np.ascontiguousarray(_GQ2[:, :NT]),
        "flag": np.full((128, 1), 1.0 if t0 > 0 else 0.0, np.float32),
        "xh": (np.ascontiguousarray(g("x")[b, t0 - 16:t0]) if t0 > 0 else np.zeros((16, D), np.float32)),
    }
    return m


def kernel(**inputs):
    B, S, _ = inputs["x"].shape
    NT = S // 2
    nc = build_program(NT)
    in_maps = [make_in_map(inputs, c // 2, (c % 2) * NT, NT) for c in range(8)]
    res = run_bass_kernel_spmd(nc, in_maps, core_ids=list(range(8)))
    out = np.empty((B, S, D), np.float32)
    for c in range(8):
        out[c // 2, (c % 2) * NT:(c % 2 + 1) * NT] = np.asarray(res.results[c]["out"])
    return out
```
